# Optimizing a Trainium2 kernel written in Bass

```python
import jax, jax.numpy as jnp
from jax import lax
import numpy as np

D_MODEL = 1024
BATCH = 16
SEQ = 2048
DEPTH = 2

CHUNK = 64
NORM_EPS = 1e-6
ROPE_THETA = 10000.0

RWKV_HEADS = 8
RWKV_HEAD_DIM = 64
RWKV_WIDTH = RWKV_HEADS * RWKV_HEAD_DIM
DECAY_LORA = 64
A_LORA = 64
GATE_LORA = 128
GN_EPS = 64e-5
LRU_HEADS = 8
LRU_WIDTH = D_MODEL // 2
LRU_BLOCK = LRU_WIDTH // LRU_HEADS
CONV_WIDTH = 4
LRU_C = 8.0
MIX_WIDTH = RWKV_WIDTH + LRU_WIDTH
RWKV_COLS = (RWKV_WIDTH, RWKV_WIDTH, RWKV_WIDTH, DECAY_LORA, A_LORA, GATE_LORA)
RWKV_PROJ = sum(RWKV_COLS)
IN0_COLS = RWKV_COLS + (LRU_WIDTH, LRU_WIDTH)
IN0_WIDTH = sum(IN0_COLS)

ATT_HEADS = 16
ATT_HEAD_DIM = 64
ATT_WIDTH = ATT_HEADS * ATT_HEAD_DIM
IDX_HEADS = 8
IDX_HEAD_DIM = 64
TOPK_MAX = 256
Q_BLOCK = 128
IN1_COLS = (ATT_WIDTH, ATT_HEAD_DIM, ATT_HEAD_DIM, IDX_HEADS * IDX_HEAD_DIM, IDX_HEAD_DIM, IDX_HEADS)
IN1_WIDTH = sum(IN1_COLS)

FFN_HIDDEN = -(-8 * D_MODEL // (3 * 256)) * 256
N_EVEN = (DEPTH + 1) // 2
N_ODD = DEPTH // 2

kernel_name = 'hybrid_rwkv7_rglru_dsa_encoder'


def split_cols(t, sizes):
    return jnp.split(t, np.cumsum(sizes)[:-1].tolist(), axis=-1)


def rmsnorm(x, g):
    xf = x.astype(jnp.float32)
    y = xf * lax.rsqrt(jnp.mean(xf * xf, axis=-1, keepdims=True) + NORM_EPS)
    return (y * g.astype(jnp.float32)).astype(x.dtype)


def rope_tables(seq, dim):
    inv_freq = ROPE_THETA ** (-jnp.arange(0, dim, 2, dtype=jnp.float32) / dim)
    ang = jnp.arange(seq, dtype=jnp.float32)[:, None] * inv_freq[None, :]
    return jnp.cos(ang), jnp.sin(ang)


def apply_rope(t, cos, sin):
    tf = t.astype(jnp.float32)
    t1, t2 = jnp.split(tf, 2, axis=-1)
    c, s = cos[None, :, None, :], sin[None, :, None, :]
    return jnp.concatenate([t1 * c - t2 * s, t1 * s + t2 * c], axis=-1).astype(t.dtype)


def swiglu(h, w_gate, w_up, w_down):
    return (jax.nn.silu(h @ w_gate) * (h @ w_up)) @ w_down


def token_shift(p, mu):
    prev = jnp.pad(p, ((0, 0), (1, 0), (0, 0)))[:, :-1]
    return p + (prev - p) * mu


def rwkv7_scan(r, decay, k, v, kk, a):
    B, _, H, N = r.shape

    def step(state, inp):
        r_t, w_t, k_t, v_t, kk_t, a_t = inp
        sa = jnp.einsum('bhvk,bhk->bhv', state, -kk_t)
        state = (state * w_t[:, :, None, :]
                 + sa[..., None] * (kk_t * a_t)[:, :, None, :]
                 + v_t[..., None] * k_t[:, :, None, :])
        return state, jnp.einsum('bhvk,bhk->bhv', state, r_t)

    xs = tuple(jnp.moveaxis(t, 1, 0) for t in (r, decay, k, v, kk, a))
    _, ys = lax.scan(step, jnp.zeros((B, H, N, N), jnp.float32), xs)
    return jnp.moveaxis(ys, 0, 1)


def linear_scan_combine(c1, c2):
    a1, b1 = c1
    a2, b2 = c2
    return a1 * a2, a2 * b1 + b2


def rwkv_lru_mixer(h, w_in, mu_shift, w_decay0, w_decay_up, a_bias, w_a_up, w_g_up, k_k, k_a, r_k,
                   lnx_w, lnx_b, conv_w, conv_b, w_rgate, b_rgate, w_igate, b_igate, lru_lambda, w_out):
    B, S, _ = h.shape
    f32 = jnp.float32
    proj = h @ w_in
    rwkv_p, lru_x, lru_gate = split_cols(proj, (RWKV_PROJ, LRU_WIDTH, LRU_WIDTH))

    r, k, v, wd, ad, gd = split_cols(token_shift(rwkv_p, mu_shift), RWKV_COLS)
    heads = lambda t: t.reshape(B, S, RWKV_HEADS, RWKV_HEAD_DIM)
    w_log = -jax.nn.softplus(-(w_decay0 + jnp.tanh(wd) @ w_decay_up).astype(f32)) - 0.5
    decay = jnp.exp(-jnp.exp(w_log))
    a = jax.nn.sigmoid((a_bias + ad @ w_a_up).astype(f32))
    g = jax.nn.sigmoid(gd) @ w_g_up
    rf, kf, vf = r.astype(f32), k.astype(f32), v.astype(f32)
    kk = heads(kf * k_k)
    kk = kk * lax.rsqrt(jnp.maximum(jnp.sum(kk * kk, axis=-1, keepdims=True), 1e-24))
    k_mod = heads(kf * (1.0 + (a - 1.0) * k_a))
    y = rwkv7_scan(heads(rf), heads(decay), k_mod, heads(vf), kk, heads(a))
    y_mean = jnp.mean(y, axis=-1, keepdims=True)
    y_var = jnp.mean(jnp.square(y - y_mean), axis=-1, keepdims=True)
    y = ((y - y_mean) * lax.rsqrt(y_var + GN_EPS)).reshape(B, S, RWKV_WIDTH) * lnx_w + lnx_b
    bonus = jnp.sum(heads(rf) * k_mod * r_k, axis=-1, keepdims=True) * heads(vf)
    rwkv_out = (y + bonus.reshape(B, S, RWKV_WIDTH)) * g

    xpad = jnp.pad(lru_x, ((0, 0), (CONV_WIDTH - 1, 0), (0, 0)))
    xc = conv_b + xpad[:, 0:S] * conv_w[0]
    for j in range(1, CONV_WIDTH):
        xc = xc + xpad[:, j:j + S] * conv_w[j]
    xh = xc.reshape(B, S, LRU_HEADS, LRU_BLOCK)
    rg = jax.nn.sigmoid((jnp.einsum('bshi,hij->bshj', xh, w_rgate) + b_rgate).astype(f32))
    ig = jax.nn.sigmoid((jnp.einsum('bshi,hij->bshj', xh, w_igate) + b_igate).astype(f32))
    log_a = -LRU_C * rg * jax.nn.softplus(-lru_lambda.astype(f32)).reshape(LRU_HEADS, LRU_BLOCK)
    a_t = jnp.exp(log_a)
    x_in = xh.astype(f32) * ig * jnp.sqrt(-jnp.expm1(2.0 * log_a))
    _, hs = lax.associative_scan(linear_scan_combine, (a_t, x_in), axis=1)
    lru_out = hs.reshape(B, S, LRU_WIDTH) * jax.nn.gelu(lru_gate.astype(f32))

    mixed = jnp.concatenate([rwkv_out, lru_out], axis=-1).astype(h.dtype)
    return mixed @ w_out


def dsa_mixer(h, w_in, w_out, cos, sin):
    B, S, _ = h.shape
    topk = min(TOPK_MAX, S // 4)
    n_blocks = S // Q_BLOCK
    q, k, v, iq, ik, iw = split_cols(h @ w_in, IN1_COLS)
    q = apply_rope(q.reshape(B, S, ATT_HEADS, ATT_HEAD_DIM), cos, sin)
    k = apply_rope(k.reshape(B, S, 1, ATT_HEAD_DIM), cos, sin)[:, :, 0]
    iq = apply_rope(iq.reshape(B, S, IDX_HEADS, IDX_HEAD_DIM), cos, sin)
    ik = apply_rope(ik.reshape(B, S, 1, IDX_HEAD_DIM), cos, sin)[:, :, 0].astype(jnp.float32)
    iw = iw.astype(jnp.float32) * IDX_HEADS ** -0.5
    key_chunk = jnp.arange(S) // CHUNK

    def to_blocks(t):
        return jnp.swapaxes(t.reshape((B, n_blocks, Q_BLOCK) + t.shape[2:]), 0, 1)

    def block(args):
        qb, iqb, iwb, bi = args
        q_chunk = (bi * Q_BLOCK + jnp.arange(Q_BLOCK)) // CHUNK
        admissible = key_chunk[None, :] <= q_chunk[:, None]
        idx_logits = jnp.einsum('bqhd,bsd->bqhs', iqb.astype(jnp.float32), ik) * IDX_HEAD_DIM ** -0.5
        score = jnp.einsum('bqh,bqhs->bqs', iwb, jax.nn.relu(idx_logits))
        score = jnp.where(admissible[None], score, -jnp.inf)
        _, sel = lax.top_k(score, topk)
        valid = (sel // CHUNK) <= q_chunk[None, :, None]
        k_sel = jax.vmap(lambda kb, ib: kb[ib])(k, sel)
        v_sel = jax.vmap(lambda vb, ib: vb[ib])(v, sel)
        logits = jnp.einsum('bqhd,bqkd->bqhk', qb, k_sel).astype(jnp.float32) * ATT_HEAD_DIM ** -0.5
        logits = jnp.where(valid[:, :, None, :], logits, -jnp.inf)
        probs = jax.nn.softmax(logits, axis=-1).astype(v_sel.dtype)
        return jnp.einsum('bqhk,bqkd->bqhd', probs, v_sel)

    out = lax.map(block, (to_blocks(q), to_blocks(iq), to_blocks(iw), jnp.arange(n_blocks)))
    out = jnp.swapaxes(out, 0, 1).reshape(B, S, ATT_WIDTH)
    return out @ w_out


def setup_inputs(seed: int = 0) -> dict:
    key = jax.random.key(seed)
    ks = iter(jax.random.split(key, 32))
    nrm = lambda shape, scale: scale * jax.random.normal(next(ks), shape, jnp.float32)
    uni = lambda shape, lo, hi: jax.random.uniform(next(ks), shape, jnp.float32, lo, hi)
    D, E, O = D_MODEL, N_EVEN, N_ODD
    a_pow_c = uni((E, LRU_WIDTH), 0.9, 0.999)
    a_base = a_pow_c ** (1.0 / LRU_C)
    lru_lambda = jnp.log(a_base) - jnp.log1p(-a_base)
    return {
        'x': nrm((BATCH, SEQ, D), 1.0),
        'norm_mix': 1.0 + nrm((DEPTH, D), 0.02),
        'w_in0': nrm((E, D, IN0_WIDTH), D ** -0.5),
        'mu_shift': uni((E, RWKV_PROJ), 0.0, 1.0),
        'w_decay0': uni((E, RWKV_WIDTH), -6.0, -1.0),
        'w_decay_up': nrm((E, DECAY_LORA, RWKV_WIDTH), 0.1 * DECAY_LORA ** -0.5),
        'a_bias': nrm((E, RWKV_WIDTH), 0.5),
        'w_a_up': nrm((E, A_LORA, RWKV_WIDTH), 0.5 * A_LORA ** -0.5),
        'w_g_up': nrm((E, GATE_LORA, RWKV_WIDTH), GATE_LORA ** -0.5),
        'k_k': 0.85 + nrm((E, RWKV_WIDTH), 0.02),
        'k_a': 1.0 + nrm((E, RWKV_WIDTH), 0.02),
        'r_k': nrm((E, RWKV_HEADS, RWKV_HEAD_DIM), 0.1),
        'lnx_w': 1.0 + nrm((E, RWKV_WIDTH), 0.02),
        'lnx_b': nrm((E, RWKV_WIDTH), 0.02),
        'conv_w': nrm((E, CONV_WIDTH, LRU_WIDTH), CONV_WIDTH ** -0.5),
        'conv_b': nrm((E, LRU_WIDTH), 0.02),
        'w_rgate': nrm((E, LRU_HEADS, LRU_BLOCK, LRU_BLOCK), LRU_BLOCK ** -0.5),
        'b_rgate': nrm((E, LRU_HEADS, LRU_BLOCK), 0.02),
        'w_igate': nrm((E, LRU_HEADS, LRU_BLOCK, LRU_BLOCK), LRU_BLOCK ** -0.5),
        'b_igate': nrm((E, LRU_HEADS, LRU_BLOCK), 0.02),
        'lru_lambda': lru_lambda,
        'w_out0': nrm((E, MIX_WIDTH, D), MIX_WIDTH ** -0.5),
        'w_in1': nrm((O, D, IN1_WIDTH), D ** -0.5),
        'w_out1': nrm((O, ATT_WIDTH, D), ATT_WIDTH ** -0.5),
        'norm_ffn': 1.0 + nrm((DEPTH, D), 0.02),
        'ffn_gate': nrm((DEPTH, D, FFN_HIDDEN), D ** -0.5),
        'ffn_up': nrm((DEPTH, D, FFN_HIDDEN), D ** -0.5),
        'ffn_down': nrm((DEPTH, FFN_HIDDEN, D), FFN_HIDDEN ** -0.5),
        'norm_final': 1.0 + nrm((D,), 0.02),
    }


def reference(x, norm_mix, w_in0, mu_shift, w_decay0, w_decay_up, a_bias, w_a_up, w_g_up, k_k, k_a, r_k,
              lnx_w, lnx_b, conv_w, conv_b, w_rgate, b_rgate, w_igate, b_igate, lru_lambda, w_out0,
              w_in1, w_out1, norm_ffn, ffn_gate, ffn_up, ffn_down, norm_final):
    cos, sin = rope_tables(x.shape[1], ATT_HEAD_DIM)
    for layer in range(DEPTH):
        h = rmsnorm(x, norm_mix[layer])
        if layer % 2 == 0:
            e = layer // 2
            x = x + rwkv_lru_mixer(h, w_in0[e], mu_shift[e], w_decay0[e], w_decay_up[e], a_bias[e],
                                   w_a_up[e], w_g_up[e], k_k[e], k_a[e], r_k[e], lnx_w[e], lnx_b[e],
                                   conv_w[e], conv_b[e], w_rgate[e], b_rgate[e], w_igate[e], b_igate[e],
                                   lru_lambda[e], w_out0[e])
        else:
            o = layer // 2
            x = x + dsa_mixer(h, w_in1[o], w_out1[o], cos, sin)
        h = rmsnorm(x, norm_ffn[layer])
        x = x + swiglu(h, ffn_gate[layer], ffn_up[layer], ffn_down[layer])
    return rmsnorm(x, norm_final)
```

```python
import numpy as np
from contextlib import ExitStack
import concourse.bass as bass
import concourse.mybir as mybir
from concourse.bass_utils import run_bass_kernel_spmd

F32 = mybir.dt.float32
BF16 = mybir.dt.bfloat16
AF = mybir.ActivationFunctionType
ALU = mybir.AluOpType
AX = mybir.AxisListType


class Res:
    __slots__ = ("name", "w", "readers", "bank")

    def __init__(self, name="", bank=None):
        self.name = name
        self.w = None
        self.readers = {}
        self.bank = bank


class _Rec:
    def __init__(self):
        self.call = None

    def __getattr__(self, name):
        def f(*a, **k):
            self.call = (name, a, k)
            return self
        return f


def _eager(fn):
    rec = _Rec()
    fn(rec)
    name, a, k = rec.call
    return lambda e: getattr(e, name)(*a, **k)


class Sched:
    COMPUTE = ("pe", "act", "dve", "pool")
    NDSEM = 6

    def __init__(self, nc, stack, dma_queues=("sp", "pool")):
        self.nc = nc
        self.sems = {}
        self.ecnt = {}
        self.prog = {e: [] for e in ("pe", "act", "dve", "pool", "sp")}
        self.known = {e: {} for e in self.prog}
        for e in self.COMPUTE:
            self.sems["e_" + e] = stack.enter_context(nc.semaphore("e_" + e))
            self.ecnt[e] = 0
        self.dq = {}
        for q in dma_queues:
            keys = []
            for i in range(self.NDSEM):
                k = f"d_{q}{i}"
                self.sems[k] = stack.enter_context(nc.semaphore(k))
                keys.append(k)
            self.dq[q] = {"keys": keys, "cnt": [0] * self.NDSEM, "next": 0}
        self.n_ops = 0

    def res(self, name="", bank=None):
        return Res(name, bank)

    def _deps(self, eng, is_dma, reads, writes):
        deps = {}

        def add(t, raw):
            key, val, peng, pdma = t
            if not pdma and peng == eng and not is_dma:
                if not raw or eng == "pe":
                    return
            if deps.get(key, 0) < val:
                deps[key] = val

        for r in reads:
            if r.w is not None:
                add(r.w, True)
        for r in list(reads) + list(writes):
            if r.bank is not None:
                for key, (val, peng, pdma) in r.bank.readers.items():
                    if peng != eng:
                        add((key, val, peng, pdma), False)
        for w in writes:
            if w.w is not None:
                add(w.w, False)
            for key, (val, peng, pdma) in w.readers.items():
                add((key, val, peng, pdma), False)
        kn = self.known[eng]
        out = []
        for key, val in deps.items():
            if kn.get(key, 0) < val:
                kn[key] = val
                out.append((key, val))
        return out

    def _commit(self, tick, reads, writes):
        key, val, eng, is_dma = tick
        for w in writes:
            w.w = tick
            w.readers = {}
        for r in list(reads) + list(writes):
            if r.bank is not None:
                r.bank.readers[key] = (val, eng, is_dma)
        for r in reads:
            if r in writes:
                continue
            r.readers[key] = (val, eng, is_dma)

    def op(self, eng, fn, reads=(), writes=()):
        reads = [r for r in reads if r is not None]
        writes = [w for w in writes if w is not None]
        waits = self._deps(eng, False, reads, writes)
        self.ecnt[eng] += 1
        tick = ("e_" + eng, self.ecnt[eng], eng, False)
        self.prog[eng].append((waits, _eager(fn), ("e_" + eng, 1)))
        self._commit(tick, reads, writes)
        self.n_ops += 1

    def dma(self, q, fn, reads=(), writes=()):
        reads = [r for r in reads if r is not None]
        writes = [w for w in writes if w is not None]
        waits = self._deps(q, True, reads, writes)
        d = self.dq[q]
        slot = d["next"] % self.NDSEM
        d["next"] += 1
        key = d["keys"][slot]
        if d["cnt"][slot] > 0 and self.known[q].get(key, 0) < d["cnt"][slot]:
            self.known[q][key] = d["cnt"][slot]
            waits.append((key, d["cnt"][slot]))
        d["cnt"][slot] += 16
        tick = (key, d["cnt"][slot], q, True)
        self.prog[q].append((waits, _eager(fn), (key, 16)))
        self._commit(tick, reads, writes)
        self.n_ops += 1

    def barrier(self):
        allv = {}
        for e in self.COMPUTE:
            if self.ecnt[e] > 0:
                allv["e_" + e] = self.ecnt[e]
        for q, d in self.dq.items():
            for k, c in zip(d["keys"], d["cnt"]):
                if c > 0:
                    allv[k] = c
        for e in self.prog:
            waits = []
            for k, v in allv.items():
                if self.known[e].get(k, 0) < v:
                    self.known[e][k] = v
                    waits.append((k, v))
            if waits:
                self.prog[e].append((waits, None, None))

    def flush(self, final=False):
        nc = self.nc
        sems = self.sems
        fin = {}
        if final:
            for q, d in self.dq.items():
                for k, c in zip(d["keys"], d["cnt"]):
                    if c > 0:
                        fin[k] = c

        def run(eng_obj, items, extra=None):
            for waits, fn, inc in items:
                for key, val in waits:
                    eng_obj.wait_ge(sems[key], val)
                if fn is not None:
                    ins = fn(eng_obj)
                    ins.then_inc(sems[inc[0]], inc[1])
            if extra:
                for key, val in extra.items():
                    eng_obj.wait_ge(sems[key], val)

        prog = self.prog
        with nc.Block() as block:
            @block.tensor
            def _(e):
                run(e, prog["pe"])

            @block.scalar
            def _(e):
                run(e, prog["act"])

            @block.vector
            def _(e):
                run(e, prog["dve"])

            @block.gpsimd
            def _(e):
                run(e, prog["pool"])

            @block.sync
            def _(e):
                run(e, prog["sp"], extra=fin)
        self.prog = {e: [] for e in prog}


D = 1024
SEQ = 2048
NSEQ = 2
NTOK = NSEQ * SEQ
FF = 2816
EPS = 1e-6


class Ctx:
    _phase = [0]

    def __init__(self, nc, S, st):
        self.nc, self.S, self.st = nc, S, st
        self.n = 0
        Ctx._phase[0] += 1
        self.pfx = f"ph{Ctx._phase[0]}_"

    def sb(self, shape, dt, name=None):
        self.n += 1
        t = self.st.enter_context(self.nc.sbuf_tensor(self.pfx + (name or f"t{self.n}"), list(shape), dt))
        return t, self.S.res(name or f"t{self.n}")

    def ps(self, shape, dt, name=None):
        self.n += 1
        t = self.st.enter_context(self.nc.psum_tensor(self.pfx + (name or f"p{self.n}"), list(shape), dt))
        r = self.S.res(name or f"p{self.n}")
        r.bank = Res("bank")
        return t, r


def load_weight(S, q, dst, src, nk, res_list):
    for k in range(nk):
        S.dma(q, lambda e, k=k: e.dma_start(out=dst[:, k, :], in_=src[k * 128:(k + 1) * 128, :]),
              writes=[res_list[k]])


def rstd_ops(S, ss, Rss, n=D, eps=EPS):
    S.op("dve", lambda e: e.tensor_scalar(out=ss[:], in0=ss[:], scalar1=1.0 / n, scalar2=eps,
                                          op0=ALU.mult, op1=ALU.add), reads=[Rss], writes=[Rss])
    S.op("act", lambda e: e.activation(out=ss[:], in_=ss[:], func=AF.Sqrt), reads=[Rss], writes=[Rss])
    S.op("dve", lambda e: e.reciprocal(out=ss[:], in_=ss[:]), reads=[Rss], writes=[Rss])


def norm_T(S, xt, Rxt, gain, Rg, hT, RhT, tmp):
    junk, Rjunk, ss, Rss, xn, Rxn, pT, RpT, ident, Rid = tmp
    S.op("act", lambda e: e.activation(out=junk[:], in_=xt, func=AF.Square, accum_out=ss[:]),
         reads=[Rxt], writes=[Rjunk, Rss])
    rstd_ops(S, ss, Rss)
    S.op("act", lambda e: e.activation(out=xn[:], in_=xt, func=AF.Copy, scale=ss[:, 0:1]),
         reads=[Rxt, Rss], writes=[Rxn])
    for k in range(8):
        S.op("pe", lambda e, k=k: e.transpose(pT[:, k, :], xn[:, k * 128:(k + 1) * 128], ident[:]),
             reads=[Rxn, Rid], writes=[RpT])
    S.op("dve", lambda e: e.tensor_tensor(out=hT, in0=pT[:], in1=gain.unsqueeze(2).to_broadcast([128, 8, 128]),
                                          op=ALU.mult), reads=[RpT, Rg], writes=[RhT])


def make_ident(S, C, dt=BF16):
    idf, Ridf = C.sb([128, 128], F32)
    S.op("pool", lambda e: e.memset(idf[:], 0.0), writes=[Ridf])
    S.op("pool", lambda e: e.affine_select(out=idf[:], in_=idf[:], pattern=[[-1, 128]], compare_op=ALU.not_equal,
                                           fill=1.0, base=0, channel_multiplier=1), reads=[Ridf], writes=[Ridf])
    idb, Ridb = C.sb([128, 128], BF16)
    S.op("dve", lambda e: e.tensor_copy(out=idb[:], in_=idf[:]), reads=[Ridf], writes=[Ridb])
    return idf, Ridf, idb, Ridb


def phase_ffn(nc, S, src, dst, Dm, l, final):
    with ExitStack() as st:
        C = Ctx(nc, S, st)
        S.barrier()
        wg, _ = C.sb([128, 8, FF], BF16, "wg")
        wu, _ = C.sb([128, 8, FF], BF16, "wu")
        wd, _ = C.sb([128, 22, D], BF16, "wd")
        Rwg = [S.res() for _ in range(8)]
        Rwu = [S.res() for _ in range(8)]
        Rwd = [S.res() for _ in range(22)]
        gain, Rg = C.sb([128, 8], F32, "gain")
        S.dma("sp", lambda e: e.dma_start(out=gain[:], in_=Dm[f"g_ffn{l}"]), writes=[Rg])
        load_weight(S, "pool", wg, Dm[f"ffn_gate{l}"], 8, Rwg)
        load_weight(S, "pool", wu, Dm[f"ffn_up{l}"], 8, Rwu)
        load_weight(S, "pool", wd, Dm[f"ffn_down{l}"], 22, Rwd)
        if final:
            gfin, Rgfin = C.sb([128, D], F32, "gfin")
            S.dma("sp", lambda e: e.dma_start(out=gfin[:], in_=Dm["g_final"][0, :].partition_broadcast(128)),
                  writes=[Rgfin])
        idf, Ridf, idb, Ridb = make_ident(S, C)
        TT = 256
        xts = [C.sb([128, 2, D], F32) for _ in range(2)]
        hTs = [C.sb([128, 8, TT], BF16) for _ in range(2)]
        actT, RactT = C.sb([128, 22, TT], BF16, "actT")
        junk, Rjunk = C.sb([128, D], BF16, "junk")
        ss, Rss = C.sb([128, 1], F32, "ss")
        xn, Rxn = C.sb([128, D], BF16, "xn")
        sgs = [C.sb([128, TT], F32) for _ in range(2)]
        pTs = [C.ps([128, 8, 128], BF16) for _ in range(2)]
        pgu = [C.ps([128, 2, TT], F32) for _ in range(2)]
        pso = [C.ps([128, 512], F32) for _ in range(2)]
        npt = 0
        for it in range(NTOK // TT):
            tok0 = it * TT
            xt, Rxt = xts[it % 2]
            hT, RhT = hTs[it % 2]
            S.dma("sp", lambda e, xt=xt, tok0=tok0: e.dma_start(
                out=xt[:], in_=src[tok0:tok0 + TT, :].rearrange("(s p) d -> p s d", p=128)), writes=[Rxt])
            for s in range(2):
                pT, RpT = pTs[npt % 2]
                npt += 1
                norm_T(S, xt[:, s, :], Rxt, gain[:], Rg, hT[:, :, s * 128:(s + 1) * 128], RhT,
                       (junk, Rjunk, ss, Rss, xn, Rxn, pT, RpT, idb, Ridb))
            for j in range(22):
                pg, Rpg = pgu[j % 2]
                sg, Rsg = sgs[j % 2]
                for k in range(8):
                    S.op("pe", lambda e, k=k, j=j, pg=pg, hT=hT: e.matmul(
                        pg[:, 0, :], lhsT=wg[:, k, j * 128:(j + 1) * 128], rhs=hT[:, k, :], start=(k == 0), stop=(k == 7)),
                        reads=[Rwg[k], RhT], writes=[Rpg])
                for k in range(8):
                    S.op("pe", lambda e, k=k, j=j, pg=pg, hT=hT: e.matmul(
                        pg[:, 1, :], lhsT=wu[:, k, j * 128:(j + 1) * 128], rhs=hT[:, k, :], start=(k == 0), stop=(k == 7)),
                        reads=[Rwu[k], RhT], writes=[Rpg])
                S.op("act", lambda e, pg=pg, sg=sg: e.activation(out=sg[:], in_=pg[:, 0, :], func=AF.Silu),
                     reads=[Rpg], writes=[Rsg])
                S.op("dve", lambda e, pg=pg, sg=sg, j=j: e.tensor_tensor(out=actT[:, j, :], in0=pg[:, 1, :], in1=sg[:],
                                                                        op=ALU.mult),
                     reads=[Rpg, Rsg], writes=[RactT])
            n = 0
            for s in range(2):
                for dh in range(2):
                    po, Rpo = pso[n % 2]
                    n += 1
                    for j in range(22):
                        S.op("pe", lambda e, j=j, s=s, dh=dh, po=po: e.matmul(
                            po[:], lhsT=actT[:, j, s * 128:(s + 1) * 128], rhs=wd[:, j, dh * 512:(dh + 1) * 512],
                            start=(j == 0), stop=(j == 21)), reads=[RactT, Rwd[j]], writes=[Rpo])
                    S.op("dve", lambda e, s=s, dh=dh, po=po, xt=xt: e.tensor_tensor(
                        out=xt[:, s, dh * 512:(dh + 1) * 512], in0=po[:], in1=xt[:, s, dh * 512:(dh + 1) * 512],
                        op=ALU.add), reads=[Rpo, Rxt], writes=[Rxt])
            if final:
                for s in range(2):
                    S.op("act", lambda e, s=s, xt=xt: e.activation(out=junk[:], in_=xt[:, s, :], func=AF.Square,
                                                                   accum_out=ss[:]), reads=[Rxt], writes=[Rjunk, Rss])
                    rstd_ops(S, ss, Rss)
                    S.op("act", lambda e, s=s, xt=xt: e.activation(out=xt[:, s, :], in_=xt[:, s, :], func=AF.Copy,
                                                                   scale=ss[:, 0:1]), reads=[Rxt, Rss], writes=[Rxt])
                    S.op("dve", lambda e, s=s, xt=xt: e.tensor_tensor(out=xt[:, s, :], in0=xt[:, s, :], in1=gfin[:],
                                                                      op=ALU.mult), reads=[Rxt, Rgfin], writes=[Rxt])
            S.dma("pool", lambda e, xt=xt, tok0=tok0: e.dma_start(
                out=dst[tok0:tok0 + TT, :].rearrange("(s p) d -> p s d", p=128), in_=xt[:]), reads=[Rxt], writes=[])
        S.flush()


def fm(v):
    v = np.asarray(v, np.float32).reshape(-1, 128)
    return np.ascontiguousarray(v.T)


def build(phases):
    nc = bass.Bass("TRN2", target_bir_lowering=False)
    Dm = {}

    def inp(name, shape):
        Dm[name] = nc.dram_tensor(name, list(shape), F32, kind="ExternalInput").ap()

    inp("x", [NTOK, D])
    for l in range(2):
        if f"F{l}" in phases:
            inp(f"ffn_gate{l}", [D, FF]); inp(f"ffn_up{l}", [D, FF]); inp(f"ffn_down{l}", [FF, D])
            inp(f"g_ffn{l}", [128, 8])
    if "F1" in phases:
        inp("g_final", [1, D])
    if "L0" in phases:
        inp("w_in0", [D, 2816]); inp("w_out0", [D, D]); inp("p0", [128, NP0]); inp("lora0", [128, 512])
        inp("w_g_up0", [128, 512]); inp("gates_bd0", [128, 8, 128]); inp("lnx0", [2, 512]); inp("c_masks0", [128, 770])
    if "L1" in phases:
        inp("w1", [D, W1C]); inp("wo1", [64, 16, D]); inp("g_mix1", [128, 8])
        inp("c_cos", [128, SEQ]); inp("c_sin", [128, SEQ]); inp("c_pm", [128, 128])
        inp("c_negmask", [128, 128]); inp("c_pow2", [128, NIT])
    out = nc.dram_tensor("out", [NTOK, D], F32, kind="ExternalOutput").ap()
    scr = [nc.dram_tensor(f"scr{i}", [NTOK, D], F32).ap() for i in range(2)]
    with ExitStack() as st:
        S = Sched(nc, st)
        cur = Dm["x"]
        for i, ph in enumerate(phases):
            dst = out if i == len(phases) - 1 else scr[i % 2]
            if ph in ("F0", "F1"):
                phase_ffn(nc, S, cur, dst, Dm, int(ph[1]), ph == "F1")
            elif ph == "L1":
                phase_dsa(nc, S, cur, dst, Dm)
            elif ph == "L0":
                phase_l0(nc, S, cur, dst, Dm)
            cur = dst
        S.barrier()
        S.prog["sp"].append(([], None, None))
        S.flush(final=True)
    return nc


def host_inputs(inputs, phases):
    x = np.asarray(inputs["x"], np.float32)
    shared = {}
    for l in range(2):
        if f"F{l}" in phases:
            shared[f"ffn_gate{l}"] = np.ascontiguousarray(inputs["ffn_gate"][l], dtype=np.float32)
            shared[f"ffn_up{l}"] = np.ascontiguousarray(inputs["ffn_up"][l], dtype=np.float32)
            shared[f"ffn_down{l}"] = np.ascontiguousarray(inputs["ffn_down"][l], dtype=np.float32)
            shared[f"g_ffn{l}"] = fm(inputs["norm_ffn"][l])
    if "F1" in phases:
        shared["g_final"] = np.asarray(inputs["norm_final"], np.float32).reshape(1, D)
    if "L0" in phases:
        shared.update(l0_host(inputs))
    if "L1" in phases:
        w = np.asarray(inputs["w_in1"][0], np.float32)
        q, k, v, iq, ik, iw = np.split(w, np.cumsum([1024, 64, 64, 512, 64])[:].tolist(), axis=1)
        shared["w1"] = np.ascontiguousarray(np.concatenate([q, iq, k, k, ik, ik, v, iw], axis=1))
        shared["wo1"] = np.ascontiguousarray(np.asarray(inputs["w_out1"][0], np.float32).reshape(16, 64, D).transpose(1, 0, 2))
        shared["g_mix1"] = fm(inputs["norm_mix"][1])
        shared.update(dsa_consts())
    maps = []
    for c in range(8):
        m = dict(shared)
        m["x"] = np.ascontiguousarray(x[c * NSEQ:(c + 1) * NSEQ].reshape(NTOK, D))
        maps.append(m)
    return maps


PHASES = ("L0", "F0", "L1", "F1")
DBG = {"nseq": NSEQ, "qbs": None, "d1_tiles": None, "l0_tiles": None, "cores": 8}


def kernel(phases=PHASES, **inputs):
    nc = build(phases)
    maps = host_inputs(inputs, phases)
    ncores = DBG["cores"]
    res = run_bass_kernel_spmd(nc, maps[:ncores], core_ids=list(range(ncores)))
    outs = [np.asarray(r["out"], np.float32).reshape(NSEQ, SEQ, D) for r in res.results]
    outs += [np.zeros((NSEQ, SEQ, D), np.float32)] * (8 - ncores)
    return np.concatenate(outs, axis=0)


W1C = 1864
NIT = 24
TOPK = 256


def phase_dsa(nc, S, src, dst, Dm):
    with ExitStack() as st:
        C = Ctx(nc, S, st)
        S.barrier()
        w1, _ = C.sb([128, 8, W1C], BF16, "w1")
        Rw1 = [S.res() for _ in range(8)]
        load_weight(S, "pool", w1, Dm["w1"], 8, Rw1)
        wo, Rwo = C.sb([64, 16, D], BF16, "wo")
        for hh_ in range(16):
            S.dma("pool", lambda e, hh_=hh_: e.dma_start(out=wo[:, hh_, :], in_=Dm["wo1"][:, hh_, :]), writes=[Rwo])
        gain, Rg = C.sb([128, 8], F32, "gain")
        S.dma("sp", lambda e: e.dma_start(out=gain[:], in_=Dm["g_mix1"]), writes=[Rg])
        pm_, Rpm = C.sb([128, 128], BF16, "Pm")
        S.dma("pool", lambda e: e.dma_start(out=pm_[:], in_=Dm["c_pm"]), writes=[Rpm])
        negm, Rnegm = C.sb([128, 128], F32, "negm")
        S.dma("sp", lambda e: e.dma_start(out=negm[:], in_=Dm["c_negmask"]), writes=[Rnegm])
        pow2, Rpow2 = C.sb([128, NIT], F32, "pow2")
        S.dma("sp", lambda e: e.dma_start(out=pow2[:], in_=Dm["c_pow2"]), writes=[Rpow2])
        idf, Ridf, idb, Ridb = make_ident(S, C)
        onesf, Rones = C.sb([128, 64], F32, "onesf")
        S.op("pool", lambda e: e.memset(onesf[:], 1.0), writes=[Rones])
        thrc, Rthrc = C.sb([128, 1], F32, "thrc")
        S.op("pool", lambda e: e.memset(thrc[:], -1e29), writes=[Rthrc])

        qT, RqT = C.sb([128, 8, SEQ], BF16, "qT")
        iqT, RiqT = C.sb([128, 4, SEQ], BF16, "iqT")
        kT2, RkT2 = C.sb([128, SEQ], BF16, "kT2")
        ikT2, RikT2 = C.sb([128, SEQ], BF16, "ikT2")
        Vaug, RV = C.sb([128, 16, 65], BF16, "Vaug")
        iw, Riw = C.sb([128, 16, 8], F32, "iw")
        S.op("pool", lambda e: e.memset(Vaug[:], 1.0), writes=[RV])

        T1 = 256
        xts = [C.sb([128, 2, D], F32) for _ in range(2)]
        hT, RhT = C.sb([128, 8, T1], BF16, "hT")
        junk, Rjunk = C.sb([128, D], BF16, "junk")
        ss, Rss = C.sb([128, 1], F32, "ss")
        xn, Rxn = C.sb([128, D], BF16, "xn")
        pbs = [C.sb([128, T1], BF16) for _ in range(2)]
        t1s = [C.sb([128, T1], F32) for _ in range(2)]
        t2s = [C.sb([128, T1], F32) for _ in range(2)]
        css = [C.sb([128, 2, T1], F32) for _ in range(2)]
        score, Rscore = C.sb([128, SEQ], F32, "score")
        jcnt, Rjcnt = C.sb([128, SEQ], BF16, "jcnt")
        maskT, RmaskT = C.sb([128, 16, 128], BF16, "maskT")
        rls = [C.sb([128, 512], F32) for _ in range(2)]
        Es = [C.sb([128, 4, 128], BF16) for _ in range(2)]
        PTs = [C.sb([128, 4, 128], BF16) for _ in range(2)]
        rs, Rrs = C.sb([128, 512], F32, "rs")
        bc, Rbc = C.sb([64, 512], F32, "bc")
        attnT = [C.sb([64, 4, 128], BF16) for _ in range(4)]
        xres, Rxres = C.sb([128, D], F32, "xres")
        bis, Rbis = C.sb([128, 8], F32, "bis")
        steps, Rsteps = C.sb([128, NIT], F32, "steps")
        cnts, Rcnts = C.sb([128, NIT], F32, "cnts")
        pT, RpT = C.ps([128, 8, 128], BF16, "pT")
        B = [C.ps([128, 4, 128], F32) for _ in range(7)]

        for sq in range(DBG["nseq"]):
            base = sq * SEQ
            for tt in range(DBG["d1_tiles"] or SEQ // T1):
                t0 = tt * T1
                xt, Rxt = xts[tt % 2]
                cs, Rcs = css[tt % 2]
                S.dma("sp", lambda e, xt=xt, a=base + t0: e.dma_start(
                    out=xt[:], in_=src[a:a + T1, :].rearrange("(s p) d -> p s d", p=128)), writes=[Rxt])
                S.dma("sp", lambda e, cs=cs, t0=t0: e.dma_start(out=cs[:, 0, :], in_=Dm["c_cos"][:, t0:t0 + T1]),
                      writes=[Rcs])
                S.dma("sp", lambda e, cs=cs, t0=t0: e.dma_start(out=cs[:, 1, :], in_=Dm["c_sin"][:, t0:t0 + T1]),
                      writes=[Rcs])
                for s in range(T1 // 128):
                    norm_T(S, xt[:, s, :], Rxt, gain[:], Rg, hT[:, :, s * 128:(s + 1) * 128], RhT,
                           (junk, Rjunk, ss, Rss, xn, Rxn, pT, RpT, idb, Ridb))
                for nt in range(DBG.get("d1_nt", 14)):
                    pp, Rpp = B[nt % 2]
                    pr, Rpr = B[2 + nt % 2]
                    pb, Rpb = pbs[nt % 2]
                    ta, Rta = t1s[nt % 2]
                    tb, Rtb = t2s[nt % 2]
                    ppv = pp[:].rearrange("p a b -> p (a b)")[:, 0:T1]
                    prv = pr[:].rearrange("p a b -> p (a b)")[:, 0:T1]
                    for k in range(8):
                        S.op("pe", lambda e, k=k, nt=nt, ppv=ppv: e.matmul(
                            ppv, lhsT=w1[:, k, nt * 128:(nt + 1) * 128], rhs=hT[:, k, :], start=(k == 0), stop=(k == 7)),
                            reads=[Rw1[k], RhT], writes=[Rpp])
                    lvl = DBG.get("d1_ops", 9)
                    if lvl < 2:
                        continue
                    S.op("act", lambda e, pb=pb, ppv=ppv: e.copy(out=pb[:], in_=ppv), reads=[Rpp], writes=[Rpb])
                    if lvl < 3:
                        continue
                    S.op("pe", lambda e, pb=pb, prv=prv: e.matmul(prv, lhsT=pm_[:], rhs=pb[:], start=True, stop=True),
                         reads=[Rpm, Rpb], writes=[Rpr])
                    if lvl < 4:
                        continue
                    if DBG.get("tt", 3) & 1:
                        S.op("dve", lambda e, ta=ta, ppv=ppv, cs=cs: e.tensor_tensor(out=ta[:], in0=ppv, in1=cs[:, 0, :],
                                                                                    op=ALU.mult),
                             reads=[Rpp, Rcs, Rpb], writes=[Rta])
                    if DBG.get("tt", 3) & 2:
                        S.op("dve", lambda e, tb=tb, prv=prv, cs=cs: e.tensor_tensor(out=tb[:], in0=prv, in1=cs[:, 1, :],
                                                                                    op=ALU.mult),
                             reads=[Rpr, Rcs], writes=[Rtb])
                    if DBG.get("tt", 3) & 4:
                        S.op("dve", lambda e, ta=ta, ppv=ppv, cs=cs: e.tensor_tensor(out=ta[:], in0=ppv, in1=ta[:],
                                                                                    op=ALU.mult),
                             reads=[Rpp, Rcs], writes=[Rta])
                    if lvl < 5:
                        continue
                    if nt < 8:
                        dest, Rd = qT[:, nt, t0:t0 + T1], RqT
                    elif nt < 12:
                        dest, Rd = iqT[:, nt - 8, t0:t0 + T1], RiqT
                    elif nt == 12:
                        dest, Rd = kT2[:, t0:t0 + T1], RkT2
                    else:
                        dest, Rd = ikT2[:, t0:t0 + T1], RikT2
                    S.op(DBG.get("addeng", "pool"), lambda e, dest=dest, ta=ta, tb=tb: e.tensor_tensor(out=dest, in0=ta[:], in1=tb[:],
                                                                                   op=ALU.add),
                         reads=[Rta, Rtb], writes=[Rd])
                for s in range(DBG.get("d1_v", T1 // 128)):
                    blk = t0 // 128 + s
                    pv, Rpv = B[4]
                    pvv = pv[:].rearrange("p a b -> p (a b)")[:, 0:72]
                    for k in range(8):
                        S.op("pe", lambda e, k=k, s=s, pvv=pvv: e.matmul(
                            pvv, lhsT=hT[:, k, s * 128:(s + 1) * 128], rhs=w1[:, k, 1792:1864], start=(k == 0),
                            stop=(k == 7)), reads=[Rw1[k], RhT], writes=[Rpv])
                    S.op("act", lambda e, blk=blk, pvv=pvv: e.copy(out=Vaug[:, blk, 0:64], in_=pvv[:, 0:64]),
                         reads=[Rpv], writes=[RV])
                    S.op("dve", lambda e, blk=blk, pvv=pvv: e.tensor_scalar(
                        out=iw[:, blk, :], in0=pvv[:, 64:72], scalar1=1.0 / (8.0 * 8.0 ** 0.5), scalar2=None,
                        op0=ALU.mult), reads=[Rpv], writes=[Riw])

            if DBG.get("dump_d1"):
                dd = [(qT[:, 0, 0:512], 0, 0, 512), (kT2[:, 0:512], 0, 512, 512), (iqT[:, 0, 0:512], 128, 0, 512),
                      (ikT2[:, 0:512], 128, 512, 512), (Vaug[:, 0:4, :].rearrange("p a b -> p (a b)"), 256, 0, 260),
                      (iw[:, 0:4, :].rearrange("p a b -> p (a b)"), 256, 512, 32)]
                for ap_, r0, c0_, w_ in dd:
                    S.dma("pool", lambda e, ap_=ap_, r0=r0, c0_=c0_, w_=w_: e.dma_start(
                        out=dst[r0:r0 + 128, c0_:c0_ + w_], in_=ap_), reads=[RqT, RkT2, RiqT, RikT2, RV, Riw], writes=[])
            for qb in (DBG["qbs"] if DBG["qbs"] is not None else range(SEQ // 128)):
                nk = (qb + 1) * 128
                q0 = qb * 128
                S.dma("sp", lambda e, a=base + q0: e.dma_start(out=xres[:], in_=src[a:a + 128, :]), writes=[Rxres])
                n = 0
                for h in range(8):
                    pr_ = (h % 2) * 64
                    for c0 in range(0, nk, 512):
                        w = min(512, nk - c0)
                        pi, Rpi = B[n % 2]
                        rl, Rrl = rls[n % 2]
                        n += 1
                        piv = pi[:].rearrange("p a b -> p (a b)")[:, 0:w]
                        S.op("pe", lambda e, h=h, pr_=pr_, c0=c0, w=w, piv=piv: e.matmul(
                            piv, lhsT=iqT[pr_:pr_ + 64, h // 2, q0:q0 + 128], rhs=ikT2[pr_:pr_ + 64, c0:c0 + w],
                            start=True, stop=True), reads=[RiqT, RikT2], writes=[Rpi])
                        S.op("act", lambda e, rl=rl, piv=piv, w=w: e.activation(out=rl[:, 0:w], in_=piv, func=AF.Relu),
                             reads=[Rpi], writes=[Rrl])
                        if h == 0:
                            S.op("dve", lambda e, rl=rl, c0=c0, w=w, qb=qb: e.tensor_scalar(
                                out=score[:, c0:c0 + w], in0=rl[:, 0:w], scalar1=iw[:, qb, 0:1], scalar2=None,
                                op0=ALU.mult), reads=[Rrl, Riw], writes=[Rscore])
                        else:
                            S.op("dve", lambda e, rl=rl, c0=c0, w=w, qb=qb, h=h: e.scalar_tensor_tensor(
                                out=score[:, c0:c0 + w], in0=rl[:, 0:w], scalar=iw[:, qb, h:h + 1],
                                in1=score[:, c0:c0 + w], op0=ALU.mult, op1=ALU.add), reads=[Rrl, Riw, Rscore],
                                writes=[Rscore])
                S.op("dve", lambda e, q0=q0, nk=nk: e.tensor_tensor(out=score[:, q0:nk], in0=score[:, q0:nk], in1=negm[:],
                                                                   op=ALU.add), reads=[Rscore, Rnegm], writes=[Rscore])
                if qb >= 2:
                    S.op("dve", lambda e, nk=nk: e.tensor_reduce(out=bis[:, 0:1], in_=score[:, 0:nk], axis=AX.X,
                                                                 op=ALU.max), reads=[Rscore], writes=[Rbis])
                    S.op("dve", lambda e, nk=nk: e.tensor_reduce(out=bis[:, 1:2], in_=score[:, 0:nk - 128], axis=AX.X,
                                                                 op=ALU.min), reads=[Rscore], writes=[Rbis])
                    S.op("dve", lambda e: e.tensor_tensor(out=bis[:, 2:3], in0=bis[:, 0:1], in1=bis[:, 1:2],
                                                          op=ALU.subtract), reads=[Rbis], writes=[Rbis])
                    S.op("dve", lambda e: e.tensor_scalar(out=steps[:], in0=pow2[:], scalar1=bis[:, 2:3], scalar2=None,
                                                          op0=ALU.mult), reads=[Rbis, Rpow2], writes=[Rsteps])
                    S.op("pool", lambda e: e.memset(cnts[:], 0.0), writes=[Rcnts])
                    for k in range(NIT):
                        S.op("dve", lambda e, k=k: e.tensor_tensor(out=bis[:, 3:4], in0=bis[:, 1:2], in1=steps[:, k:k + 1],
                                                                   op=ALU.add), reads=[Rbis, Rsteps], writes=[Rbis])
                        S.op("dve", lambda e, k=k, nk=nk: e.tensor_scalar(
                            out=jcnt[:, 0:nk], in0=score[:, 0:nk], scalar1=bis[:, 3:4], scalar2=0.0, op0=ALU.is_ge,
                            op1=ALU.add, accum_out=cnts[:, k:k + 1]), reads=[Rscore, Rbis, Rcnts],
                            writes=[Rjcnt, Rcnts])
                        S.op("dve", lambda e, k=k: e.scalar_tensor_tensor(
                            out=bis[:, 4:5], in0=cnts[:, k:k + 1], scalar=float(TOPK), in1=steps[:, k:k + 1],
                            op0=ALU.is_ge, op1=ALU.mult), reads=[Rcnts, Rsteps], writes=[Rbis])
                        S.op("dve", lambda e: e.tensor_tensor(out=bis[:, 1:2], in0=bis[:, 1:2], in1=bis[:, 4:5],
                                                              op=ALU.add), reads=[Rbis], writes=[Rbis])
                    thr, Rthr = bis[:, 1:2], Rbis
                else:
                    thr, Rthr = thrc[:, 0:1], Rthrc
                S.op("dve", lambda e, nk=nk, thr=thr: e.tensor_scalar(out=score[:, 0:nk], in0=score[:, 0:nk], scalar1=thr,
                                                                      scalar2=None, op0=ALU.is_ge),
                     reads=[Rscore, Rthr], writes=[Rscore])
                for jb in range(0, qb + 1, 4):
                    nb = min(4, qb + 1 - jb)
                    pmk, Rpmk = B[2 + (jb // 4) % 2]
                    for i in range(nb):
                        S.op("pe", lambda e, i=i, jb=jb, pmk=pmk: e.transpose(
                            pmk[:, i, :], score[:, (jb + i) * 128:(jb + i + 1) * 128], idf[:]),
                            reads=[Rscore, Ridf], writes=[Rpmk])
                    S.op("act", lambda e, jb=jb, nb=nb, pmk=pmk: e.copy(out=maskT[:, jb:jb + nb, :], in_=pmk[:, 0:nb, :]),
                         reads=[Rpmk], writes=[RmaskT])
                n = 0
                for j in range(qb + 1):
                    for g in range(4):
                        par, half = g % 2, g // 2
                        pst, Rpst = B[4 + n % 2]
                        E, RE = Es[n % 2]
                        PT, RPT = PTs[n % 2]
                        n += 1
                        S.op("pe", lambda e, par=par, half=half, j=j, pst=pst: e.matmul(
                            pst[:], lhsT=kT2[par * 64:par * 64 + 64, j * 128:(j + 1) * 128],
                            rhs=qT[par * 64:par * 64 + 64, 4 * half:4 * half + 4, q0:q0 + 128], start=True, stop=True),
                            reads=[RkT2, RqT], writes=[Rpst])
                        S.op("act", lambda e, E=E, pst=pst: e.activation(out=E[:], in_=pst[:], func=AF.Exp, scale=0.125),
                             reads=[Rpst], writes=[RE])
                        S.op(DBG.get("addeng", "pool"), lambda e, E=E, PT=PT, j=j: e.tensor_tensor(
                            out=PT[:], in0=E[:], in1=maskT[:, j:j + 1, :].to_broadcast([128, 4, 128]), op=ALU.mult),
                            reads=[RE, RmaskT], writes=[RPT])
                        ot, Rot = B[g]
                        S.op("pe", lambda e, ot=ot, PT=PT, j=j: e.matmul(
                            ot[0:65], lhsT=Vaug[:, j, :], rhs=PT[:], start=(j == 0), stop=(j == qb)),
                            reads=[RV, RPT], writes=[Rot])
                for g in range(4):
                    ot, Rot = B[g]
                    otv = ot[:].rearrange("p a b -> p (a b)")
                    pbc, Rpbc = B[6]
                    pbcv = pbc[:].rearrange("p a b -> p (a b)")
                    at, Rat = attnT[g]
                    S.op("dve", lambda e, otv=otv: e.reciprocal(out=rs[64:65, :], in_=otv[64:65, :]), reads=[Rot],
                         writes=[Rrs])
                    S.op("pe", lambda e, pbcv=pbcv: e.matmul(pbcv[0:64, :], lhsT=onesf[64:65, 0:64], rhs=rs[64:65, :],
                                                             start=True, stop=True), reads=[Rones, Rrs], writes=[Rpbc])
                    S.op("act", lambda e, pbcv=pbcv: e.copy(out=bc[:], in_=pbcv[0:64, :]), reads=[Rpbc], writes=[Rbc])
                    S.op("dve", lambda e, at=at, otv=otv: e.tensor_tensor(
                        out=at[:].rearrange("p a b -> p (a b)"), in0=otv[0:64, :], in1=bc[:], op=ALU.mult),
                        reads=[Rot, Rbc], writes=[Rat])
                for dh in range(2):
                    po, Rpo = B[6]
                    pov = po[:].rearrange("p a b -> p (a b)")
                    n2 = 0
                    for g in range(4):
                        par, half = g % 2, g // 2
                        at, Rat = attnT[g]
                        for i in range(4):
                            hh = 2 * (4 * half + i) + par
                            S.op("pe", lambda e, at=at, i=i, hh=hh, dh=dh, pov=pov, n2=n2: e.matmul(
                                pov, lhsT=at[:, i, :], rhs=wo[:, hh, dh * 512:(dh + 1) * 512], start=(n2 == 0),
                                stop=(n2 == 15)), reads=[Rat, Rwo], writes=[Rpo])
                            n2 += 1
                    S.op("dve", lambda e, dh=dh, pov=pov: e.tensor_tensor(
                        out=xres[:, dh * 512:(dh + 1) * 512], in0=pov, in1=xres[:, dh * 512:(dh + 1) * 512], op=ALU.add),
                        reads=[Rpo, Rxres], writes=[Rxres])
                S.dma("pool", lambda e, a=base + q0: e.dma_start(out=dst[a:a + 128, :], in_=xres[:]), reads=[Rxres],
                      writes=[])
                if DBG.get("dump_mask") is not None and qb == DBG["dump_mask"]:
                    S.dma("pool", lambda e: e.dma_start(out=dst[2048:2176, 0:nk], in_=score[:, 0:nk]), reads=[Rscore])
                    S.dma("pool", lambda e: e.dma_start(out=dst[2176:2304, 0:8], in_=bis[:]), reads=[Rbis])
                    S.dma("pool", lambda e: e.dma_start(out=dst[2176:2304, 8:8 + NIT], in_=cnts[:]), reads=[Rcnts])
                if DBG.get("dump_d2") and qb == 0:
                    S.dma("pool", lambda e: e.dma_start(out=dst[1024:1152, 0:128], in_=score[:, 0:128]), reads=[Rscore])
                    S.dma("pool", lambda e: e.dma_start(out=dst[1024:1152, 128:256], in_=maskT[:, 0, :]), reads=[RmaskT])
                    for g in range(4):
                        S.dma("pool", lambda e, g=g: e.dma_start(out=dst[1152 + 64 * g:1216 + 64 * g, 0:512],
                                                                 in_=attnT[g][0][:].rearrange("p a b -> p (a b)")),
                              reads=[attnT[g][1]])
                    S.dma("pool", lambda e: e.dma_start(out=dst[1024:1152, 512:1024], in_=rs[:]), reads=[Rrs])
        S.flush()


def dsa_consts():
    inv = (10000.0 ** (-(np.arange(0, 64, 2, dtype=np.float32)) / np.float32(64))).astype(np.float32)
    ang = (np.arange(SEQ, dtype=np.float32)[:, None] * inv[None, :]).astype(np.float32)
    cos, sin = np.cos(ang).astype(np.float32), np.sin(ang).astype(np.float32)
    p = np.arange(128)
    d = p % 64
    c_cos = np.ascontiguousarray(cos[:, d % 32].T)
    sgn = np.where(d < 32, -1.0, 1.0).astype(np.float32)
    c_sin = np.ascontiguousarray((sin[:, d % 32] * sgn[None, :]).T.astype(np.float32))
    pm = np.zeros((128, 128), np.float32)
    partner = (d + 32) % 64 + 64 * (p // 64)
    pm[p, partner] = 1.0
    negmask = np.zeros((128, 128), np.float32)
    negmask[:64, 64:] = -1e30
    pow2 = np.tile((2.0 ** -(1.0 + np.arange(NIT))).astype(np.float32)[None, :], (128, 1))
    return {"c_cos": c_cos, "c_sin": c_sin, "c_pm": pm, "c_negmask": negmask, "c_pow2": np.ascontiguousarray(pow2)}


TL = 256
NCH = TL // 128
CDEC = float(np.exp(-0.5))
P_MU, P_W0, P_AB, P_KK, P_KA, P_RK, P_CB, P_BRG, P_BIG, P_LAM, P_CW, P_G = 0, 14, 18, 22, 26, 30, 34, 38, 42, 46, 50, 66
NP0 = 74


def phase_l0(nc, S, src, dst, Dm):
    with ExitStack() as st:
        C = Ctx(nc, S, st)
        S.barrier()
        win, _ = C.sb([128, 8, 2816], BF16, "win")
        Rwin = [S.res() for _ in range(8)]
        load_weight(S, "pool", win, Dm["w_in0"], 8, Rwin)
        wo, _ = C.sb([128, 8, D], BF16, "wo0")
        Rwo = [S.res() for _ in range(8)]
        load_weight(S, "pool", wo, Dm["w_out0"], 8, Rwo)
        P0, RP0 = C.sb([128, NP0], F32, "P0")
        S.dma("sp", lambda e: e.dma_start(out=P0[:], in_=Dm["p0"]), writes=[RP0])
        lora, Rlora = C.sb([128, 512], BF16, "lora")
        S.dma("pool", lambda e: e.dma_start(out=lora[:], in_=Dm["lora0"]), writes=[Rlora])
        wgu, Rwgu = C.sb([128, 512], BF16, "wgu")
        S.dma("pool", lambda e: e.dma_start(out=wgu[:], in_=Dm["w_g_up0"]), writes=[Rwgu])
        gbd, Rgbd = C.sb([128, 8, 128], BF16, "gbd")
        S.dma("pool", lambda e: e.dma_start(out=gbd[:], in_=Dm["gates_bd0"]), writes=[Rgbd])
        lnw, Rlnw = C.sb([128, 2, 512], F32, "lnw")
        S.dma("sp", lambda e: e.dma_start(out=lnw[:, 0, :], in_=Dm["lnx0"][0, :].partition_broadcast(128)), writes=[Rlnw])
        S.dma("sp", lambda e: e.dma_start(out=lnw[:, 1, :], in_=Dm["lnx0"][1, :].partition_broadcast(128)), writes=[Rlnw])
        cm, Rcm = C.sb([128, 128 + 256 + 256 + 128 + 2], BF16, "cm")
        S.dma("pool", lambda e: e.dma_start(out=cm[:], in_=Dm["c_masks0"]), writes=[Rcm])
        maskL, maskU2, maskU3, BD, IND = cm[:, 0:128], cm[:, 128:384], cm[:, 384:640], cm[:, 640:768], cm[:, 768:770]
        idf, Ridf, idb, Ridb = make_ident(S, C)
        onesT, Rones = C.sb([128, 128], F32, "onesT")
        S.op("pool", lambda e: e.memset(onesT[:], 1.0), writes=[Rones])
        spt, Rspt = C.sb([128, 16], F32, "spt")
        S.op("act", lambda e: e.activation(out=spt[:, 0:4], in_=P0[:, P_LAM:P_LAM + 4], func=AF.Exp, scale=-1.0),
             reads=[RP0], writes=[Rspt])
        S.op("act", lambda e: e.activation(out=spt[:, 0:4], in_=spt[:, 0:4], func=AF.Ln, bias=1.0), reads=[Rspt],
             writes=[Rspt])
        for i, m in ((1, 8.0), (2, -8.0), (3, -16.0)):
            S.op("dve", lambda e, i=i, m=m: e.tensor_scalar(out=spt[:, 4 * i:4 * i + 4], in0=spt[:, 0:4], scalar1=m,
                                                            scalar2=None, op0=ALU.mult), reads=[Rspt], writes=[Rspt])

        xts = [C.sb([128, NCH, D], F32) for _ in range(2)]
        hT, RhT = C.sb([128, 8, TL], BF16, "hT")
        junk, Rjunk = C.sb([128, D], BF16, "junk")
        ss, Rss = C.sb([128, 1], F32, "ss")
        xn, Rxn = C.sb([128, D], BF16, "xn")
        Pb = [C.sb([128, TL + 1], F32) for _ in range(14)]
        LX = [C.sb([128, TL + 3], F32) for _ in range(4)]
        hprev, Rhprev = C.sb([128, 4], F32, "hprev")
        tmps = {}

        def tmp(name, dt=F32, shape=None):
            if name not in tmps:
                tmps[name] = C.sb(shape or [128, TL], dt, "tmp_" + name)
            return tmps[name]

        ART = [C.sb([128, NCH, 2, 128], BF16) for _ in range(4)]
        BTt = [C.sb([128, TL], BF16) for _ in range(4)]
        KTt = [C.sb([128, TL], BF16) for _ in range(4)]
        BTm = [C.sb([128, 2, TL], BF16) for _ in range(4)]
        KTm = [C.sb([128, 2, TL], BF16) for _ in range(4)]
        Hm = [C.sb([128, 2, 64], BF16) for _ in range(4)]
        hmk, Rhmk = C.sb([128, 2], F32, "hmk")
        S.op("pool", lambda e: e.memset(hmk[:], 0.0), writes=[Rhmk])
        S.op("pool", lambda e: e.memset(hmk[0:64, 0:1], 1.0), writes=[Rhmk])
        S.op("pool", lambda e: e.memset(hmk[64:128, 1:2], 1.0), writes=[Rhmk])
        TM = [C.sb([128, 3, 4, 128], BF16) for _ in range(NCH)]
        WL, RWL = C.sb([128, 4, NCH], F32, "WL")
        Hs = [C.sb([128, 64], F32) for _ in range(4)]
        Hb = [C.sb([128, 64], BF16) for _ in range(4)]
        mixT, RmixT = C.sb([128, 8, TL], BF16, "mixT")
        bons, Rbons = C.sb([128, NCH, 8], F32, "bons")
        pT, RpT = C.ps([128, 8, 128], BF16, "pT")
        PP, RPP_ = C.ps([128, 2, TL], F32, "PP")
        RPP = [S.res(bank=RPP_.bank), S.res(bank=RPP_.bank)]
        BKA, RBKA_ = C.ps([128, 2, 256], F32, "BKA")
        BKB, RBKB_ = C.ps([128, 2, 256], F32, "BKB")
        RBK = [S.res(bank=RBKA_.bank), S.res(bank=RBKA_.bank), S.res(bank=RBKB_.bank), S.res(bank=RBKB_.bank)]
        slots = [BKA[:, 0, :], BKA[:, 1, :], BKB[:, 0, :], BKB[:, 1, :]]
        BKC, RBKC_ = C.ps([128, 4, 128], F32, "BKC")
        RBKC = [S.res(bank=RBKC_.bank), S.res(bank=RBKC_.bank)]
        BKD, RBKD_ = C.ps([128, 512], F32, "BKD")
        RX, RU, RdH, Rbon = [S.res(bank=RBKD_.bank) for _ in range(4)]
        YT, RYT = C.ps([128, 8, 64], F32, "YT")
        GP, RGP = C.ps([128, 512], F32, "GP")
        nslot = [0]

        def slot():
            i = nslot[0] % 4
            nslot[0] += 1
            return slots[i], RBK[i]

        def PV(col, n=4):
            return P0[:, col:col + 1] if n == 1 else P0[:, col:col + n]

        for sq in range(DBG["nseq"]):
            base = sq * SEQ
            for i in range(14):
                S.op("pool", lambda e, i=i: e.memset(Pb[i][0][:, 0:1], 0.0), writes=[Pb[i][1]])
            for i in range(4):
                S.op("pool", lambda e, i=i: e.memset(LX[i][0][:, 0:3], 0.0), writes=[LX[i][1]])
                S.op("pool", lambda e, i=i: e.memset(Hs[i][0][:], 0.0), writes=[Hs[i][1]])
                S.op("pool", lambda e, i=i: e.memset(Hb[i][0][:], 0.0), writes=[Hb[i][1]])
                S.op("pool", lambda e, i=i: e.memset(Hm[i][0][:], 0.0), writes=[Hm[i][1]])
            S.op("pool", lambda e: e.memset(hprev[:], 0.0), writes=[Rhprev])
            for tt in range(DBG["l0_tiles"] or SEQ // TL):
                t0 = tt * TL
                xt, Rxt = xts[tt % 2]
                S.dma("sp", lambda e, xt=xt, a=base + t0: e.dma_start(
                    out=xt[:], in_=src[a:a + TL, :].rearrange("(s p) d -> p s d", p=128)), writes=[Rxt])
                for s in range(NCH):
                    norm_T(S, xt[:, s, :], Rxt, P0[:, P_G:P_G + 8], RP0, hT[:, :, s * 128:(s + 1) * 128], RhT,
                           (junk, Rjunk, ss, Rss, xn, Rxn, pT, RpT, idb, Ridb))
                npp = [0]

                def proj(nt):
                    i = npp[0] % 2
                    npp[0] += 1
                    for k in range(8):
                        S.op("pe", lambda e, k=k, nt=nt, i=i: e.matmul(
                            PP[:, i, :], lhsT=win[:, k, nt * 128:(nt + 1) * 128], rhs=hT[:, k, :], start=(k == 0),
                            stop=(k == 7)), reads=[Rwin[k], RhT], writes=[RPP[i]])
                    return PP[:, i, :], RPP[i]

                def shift(nt, name):
                    pp, Rpp = proj(nt)
                    Pt, RPt = Pb[nt]
                    o, Ro = tmp(name)
                    d_, Rd_ = tmp("shd")
                    S.op("act", lambda e: e.copy(out=Pt[:, 1:TL + 1], in_=pp), reads=[Rpp], writes=[RPt])
                    S.op("dve", lambda e: e.tensor_tensor(out=d_[:], in0=Pt[:, 0:TL], in1=Pt[:, 1:TL + 1], op=ALU.subtract),
                         reads=[RPt], writes=[Rd_])
                    S.op("dve", lambda e: e.scalar_tensor_tensor(out=o[:], in0=d_[:], scalar=P0[:, P_MU + nt:P_MU + nt + 1],
                                                                 in1=Pt[:, 1:TL + 1], op0=ALU.mult, op1=ALU.add),
                         reads=[Rd_, RPt, RP0], writes=[Ro])
                    S.op("act", lambda e: e.copy(out=Pt[:, 0:1], in_=Pt[:, TL:TL + 1]), reads=[RPt], writes=[RPt])
                    return o, Ro

                LV = DBG.get('l0_lvl', 9)
                if LV < 2:
                    continue
                swa, Rswa = shift(12, "s_wa")
                lor, Rlor = tmp("lor", BF16)
                S.op("act", lambda e: e.activation(out=lor[0:64, :], in_=swa[0:64, :], func=AF.Tanh), reads=[Rswa],
                     writes=[Rlor])
                S.op("act", lambda e: e.copy(out=lor[64:128, :], in_=swa[64:128, :]), reads=[Rswa], writes=[Rlor])
                sgd_, Rsgd_ = shift(13, "s_gd")
                sgd, Rsgd = tmp("sgd", BF16)
                S.op("act", lambda e: e.activation(out=sgd[:], in_=sgd_[:], func=AF.Sigmoid), reads=[Rsgd_], writes=[Rsgd])

                if LV < 3:
                    continue
                for j in range(4):
                    r_, Rr = shift(j, "s_r")
                    k_, Rk = shift(4 + j, "s_k")
                    v_, Rv = shift(8 + j, "s_v")
                    art, Rart = ART[j]
                    bt, Rbt = BTt[j]
                    kt, Rkt = KTt[j]
                    zw, Rzw = slot()
                    S.op("pe", lambda e, j=j, zw=zw: e.matmul(zw, lhsT=lora[0:64, j * 128:(j + 1) * 128], rhs=lor[0:64, :],
                                                              start=True, stop=True), reads=[Rlora, Rlor], writes=[Rzw])
                    ee, Ree = tmp("ee")
                    S.op("act", lambda e, j=j, zw=zw: e.activation(out=ee[:], in_=zw, func=AF.Sigmoid,
                                                                  bias=P0[:, P_W0 + j:P_W0 + j + 1]),
                         reads=[Rzw, RP0], writes=[Ree])
                    za, Rza = slot()
                    S.op("pe", lambda e, j=j, za=za: e.matmul(za, lhsT=lora[64:128, j * 128:(j + 1) * 128],
                                                              rhs=lor[64:128, :], start=True, stop=True),
                         reads=[Rlora, Rlor], writes=[Rza])
                    aa, Raa = tmp("aa")
                    S.op("act", lambda e, j=j, za=za: e.activation(out=aa[:], in_=za, func=AF.Sigmoid,
                                                                  bias=P0[:, P_AB + j:P_AB + j + 1]),
                         reads=[Rza, RP0], writes=[Raa])
                    cs, Rcs = tmp("cs")
                    for c in range(NCH):
                        S.op("dve", lambda e, c=c: e.tensor_tensor_scan(
                            out=cs[:, c * 128:(c + 1) * 128], data0=onesT[:], data1=ee[:, c * 128:(c + 1) * 128],
                            initial=0.0, op0=ALU.mult, op1=ALU.add), reads=[Rones, Ree], writes=[Rcs])
                    csm, Rcsm = tmp("csm")
                    S.op("dve", lambda e: e.tensor_tensor(out=csm[:], in0=cs[:], in1=ee[:], op=ALU.subtract),
                         reads=[Rcs, Ree], writes=[Rcsm])
                    einv, Reinv = tmp("einv")
                    edec, Redec = tmp("edec")
                    eprev, Reprev = tmp("eprev")
                    S.op("act", lambda e: e.activation(out=einv[:], in_=cs[:], func=AF.Exp, scale=CDEC), reads=[Rcs],
                         writes=[Reinv])
                    S.op("act", lambda e: e.activation(out=edec[:], in_=cs[:], func=AF.Exp, scale=-CDEC), reads=[Rcs],
                         writes=[Redec])
                    S.op("act", lambda e: e.activation(out=eprev[:], in_=csm[:], func=AF.Exp, scale=-CDEC), reads=[Rcsm],
                         writes=[Reprev])
                    S.op("dve", lambda e, j=j: e.tensor_copy(
                        out=WL[:, j, :], in_=edec[:].rearrange("p (c t) -> p c t", t=128)[:, :, 127]), reads=[Redec],
                        writes=[RWL])
                    kk, Rkk = tmp("kk")
                    S.op("dve", lambda e, j=j: e.tensor_scalar(out=kk[:], in0=k_[:], scalar1=P0[:, P_KK + j:P_KK + j + 1],
                                                               scalar2=None, op0=ALU.mult), reads=[Rk, RP0], writes=[Rkk])
                    kk2, Rkk2 = tmp("kk2", BF16)
                    S.op("pool", lambda e: e.tensor_tensor(out=kk2[:], in0=kk[:], in1=kk[:], op=ALU.mult), reads=[Rkk],
                         writes=[Rkk2])
                    zs, Rzs = slot()
                    S.op("pe", lambda e, zs=zs: e.matmul(zs, lhsT=BD, rhs=kk2[:], start=True, stop=True),
                         reads=[Rcm, Rkk2], writes=[Rzs])
                    rn, Rrn = tmp("rn")
                    S.op("act", lambda e, zs=zs: e.activation(out=rn[:], in_=zs, func=AF.Ln, bias=1e-24), reads=[Rzs],
                         writes=[Rrn])
                    S.op("act", lambda e: e.activation(out=rn[:], in_=rn[:], func=AF.Exp, scale=-0.5), reads=[Rrn],
                         writes=[Rrn])
                    S.op("dve", lambda e: e.tensor_tensor(out=kk[:], in0=kk[:], in1=rn[:], op=ALU.mult), reads=[Rkk, Rrn],
                         writes=[Rkk])
                    km, Rkm = tmp("km")
                    S.op("dve", lambda e, j=j: e.tensor_scalar(out=km[:], in0=aa[:], scalar1=-1.0,
                                                               scalar2=P0[:, P_KA + j:P_KA + j + 1], op0=ALU.add,
                                                               op1=ALU.mult), reads=[Raa, RP0], writes=[Rkm])
                    S.op("dve", lambda e: e.scalar_tensor_tensor(out=km[:], in0=km[:], scalar=1.0, in1=k_[:], op0=ALU.add,
                                                                 op1=ALU.mult), reads=[Rkm, Rk], writes=[Rkm])
                    S.op("dve", lambda e, art=art: e.tensor_tensor(
                        out=art[:, :, 0, :], in0=kk[:].rearrange("p (c t) -> p c t", t=128),
                        in1=eprev[:].rearrange("p (c t) -> p c t", t=128), op=ALU.mult), reads=[Rkk, Reprev], writes=[Rart])
                    S.op("pool", lambda e, art=art: e.tensor_tensor(
                        out=art[:, :, 1, :], in0=r_[:].rearrange("p (c t) -> p c t", t=128),
                        in1=edec[:].rearrange("p (c t) -> p c t", t=128), op=ALU.mult), reads=[Rr, Redec], writes=[Rart])
                    tb, Rtb = tmp("tb")
                    S.op("pool", lambda e: e.tensor_tensor(out=tb[:], in0=kk[:], in1=aa[:], op=ALU.mult), reads=[Rkk, Raa],
                         writes=[Rtb])
                    S.op("dve", lambda e, bt=bt: e.tensor_tensor(out=bt[:], in0=tb[:], in1=einv[:], op=ALU.mult),
                         reads=[Rtb, Reinv], writes=[Rbt])
                    S.op("pool", lambda e, kt=kt: e.tensor_tensor(out=kt[:], in0=km[:], in1=einv[:], op=ALU.mult),
                         reads=[Rkm, Reinv], writes=[Rkt])
                    btm, Rbtm = BTm[j]
                    ktm, Rktm = KTm[j]
                    for h in range(2):
                        S.op("dve", lambda e, h=h, btm=btm, bt=bt: e.tensor_scalar(
                            out=btm[:, h, :], in0=bt[:], scalar1=hmk[:, h:h + 1], scalar2=None, op0=ALU.mult),
                            reads=[Rbt, Rhmk], writes=[Rbtm])
                        S.op("pool", lambda e, h=h, ktm=ktm, kt=kt: e.tensor_scalar(
                            out=ktm[:, h, :], in0=kt[:], scalar1=hmk[:, h:h + 1], scalar2=None, op0=ALU.mult),
                            reads=[Rkt, Rhmk], writes=[Rktm])
                    rkr, Rrkr = tmp("rkr", BF16)
                    S.op("dve", lambda e, j=j: e.scalar_tensor_tensor(out=rkr[:], in0=r_[:],
                                                                      scalar=P0[:, P_RK + j:P_RK + j + 1], in1=km[:],
                                                                      op0=ALU.mult, op1=ALU.mult), reads=[Rr, Rkm, RP0],
                         writes=[Rrkr])
                    for c in range(NCH):
                        S.op("pe", lambda e, c=c, j=j: e.matmul(BKD[:, 384 + c * 8 + 2 * j:384 + c * 8 + 2 * j + 2],
                                                                lhsT=rkr[:, c * 128:(c + 1) * 128], rhs=IND, start=True,
                                                                stop=True), reads=[Rrkr, Rcm], writes=[Rbon])
                    vb, Rvb = tmp("vb", BF16)
                    S.op("act", lambda e: e.copy(out=vb[:], in_=v_[:]), reads=[Rv], writes=[Rvb])
                    for c in range(NCH):
                        tm, Rtm = TM[c]
                        for i, (srct, Rs) in enumerate(((kt, Rkt), (bt, Rbt), (vb, Rvb))):
                            S.op("pe", lambda e, i=i, c=c, srct=srct: e.transpose(
                                pT[:, i, :], srct[:, c * 128:(c + 1) * 128], idb[:]), reads=[Rs, Ridb], writes=[RpT])
                        S.op("act", lambda e, tm=tm, j=j: e.copy(out=tm[:, :, j, :], in_=pT[:, 0:3, :]), reads=[RpT],
                             writes=[Rtm])
                S.op("act", lambda e: e.copy(out=bons[:].rearrange("p c h -> p (c h)"), in_=BKD[:, 384:384 + NCH * 8]),
                     reads=[Rbon], writes=[Rbons])

                if DBG.get("dummy_alloc"):
                    for j_ in range(4):
                        tmp(f"M0_{j_}", BF16, [128, 2, 128]); tmp(f"NB_{j_}", BF16, [128, 2, 256]); tmp(f"NK_{j_}", BF16, [128, 2, 256])
                if LV < 4:
                    continue
                for c in range(NCH):
                    tm, Rtm = TM[c]
                    for j in range(4):
                        art, Rart = ART[j]
                        bt, Rbt = BTt[j]
                        kt, Rkt = KTt[j]
                        Hf, RHf = Hs[j]
                        Hh, RHh = Hm[j]
                        btm, Rbtm = BTm[j]
                        ktm, Rktm = KTm[j]
                        M0, RM0 = tmp(f"M0_{j}", BF16, [128, 2, 128])
                        NB, RNB = tmp(f"NB_{j}", BF16, [128, 2, 256])
                        NK, RNK = tmp(f"NK_{j}", BF16, [128, 2, 256])
                        pa, Rpa = BKC[:, 0:2, :], RBKC[0]
                        for h in range(2):
                            po = h * 64
                            v_ = DBG.get("p1var", 0)
                            if v_ == 3:
                                if c == 0 and j == 0 and h == 0:
                                    S.op("pe", lambda e, h=h, po=po, art=art, bt=bt, c=c: e.matmul(
                                        GP[:, 0:128], lhsT=bt[0:64, 0:128], rhs=bt[0:64, 0:128],
                                        start=True, stop=True), reads=[Rbt], writes=[RGP])
                                continue
                            if v_ in (5, 6, 7):
                                if (v_ == 5 and c == 0) or (v_ == 6 and j == 0) or (v_ == 7 and h == 0):
                                    S.op("pe", lambda e, h=h, po=po, art=art, bt=bt, c=c: e.matmul(
                                        GP[:, 0:128], lhsT=bt[po:po + 64, 0:128], rhs=bt[po:po + 64, 0:128],
                                        start=True, stop=True), reads=[Rbt], writes=[RGP])
                                continue
                            if v_ == 4:
                                if c == 0 and j == 0 and h == 0:
                                    S.op("pe", lambda e, h=h, po=po, art=art, bt=bt, c=c: e.matmul(
                                        GP[:, 0:128], lhsT=idb[:], rhs=idb[:],
                                        start=True, stop=True), reads=[Ridb], writes=[RGP])
                                continue
                            if v_ == 1:
                                S.op("pe", lambda e, h=h, po=po, art=art, bt=bt, c=c: e.matmul(
                                    BKC[:, h, :], lhsT=bt[po:po + 64, c * 128:(c + 1) * 128], rhs=bt[po:po + 64, c * 128:(c + 1) * 128],
                                    start=True, stop=True), reads=[Rbt], writes=[Rpa])
                                continue
                            if v_ == 2:
                                S.op("pe", lambda e, h=h, po=po, art=art, bt=bt, c=c: e.matmul(
                                    GP[:, h * 128:(h + 1) * 128], lhsT=art[po:po + 64, c, 0, :], rhs=bt[po:po + 64, c * 128:(c + 1) * 128],
                                    start=True, stop=True), reads=[Rart, Rbt], writes=[RGP])
                                continue
                            S.op("pe", lambda e, h=h, po=po, art=art, btm=btm, c=c: e.matmul(
                                BKC[:, h, :], lhsT=art[:, c, 0, :], rhs=btm[:, h, c * 128:(c + 1) * 128],
                                start=True, stop=True), reads=[Rart, Rbtm], writes=[Rpa])
                        if DBG.get('l0_n', 9) < 1:
                            continue
                        S.op("dve", lambda e, M0=M0: e.scalar_tensor_tensor(
                            out=M0[:], in0=BKC[:, 0:2, :], scalar=-1.0, in1=maskL.unsqueeze(1).to_broadcast([128, 2, 128]),
                            op0=ALU.mult, op1=ALU.mult), reads=[Rpa, Rcm], writes=[RM0])
                        if DBG.get('l0_n', 9) < 2:
                            continue
                        for h in range(2):
                            po = h * 64
                            S.op("pe", lambda e, h=h, po=po, art=art, btm=btm, c=c: e.matmul(
                                BKA[:, h, :], lhsT=btm[:, h, c * 128:(c + 1) * 128],
                                rhs=art[:, c, :, :].rearrange("p a t -> p (a t)"), start=True, stop=True),
                                reads=[Rart, Rbtm], writes=[RBK[0], RBK[1]])
                        if DBG.get('l0_n', 9) < 3:
                            continue
                        S.op("dve", lambda e, NB=NB: e.tensor_tensor(
                            out=NB[:], in0=BKA[:], in1=maskU2.unsqueeze(1).to_broadcast([128, 2, 256]), op=ALU.mult),
                            reads=[RBK[0], RBK[1], Rcm], writes=[RNB])
                        for h in range(2):
                            po = h * 64
                            S.op("pe", lambda e, h=h, po=po, art=art, ktm=ktm, c=c: e.matmul(
                                BKB[:, h, :], lhsT=ktm[:, h, c * 128:(c + 1) * 128],
                                rhs=art[:, c, :, :].rearrange("p a t -> p (a t)"), start=True, stop=True),
                                reads=[Rart, Rktm], writes=[RBK[2], RBK[3]])
                        S.op("dve", lambda e, NK=NK: e.tensor_tensor(
                            out=NK[:], in0=BKB[:], in1=maskU3.unsqueeze(1).to_broadcast([128, 2, 256]), op=ALU.mult),
                            reads=[RBK[2], RBK[3], Rcm], writes=[RNK])
                        SL = DBG.get('l0_sub', 9)
                        if SL < 2:
                            continue
                        Q, RQ = tmp(f"Q_{j}", BF16, [128, 2, 128])
                        S.op("pool", lambda e, Q=Q, NB=NB: e.tensor_tensor(
                            out=Q[:], in0=NB[:, :, 0:128], in1=idb[:].unsqueeze(1).to_broadcast([128, 2, 128]), op=ALU.add),
                            reads=[RNB, Ridb], writes=[RQ])
                        Mc, RMc = M0, RM0
                        McT, RMcT = NB[:, :, 0:128], RNB
                        for i in range(1, 7):
                            Mn, RMn = tmp(f"M{i % 2}_{j}", BF16, [128, 2, 128])
                            for h in range(2):
                                S.op("pe", lambda e, h=h, Mc=Mc, McT=McT: e.matmul(
                                    BKC[:, 2 + h, :], lhsT=McT[:, h, :], rhs=Mc[:, h, :], start=True, stop=True),
                                    reads=[RMc, RMcT], writes=[RBKC[1]])
                            S.op("act", lambda e, Mn=Mn: e.copy(out=Mn[:], in_=BKC[:, 2:4, :]), reads=[RBKC[1]],
                                 writes=[RMn])
                            if i < 6:
                                MnT, RMnT = tmp(f"MT{i % 2}_{j}", BF16, [128, 2, 128])
                                for h in range(2):
                                    S.op("pe", lambda e, h=h, Mc=Mc, McT=McT: e.matmul(
                                        BKC[:, h, :], lhsT=Mc[:, h, :], rhs=McT[:, h, :], start=True, stop=True),
                                        reads=[RMc, RMcT], writes=[RBKC[0]])
                                S.op("act", lambda e, MnT=MnT: e.copy(out=MnT[:], in_=BKC[:, 0:2, :]), reads=[RBKC[0]],
                                     writes=[RMnT])
                            qs, Rqs = slot()
                            qsv = qs.rearrange("p (a t) -> p a t", t=128)
                            for h in range(2):
                                S.op("pe", lambda e, h=h, Mn=Mn, Q=Q, qsv=qsv: e.matmul(
                                    qsv[:, h, :], lhsT=Mn[:, h, :], rhs=Q[:, h, :], start=True, stop=True),
                                    reads=[RMn, RQ], writes=[Rqs])
                            S.op("dve", lambda e, Q=Q, qsv=qsv: e.tensor_tensor(out=Q[:], in0=qsv, in1=Q[:], op=ALU.add),
                                 reads=[Rqs, RQ], writes=[RQ])
                            Mc, RMc = Mn, RMn
                            if i < 6:
                                McT, RMcT = MnT[:], RMnT
                        if SL < 3:
                            continue
                        Xb, RXb = tmp(f"Xb_{j}", BF16, [128, 2, 64])
                        Un, RUn = tmp(f"Un_{j}", BF16, [128, 2, 64])
                        for h in range(2):
                            po = h * 64
                            S.op("pe", lambda e, h=h, po=po, art=art, Hh=Hh, c=c: e.matmul(
                                BKD[:, h * 64:(h + 1) * 64], lhsT=art[:, c, 0, :], rhs=Hh[:, h, :],
                                start=True, stop=False), reads=[Rart, RHh], writes=[RX])
                            S.op("pe", lambda e, h=h, po=po, NK=NK, tm=tm, j=j: e.matmul(
                                BKD[:, h * 64:(h + 1) * 64], lhsT=NK[:, h, 0:128], rhs=tm[:, 2, j, po:po + 64],
                                start=False, stop=True), reads=[RNK, Rtm], writes=[RX])
                        S.op("act", lambda e, Xb=Xb: e.copy(out=Xb[:].rearrange("p a v -> p (a v)"), in_=BKD[:, 0:128]),
                             reads=[RX], writes=[RXb])
                        for h in range(2):
                            S.op("pe", lambda e, h=h, Q=Q, Xb=Xb: e.matmul(
                                BKD[:, 128 + h * 64:128 + (h + 1) * 64], lhsT=Q[:, h, :], rhs=Xb[:, h, :], start=True,
                                stop=True), reads=[RQ, RXb], writes=[RU])
                        S.op("act", lambda e, Un=Un: e.mul(out=Un[:].rearrange("p a v -> p (a v)"), in_=BKD[:, 128:256],
                                                           mul=-1.0), reads=[RU], writes=[RUn])
                        if SL < 4:
                            continue
                        for h in range(2):
                            po = h * 64
                            hd = 2 * j + h
                            S.op("pe", lambda e, h=h, po=po, hd=hd, art=art, Hh=Hh, c=c: e.matmul(
                                YT[:, hd, :], lhsT=art[:, c, 1, :], rhs=Hh[:, h, :], start=True, stop=False),
                                reads=[Rart, RHh], writes=[RYT])
                            S.op("pe", lambda e, h=h, po=po, hd=hd, NK=NK, tm=tm, j=j: e.matmul(
                                YT[:, hd, :], lhsT=NK[:, h, 128:256], rhs=tm[:, 2, j, po:po + 64], start=False, stop=False),
                                reads=[RNK, Rtm], writes=[RYT])
                            S.op("pe", lambda e, h=h, hd=hd, NB=NB, Un=Un: e.matmul(
                                YT[:, hd, :], lhsT=NB[:, h, 128:256], rhs=Un[:, h, :], start=False, stop=True),
                                reads=[RNB, RUn], writes=[RYT])
                        if SL < 5:
                            continue
                        for h in range(2):
                            po = h * 64
                            S.op("pe", lambda e, h=h, po=po, tm=tm, j=j: e.matmul(
                                BKD[:, 256 + h * 64:256 + (h + 1) * 64], lhsT=tm[:, 0, j, :], rhs=tm[:, 2, j, po:po + 64],
                                start=True, stop=False), reads=[Rtm], writes=[RdH])
                            S.op("pe", lambda e, h=h, tm=tm, j=j, Un=Un: e.matmul(
                                BKD[:, 256 + h * 64:256 + (h + 1) * 64], lhsT=tm[:, 1, j, :], rhs=Un[:, h, :], start=False,
                                stop=True), reads=[Rtm, RUn], writes=[RdH])
                        for h in range(2):
                            po = h * 64
                            S.op("dve", lambda e, h=h, po=po, Hf=Hf: e.tensor_tensor(
                                out=Hf[po:po + 64, :], in0=BKD[po:po + 64, 256 + h * 64:256 + (h + 1) * 64],
                                in1=Hf[po:po + 64, :], op=ALU.add), reads=[RdH, RHf], writes=[RHf])
                        S.op("dve", lambda e, Hf=Hf, j=j, c=c: e.tensor_scalar(out=Hf[:], in0=Hf[:], scalar1=WL[:, j, c:c + 1],
                                                                               scalar2=None, op0=ALU.mult),
                             reads=[RHf, RWL], writes=[RHf])
                        for h in range(2):
                            S.op("pool", lambda e, h=h, Hf=Hf, Hh=Hh: e.tensor_scalar(
                                out=Hh[:, h, :], in0=Hf[:], scalar1=hmk[:, h:h + 1], scalar2=None, op0=ALU.mult),
                                reads=[RHf, Rhmk], writes=[RHh])

                    if LV < 5:
                        continue
                    ysb, Rysb = tmp("ysb", F32, [128, 8, 64])
                    st8, Rst8 = tmp("st8", F32, [128, 16])
                    S.op("act", lambda e: e.copy(out=ysb[:], in_=YT[:]), reads=[RYT], writes=[Rysb])
                    S.op("dve", lambda e: e.tensor_reduce(out=st8[:, 0:8], in_=ysb[:], axis=AX.X, op=ALU.add), reads=[Rysb],
                         writes=[Rst8])
                    S.op("dve", lambda e: e.tensor_scalar(out=st8[:, 0:8], in0=st8[:, 0:8], scalar1=1.0 / 64, scalar2=None,
                                                          op0=ALU.mult), reads=[Rst8], writes=[Rst8])
                    S.op("dve", lambda e: e.tensor_tensor(out=ysb[:], in0=ysb[:],
                                                          in1=st8[:, 0:8].unsqueeze(2).to_broadcast([128, 8, 64]),
                                                          op=ALU.subtract), reads=[Rysb, Rst8], writes=[Rysb])
                    ysq, Rysq = tmp("ysq", F32, [128, 8, 64])
                    S.op("pool", lambda e: e.tensor_tensor(out=ysq[:], in0=ysb[:], in1=ysb[:], op=ALU.mult), reads=[Rysb],
                         writes=[Rysq])
                    S.op("dve", lambda e: e.tensor_reduce(out=st8[:, 8:16], in_=ysq[:], axis=AX.X, op=ALU.add), reads=[Rysq],
                         writes=[Rst8])
                    sv, Rsv = tmp("sv", F32, [128, 8])
                    S.op("dve", lambda e: e.tensor_copy(out=sv[:], in_=st8[:, 8:16]), reads=[Rst8], writes=[Rsv])
                    rstd_ops(S, sv, Rsv, n=64, eps=64e-5)
                    S.op("dve", lambda e: e.tensor_tensor(out=ysb[:], in0=ysb[:],
                                                          in1=sv[:].unsqueeze(2).to_broadcast([128, 8, 64]), op=ALU.mult),
                         reads=[Rysb, Rsv], writes=[Rysb])
                    yf = ysb[:].rearrange("p h v -> p (h v)")
                    S.op("dve", lambda e: e.tensor_tensor(out=yf, in0=yf, in1=lnw[:, 0, :], op=ALU.mult), reads=[Rysb, Rlnw],
                         writes=[Rysb])
                    S.op("pool", lambda e: e.tensor_tensor(out=yf, in0=yf, in1=lnw[:, 1, :], op=ALU.add), reads=[Rysb, Rlnw],
                         writes=[Rysb])
                    bv, Rbv = tmp("bv", F32, [128, 8, 64])
                    S.op("pool", lambda e, tm=tm, c=c: e.tensor_tensor(
                        out=bv[:], in0=tm[:, 2, :, :].rearrange("p j (h v) -> p (j h) v", v=64),
                        in1=bons[:, c, :].unsqueeze(2).to_broadcast([128, 8, 64]), op=ALU.mult), reads=[Rtm, Rbons],
                        writes=[Rbv])
                    S.op("dve", lambda e: e.tensor_tensor(out=ysb[:], in0=ysb[:], in1=bv[:], op=ALU.add), reads=[Rysb, Rbv],
                         writes=[Rysb])
                    S.op("pe", lambda e, c=c: e.matmul(GP[:], lhsT=sgd[:, c * 128:(c + 1) * 128], rhs=wgu[:], start=True,
                                                       stop=True), reads=[Rsgd, Rwgu], writes=[RGP])
                    rwb, Rrwb = tmp("rwb", BF16, [128, 512])
                    S.op("dve", lambda e: e.tensor_tensor(out=rwb[:], in0=GP[:], in1=yf, op=ALU.mult), reads=[RGP, Rysb],
                         writes=[Rrwb])
                    for jj in range(4):
                        S.op("pe", lambda e, jj=jj: e.transpose(pT[:, 4 + jj, :], rwb[:, jj * 128:(jj + 1) * 128], idb[:]),
                             reads=[Rrwb, Ridb], writes=[RpT])
                    S.op("act", lambda e, c=c: e.copy(out=mixT[:, 0:4, c * 128:(c + 1) * 128], in_=pT[:, 4:8, :]),
                         reads=[RpT], writes=[RmixT])

                if LV < 6:
                    continue
                for jt in range(4):
                    lx, Rlx = LX[jt]
                    pp, Rpp = proj(14 + jt)
                    S.op("act", lambda e, lx=lx, pp=pp: e.copy(out=lx[:, 3:TL + 3], in_=pp), reads=[Rpp], writes=[Rlx])
                    xc, Rxc = tmp("ee")
                    cw = P_CW + 4 * jt
                    S.op("dve", lambda e, lx=lx, cw=cw, jt=jt: e.tensor_scalar(
                        out=xc[:], in0=lx[:, 3:TL + 3], scalar1=P0[:, cw + 3:cw + 4], scalar2=P0[:, P_CB + jt:P_CB + jt + 1],
                        op0=ALU.mult, op1=ALU.add), reads=[Rlx, RP0], writes=[Rxc])
                    for i in range(3):
                        S.op("dve", lambda e, lx=lx, cw=cw, i=i: e.scalar_tensor_tensor(
                            out=xc[:], in0=lx[:, i:TL + i], scalar=P0[:, cw + i:cw + i + 1], in1=xc[:], op0=ALU.mult,
                            op1=ALU.add), reads=[Rlx, RP0, Rxc], writes=[Rxc])
                    S.op("act", lambda e, lx=lx: e.copy(out=lx[:, 0:3], in_=lx[:, TL:TL + 3]), reads=[Rlx], writes=[Rlx])
                    xcb, Rxcb = tmp("kk2", BF16)
                    S.op("act", lambda e: e.copy(out=xcb[:], in_=xc[:]), reads=[Rxc], writes=[Rxcb])
                    zr, Rzr = slot()
                    S.op("pe", lambda e, zr=zr, jt=jt: e.matmul(zr, lhsT=gbd[:, jt, :], rhs=xcb[:], start=True, stop=True),
                         reads=[Rgbd, Rxcb], writes=[Rzr])
                    zi, Rzi = slot()
                    S.op("pe", lambda e, zi=zi, jt=jt: e.matmul(zi, lhsT=gbd[:, 4 + jt, :], rhs=xcb[:], start=True,
                                                                stop=True), reads=[Rgbd, Rxcb], writes=[Rzi])
                    rg, Rrg = tmp("aa")
                    ig, Rig = tmp("cs")
                    S.op("act", lambda e, zr=zr, jt=jt: e.activation(out=rg[:], in_=zr, func=AF.Sigmoid,
                                                                    bias=P0[:, P_BRG + jt:P_BRG + jt + 1]),
                         reads=[Rzr, RP0], writes=[Rrg])
                    S.op("act", lambda e, zi=zi, jt=jt: e.activation(out=ig[:], in_=zi, func=AF.Sigmoid,
                                                                    bias=P0[:, P_BIG + jt:P_BIG + jt + 1]),
                         reads=[Rzi, RP0], writes=[Rig])
                    th, Rth = tmp("csm")
                    a2, Ra2 = tmp("einv")
                    at_, Rat_ = tmp("edec")
                    S.op("act", lambda e, jt=jt: e.activation(out=th[:], in_=rg[:], func=AF.Tanh,
                                                             scale=spt[:, 4 + jt:5 + jt]), reads=[Rrg, Rspt], writes=[Rth])
                    S.op("act", lambda e, jt=jt: e.activation(out=a2[:], in_=rg[:], func=AF.Exp,
                                                             scale=spt[:, 12 + jt:13 + jt]), reads=[Rrg, Rspt], writes=[Ra2])
                    S.op("act", lambda e, jt=jt: e.activation(out=at_[:], in_=rg[:], func=AF.Exp,
                                                             scale=spt[:, 8 + jt:9 + jt]), reads=[Rrg, Rspt], writes=[Rat_])
                    S.op("dve", lambda e: e.scalar_tensor_tensor(out=a2[:], in0=a2[:], scalar=1.0, in1=th[:], op0=ALU.add,
                                                                 op1=ALU.mult), reads=[Ra2, Rth], writes=[Ra2])
                    S.op("act", lambda e: e.activation(out=a2[:], in_=a2[:], func=AF.Sqrt), reads=[Ra2], writes=[Ra2])
                    S.op("pool", lambda e: e.tensor_tensor(out=xc[:], in0=xc[:], in1=ig[:], op=ALU.mult), reads=[Rxc, Rig],
                         writes=[Rxc])
                    S.op("dve", lambda e: e.tensor_tensor(out=xc[:], in0=xc[:], in1=a2[:], op=ALU.mult), reads=[Rxc, Ra2],
                         writes=[Rxc])
                    hs, Rhs = tmp("eprev")
                    S.op("dve", lambda e, jt=jt: e.tensor_tensor_scan(out=hs[:], data0=at_[:], data1=xc[:],
                                                                      initial=hprev[:, jt:jt + 1], op0=ALU.mult,
                                                                      op1=ALU.add), reads=[Rat_, Rxc, Rhprev], writes=[Rhs])
                    S.op("dve", lambda e, jt=jt: e.tensor_copy(out=hprev[:, jt:jt + 1], in_=hs[:, TL - 1:TL]), reads=[Rhs],
                         writes=[Rhprev])
                    pg, Rpg = proj(18 + jt)
                    lg, Rlg = tmp("kk")
                    sqg, Rsqg = tmp("rn")
                    S.op("act", lambda e, pg=pg: e.copy(out=lg[:], in_=pg), reads=[Rpg], writes=[Rlg])
                    S.op("act", lambda e, pg=pg: e.activation(out=sqg[:], in_=pg, func=AF.Square), reads=[Rpg], writes=[Rsqg])
                    S.op("dve", lambda e: e.tensor_scalar(out=sqg[:], in0=sqg[:], scalar1=0.044715, scalar2=1.0, op0=ALU.mult,
                                                          op1=ALU.add), reads=[Rsqg], writes=[Rsqg])
                    S.op("pool", lambda e: e.tensor_tensor(out=sqg[:], in0=sqg[:], in1=lg[:], op=ALU.mult), reads=[Rsqg, Rlg],
                         writes=[Rsqg])
                    S.op("act", lambda e: e.activation(out=sqg[:], in_=sqg[:], func=AF.Sigmoid, scale=1.5957691216),
                         reads=[Rsqg], writes=[Rsqg])
                    S.op("pool", lambda e: e.tensor_tensor(out=sqg[:], in0=sqg[:], in1=lg[:], op=ALU.mult), reads=[Rsqg, Rlg],
                         writes=[Rsqg])
                    S.op("dve", lambda e, jt=jt: e.tensor_tensor(out=mixT[:, 4 + jt, :], in0=sqg[:], in1=hs[:], op=ALU.mult),
                         reads=[Rsqg, Rhs], writes=[RmixT])

                if LV < 7:
                    continue
                n = 0
                for s in range(NCH):
                    for dh in range(2):
                        po_, Rpo_ = slot()
                        pov = po_
                        for kc in range(8):
                            S.op("pe", lambda e, kc=kc, s=s, dh=dh: e.matmul(
                                GP[:], lhsT=mixT[:, kc, s * 128:(s + 1) * 128], rhs=wo[:, kc, dh * 512:(dh + 1) * 512],
                                start=(kc == 0), stop=(kc == 7)), reads=[RmixT, Rwo[kc]], writes=[RGP])
                        S.op("dve", lambda e, s=s, dh=dh, xt=xt: e.tensor_tensor(
                            out=xt[:, s, dh * 512:(dh + 1) * 512], in0=GP[:], in1=xt[:, s, dh * 512:(dh + 1) * 512],
                            op=ALU.add), reads=[RGP, Rxt], writes=[Rxt])
                S.dma("pool", lambda e, xt=xt, a=base + t0: e.dma_start(
                    out=dst[a:a + TL, :].rearrange("(s p) d -> p s d", p=128), in_=xt[:]), reads=[Rxt], writes=[])
        if DBG.get("verbose"):
            print("L0 sbuf bytes remaining", nc.sbuf_bytes_remaining, "ops", S.n_ops, {e: len(v) for e, v in S.prog.items()})
        S.flush()


def l0_host(inputs):
    f = lambda a: np.asarray(a, np.float32)
    p0 = np.zeros((128, NP0), np.float32)
    p0[:, P_MU:P_MU + 14] = fm(inputs["mu_shift"][0])
    p0[:, P_W0:P_W0 + 4] = fm(inputs["w_decay0"][0])
    p0[:, P_AB:P_AB + 4] = fm(inputs["a_bias"][0])
    p0[:, P_KK:P_KK + 4] = fm(inputs["k_k"][0])
    p0[:, P_KA:P_KA + 4] = fm(inputs["k_a"][0])
    p0[:, P_RK:P_RK + 4] = fm(f(inputs["r_k"][0]).reshape(-1))
    p0[:, P_CB:P_CB + 4] = fm(inputs["conv_b"][0])
    p0[:, P_BRG:P_BRG + 4] = fm(f(inputs["b_rgate"][0]).reshape(-1))
    p0[:, P_BIG:P_BIG + 4] = fm(f(inputs["b_igate"][0]).reshape(-1))
    p0[:, P_LAM:P_LAM + 4] = fm(inputs["lru_lambda"][0])
    cw = f(inputs["conv_w"][0])
    for jt in range(4):
        p0[:, P_CW + 4 * jt:P_CW + 4 * jt + 4] = cw[:, jt * 128:(jt + 1) * 128].T
    p0[:, P_G:P_G + 8] = fm(inputs["norm_mix"][0])
    lora = np.concatenate([f(inputs["w_decay_up"][0]), f(inputs["w_a_up"][0])], axis=0)
    gbd = np.zeros((128, 8, 128), np.float32)
    for gi, w in enumerate((f(inputs["w_rgate"][0]), f(inputs["w_igate"][0]))):
        for jt in range(4):
            gbd[0:64, gi * 4 + jt, 0:64] = w[2 * jt]
            gbd[64:128, gi * 4 + jt, 64:128] = w[2 * jt + 1]
    lnx = np.stack([f(inputs["lnx_w"][0]), f(inputs["lnx_b"][0])], axis=0)
    i = np.arange(128)
    low = (i[:, None] > i[None, :]).astype(np.float32)
    up_s = (i[:, None] < i[None, :]).astype(np.float32)
    up_i = (i[:, None] <= i[None, :]).astype(np.float32)
    bd = np.zeros((128, 128), np.float32)
    bd[:64, :64] = 1.0
    bd[64:, 64:] = 1.0
    ind = np.zeros((128, 2), np.float32)
    ind[:64, 0] = 1.0
    ind[64:, 1] = 1.0
    masks = np.concatenate([low, -up_s, up_i, up_s, up_i, bd, ind], axis=1)
    return {"w_in0": np.ascontiguousarray(f(inputs["w_in0"][0])), "w_out0": np.ascontiguousarray(f(inputs["w_out0"][0])),
            "p0": p0, "lora0": np.ascontiguousarray(lora), "w_g_up0": np.ascontiguousarray(f(inputs["w_g_up"][0])),
            "gates_bd0": gbd, "lnx0": np.ascontiguousarray(lnx), "c_masks0": np.ascontiguousarray(masks)}
```

```python
import numpy as np
from contextlib import ExitStack
import concourse.bass as bass
import concourse.mybir as mybir
from concourse.bass_utils import run_bass_kernel_spmd

F32 = mybir.dt.float32
BF16 = mybir.dt.bfloat16
AF = mybir.ActivationFunctionType
ALU = mybir.AluOpType
AX = mybir.AxisListType


class Res:
    __slots__ = ("name", "w", "readers", "bank")

    def __init__(self, name="", bank=None):
        self.name = name
        self.w = None
        self.readers = {}
        self.bank = bank


class _Rec:
    def __init__(self):
        self.call = None

    def __getattr__(self, name):
        def f(*a, **k):
            self.call = (name, a, k)
            return self
        return f


def _eager(fn):
    rec = _Rec()
    fn(rec)
    name, a, k = rec.call
    return lambda e: getattr(e, name)(*a, **k)


class Sched:
    COMPUTE = ("pe", "act", "dve", "pool")
    NDSEM = 6

    def __init__(self, nc, stack, dma_queues=("sp", "pool")):
        self.nc = nc
        self.sems = {}
        self.ecnt = {}
        self.prog = {e: [] for e in ("pe", "act", "dve", "pool", "sp")}
        self.known = {e: {} for e in self.prog}
        for e in self.COMPUTE:
            self.sems["e_" + e] = stack.enter_context(nc.semaphore("e_" + e))
            self.ecnt[e] = 0
        self.dq = {}
        for q in dma_queues:
            keys = []
            for i in range(self.NDSEM):
                k = f"d_{q}{i}"
                self.sems[k] = stack.enter_context(nc.semaphore(k))
                keys.append(k)
            self.dq[q] = {"keys": keys, "cnt": [0] * self.NDSEM, "next": 0}
        self.n_ops = 0

    def res(self, name="", bank=None):
        return Res(name, bank)

    def _deps(self, eng, is_dma, reads, writes):
        deps = {}

        def add(t, raw):
            key, val, peng, pdma = t
            if not pdma and peng == eng and not is_dma:
                if not raw or eng == "pe":
                    return
            if deps.get(key, 0) < val:
                deps[key] = val

        for r in reads:
            if r.w is not None:
                add(r.w, True)
        for r in list(reads) + list(writes):
            if r.bank is not None:
                for key, (val, peng, pdma) in r.bank.readers.items():
                    if peng != eng:
                        add((key, val, peng, pdma), False)
        for w in writes:
            if w.w is not None:
                add(w.w, False)
            for key, (val, peng, pdma) in w.readers.items():
                add((key, val, peng, pdma), False)
        kn = self.known[eng]
        out = []
        for key, val in deps.items():
            if kn.get(key, 0) < val:
                kn[key] = val
                out.append((key, val))
        return out

    def _commit(self, tick, reads, writes):
        key, val, eng, is_dma = tick
        for w in writes:
            w.w = tick
            w.readers = {}
        for r in list(reads) + list(writes):
            if r.bank is not None:
                r.bank.readers[key] = (val, eng, is_dma)
        for r in reads:
            if r in writes:
                continue
            r.readers[key] = (val, eng, is_dma)

    def op(self, eng, fn, reads=(), writes=()):
        reads = [r for r in reads if r is not None]
        writes = [w for w in writes if w is not None]
        waits = self._deps(eng, False, reads, writes)
        self.ecnt[eng] += 1
        tick = ("e_" + eng, self.ecnt[eng], eng, False)
        self.prog[eng].append((waits, _eager(fn), ("e_" + eng, 1)))
        self._commit(tick, reads, writes)
        self.n_ops += 1

    def dma(self, q, fn, reads=(), writes=()):
        reads = [r for r in reads if r is not None]
        writes = [w for w in writes if w is not None]
        waits = self._deps(q, True, reads, writes)
        d = self.dq[q]
        slot = d["next"] % self.NDSEM
        d["next"] += 1
        key = d["keys"][slot]
        if d["cnt"][slot] > 0 and self.known[q].get(key, 0) < d["cnt"][slot]:
            self.known[q][key] = d["cnt"][slot]
            waits.append((key, d["cnt"][slot]))
        d["cnt"][slot] += 16
        tick = (key, d["cnt"][slot], q, True)
        self.prog[q].append((waits, _eager(fn), (key, 16)))
        self._commit(tick, reads, writes)
        self.n_ops += 1

    def barrier(self):
        allv = {}
        for e in self.COMPUTE:
            if self.ecnt[e] > 0:
                allv["e_" + e] = self.ecnt[e]
        for q, d in self.dq.items():
            for k, c in zip(d["keys"], d["cnt"]):
                if c > 0:
                    allv[k] = c
        for e in self.prog:
            waits = []
            for k, v in allv.items():
                if self.known[e].get(k, 0) < v:
                    self.known[e][k] = v
                    waits.append((k, v))
            if waits:
                self.prog[e].append((waits, None, None))

    def flush(self, final=False):
        nc = self.nc
        sems = self.sems
        fin = {}
        if final:
            for q, d in self.dq.items():
                for k, c in zip(d["keys"], d["cnt"]):
                    if c > 0:
                        fin[k] = c

        def run(eng_obj, items, extra=None):
            for waits, fn, inc in items:
                for key, val in waits:
                    eng_obj.wait_ge(sems[key], val)
                if fn is not None:
                    ins = fn(eng_obj)
                    ins.then_inc(sems[inc[0]], inc[1])
            if extra:
                for key, val in extra.items():
                    eng_obj.wait_ge(sems[key], val)

        prog = self.prog
        with nc.Block() as block:
            @block.tensor
            def _(e):
                run(e, prog["pe"])

            @block.scalar
            def _(e):
                run(e, prog["act"])

            @block.vector
            def _(e):
                run(e, prog["dve"])

            @block.gpsimd
            def _(e):
                run(e, prog["pool"])

            @block.sync
            def _(e):
                run(e, prog["sp"], extra=fin)
        self.prog = {e: [] for e in prog}


D = 1024
SEQ = 2048
NSEQ = 2
NTOK = NSEQ * SEQ
FF = 2816
EPS = 1e-6


class Ctx:
    _phase = [0]

    def __init__(self, nc, S, st):
        self.nc, self.S, self.st = nc, S, st
        self.n = 0
        Ctx._phase[0] += 1
        self.pfx = f"ph{Ctx._phase[0]}_"

    def sb(self, shape, dt, name=None):
        self.n += 1
        t = self.st.enter_context(self.nc.sbuf_tensor(self.pfx + (name or f"t{self.n}"), list(shape), dt))
        return t, self.S.res(name or f"t{self.n}")

    def ps(self, shape, dt, name=None):
        self.n += 1
        t = self.st.enter_context(self.nc.psum_tensor(self.pfx + (name or f"p{self.n}"), list(shape), dt))
        r = self.S.res(name or f"p{self.n}")
        r.bank = Res("bank")
        return t, r


def load_weight(S, q, dst, src, nk, res_list):
    for k in range(nk):
        S.dma(q, lambda e, k=k: e.dma_start(out=dst[:, k, :], in_=src[k * 128:(k + 1) * 128, :]),
              writes=[res_list[k]])


def rstd_ops(S, ss, Rss, n=D, eps=EPS):
    S.op("dve", lambda e: e.tensor_scalar(out=ss[:], in0=ss[:], scalar1=1.0 / n, scalar2=eps,
                                          op0=ALU.mult, op1=ALU.add), reads=[Rss], writes=[Rss])
    S.op("act", lambda e: e.activation(out=ss[:], in_=ss[:], func=AF.Sqrt), reads=[Rss], writes=[Rss])
    S.op("dve", lambda e: e.reciprocal(out=ss[:], in_=ss[:]), reads=[Rss], writes=[Rss])


def norm_T(S, xt, Rxt, gain, Rg, hT, RhT, tmp):
    junk, Rjunk, ss, Rss, xn, Rxn, pT, RpT, ident, Rid = tmp
    S.op("act", lambda e: e.activation(out=junk[:], in_=xt, func=AF.Square, accum_out=ss[:]),
         reads=[Rxt], writes=[Rjunk, Rss])
    rstd_ops(S, ss, Rss)
    S.op("act", lambda e: e.activation(out=xn[:], in_=xt, func=AF.Copy, scale=ss[:, 0:1]),
         reads=[Rxt, Rss], writes=[Rxn])
    for k in range(8):
        S.op("pe", lambda e, k=k: e.transpose(pT[:, k, :], xn[:, k * 128:(k + 1) * 128], ident[:]),
             reads=[Rxn, Rid], writes=[RpT])
    S.op("dve", lambda e: e.tensor_tensor(out=hT, in0=pT[:], in1=gain.unsqueeze(2).to_broadcast([128, 8, 128]),
                                          op=ALU.mult), reads=[RpT, Rg], writes=[RhT])


def make_ident(S, C, dt=BF16):
    idf, Ridf = C.sb([128, 128], F32)
    S.op("pool", lambda e: e.memset(idf[:], 0.0), writes=[Ridf])
    S.op("pool", lambda e: e.affine_select(out=idf[:], in_=idf[:], pattern=[[-1, 128]], compare_op=ALU.not_equal,
                                           fill=1.0, base=0, channel_multiplier=1), reads=[Ridf], writes=[Ridf])
    idb, Ridb = C.sb([128, 128], BF16)
    S.op("dve", lambda e: e.tensor_copy(out=idb[:], in_=idf[:]), reads=[Ridf], writes=[Ridb])
    return idf, Ridf, idb, Ridb


def phase_ffn(nc, S, src, dst, Dm, l, final):
    with ExitStack() as st:
        C = Ctx(nc, S, st)
        S.barrier()
        wg, _ = C.sb([128, 8, FF], BF16, "wg")
        wu, _ = C.sb([128, 8, FF], BF16, "wu")
        wd, _ = C.sb([128, 22, D], BF16, "wd")
        Rwg = [S.res() for _ in range(8)]
        Rwu = [S.res() for _ in range(8)]
        Rwd = [S.res() for _ in range(22)]
        gain, Rg = C.sb([128, 8], F32, "gain")
        S.dma("sp", lambda e: e.dma_start(out=gain[:], in_=Dm[f"g_ffn{l}"]), writes=[Rg])
        load_weight(S, "pool", wg, Dm[f"ffn_gate{l}"], 8, Rwg)
        load_weight(S, "pool", wu, Dm[f"ffn_up{l}"], 8, Rwu)
        load_weight(S, "pool", wd, Dm[f"ffn_down{l}"], 22, Rwd)
        if final:
            gfin, Rgfin = C.sb([128, D], F32, "gfin")
            S.dma("sp", lambda e: e.dma_start(out=gfin[:], in_=Dm["g_final"][0, :].partition_broadcast(128)),
                  writes=[Rgfin])
        idf, Ridf, idb, Ridb = make_ident(S, C)
        TT = 256
        xts = [C.sb([128, 2, D], F32) for _ in range(2)]
        hTs = [C.sb([128, 8, TT], BF16) for _ in range(2)]
        actT, RactT = C.sb([128, 22, TT], BF16, "actT")
        junk, Rjunk = C.sb([128, D], BF16, "junk")
        ss, Rss = C.sb([128, 1], F32, "ss")
        xn, Rxn = C.sb([128, D], BF16, "xn")
        sgs = [C.sb([128, TT], F32) for _ in range(2)]
        pTs = [C.ps([128, 8, 128], BF16) for _ in range(2)]
        pgu = [C.ps([128, 2, TT], F32) for _ in range(2)]
        pso = [C.ps([128, 512], F32) for _ in range(2)]
        npt = 0
        for it in range(NTOK // TT):
            tok0 = it * TT
            xt, Rxt = xts[it % 2]
            hT, RhT = hTs[it % 2]
            S.dma("sp", lambda e, xt=xt, tok0=tok0: e.dma_start(
                out=xt[:], in_=src[tok0:tok0 + TT, :].rearrange("(s p) d -> p s d", p=128)), writes=[Rxt])
            for s in range(2):
                pT, RpT = pTs[npt % 2]
                npt += 1
                norm_T(S, xt[:, s, :], Rxt, gain[:], Rg, hT[:, :, s * 128:(s + 1) * 128], RhT,
                       (junk, Rjunk, ss, Rss, xn, Rxn, pT, RpT, idb, Ridb))
            for j in range(22):
                pg, Rpg = pgu[j % 2]
                sg, Rsg = sgs[j % 2]
                for k in range(8):
                    S.op("pe", lambda e, k=k, j=j, pg=pg, hT=hT: e.matmul(
                        pg[:, 0, :], lhsT=wg[:, k, j * 128:(j + 1) * 128], rhs=hT[:, k, :], start=(k == 0), stop=(k == 7)),
                        reads=[Rwg[k], RhT], writes=[Rpg])
                for k in range(8):
                    S.op("pe", lambda e, k=k, j=j, pg=pg, hT=hT: e.matmul(
                        pg[:, 1, :], lhsT=wu[:, k, j * 128:(j + 1) * 128], rhs=hT[:, k, :], start=(k == 0), stop=(k == 7)),
                        reads=[Rwu[k], RhT], writes=[Rpg])
                S.op("act", lambda e, pg=pg, sg=sg: e.activation(out=sg[:], in_=pg[:, 0, :], func=AF.Silu),
                     reads=[Rpg], writes=[Rsg])
                S.op("dve", lambda e, pg=pg, sg=sg, j=j: e.tensor_tensor(out=actT[:, j, :], in0=pg[:, 1, :], in1=sg[:],
                                                                        op=ALU.mult),
                     reads=[Rpg, Rsg], writes=[RactT])
            n = 0
            for s in range(2):
                for dh in range(2):
                    po, Rpo = pso[n % 2]
                    n += 1
                    for j in range(22):
                        S.op("pe", lambda e, j=j, s=s, dh=dh, po=po: e.matmul(
                            po[:], lhsT=actT[:, j, s * 128:(s + 1) * 128], rhs=wd[:, j, dh * 512:(dh + 1) * 512],
                            start=(j == 0), stop=(j == 21)), reads=[RactT, Rwd[j]], writes=[Rpo])
                    S.op("dve", lambda e, s=s, dh=dh, po=po, xt=xt: e.tensor_tensor(
                        out=xt[:, s, dh * 512:(dh + 1) * 512], in0=po[:], in1=xt[:, s, dh * 512:(dh + 1) * 512],
                        op=ALU.add), reads=[Rpo, Rxt], writes=[Rxt])
            if final:
                for s in range(2):
                    S.op("act", lambda e, s=s, xt=xt: e.activation(out=junk[:], in_=xt[:, s, :], func=AF.Square,
                                                                   accum_out=ss[:]), reads=[Rxt], writes=[Rjunk, Rss])
                    rstd_ops(S, ss, Rss)
                    S.op("act", lambda e, s=s, xt=xt: e.activation(out=xt[:, s, :], in_=xt[:, s, :], func=AF.Copy,
                                                                   scale=ss[:, 0:1]), reads=[Rxt, Rss], writes=[Rxt])
                    S.op("dve", lambda e, s=s, xt=xt: e.tensor_tensor(out=xt[:, s, :], in0=xt[:, s, :], in1=gfin[:],
                                                                      op=ALU.mult), reads=[Rxt, Rgfin], writes=[Rxt])
            S.dma("pool", lambda e, xt=xt, tok0=tok0: e.dma_start(
                out=dst[tok0:tok0 + TT, :].rearrange("(s p) d -> p s d", p=128), in_=xt[:]), reads=[Rxt], writes=[])
        S.flush()


def fm(v):
    v = np.asarray(v, np.float32).reshape(-1, 128)
    return np.ascontiguousarray(v.T)


def build(phases):
    nc = bass.Bass("TRN2", target_bir_lowering=False)
    Dm = {}

    def inp(name, shape):
        Dm[name] = nc.dram_tensor(name, list(shape), F32, kind="ExternalInput").ap()

    inp("x", [NTOK, D])
    for l in range(2):
        if f"F{l}" in phases:
            inp(f"ffn_gate{l}", [D, FF]); inp(f"ffn_up{l}", [D, FF]); inp(f"ffn_down{l}", [FF, D])
            inp(f"g_ffn{l}", [128, 8])
    if "F1" in phases:
        inp("g_final", [1, D])
    if "L0" in phases:
        inp("w_in0", [D, 2816]); inp("w_out0", [D, D]); inp("p0", [128, NP0]); inp("lora0", [128, 512])
        inp("w_g_up0", [128, 512]); inp("gates_bd0", [128, 8, 128]); inp("lnx0", [2, 512]); inp("c_masks0", [128, 770])
    if "L1" in phases:
        inp("w1", [D, W1C]); inp("wo1", [64, 16, D]); inp("g_mix1", [128, 8])
        inp("c_cos", [128, SEQ]); inp("c_sin", [128, SEQ]); inp("c_pm", [128, 128])
        inp("c_negmask", [128, 128]); inp("c_pow2", [128, NIT + 1])
    out = nc.dram_tensor("out", [NTOK, D], F32, kind="ExternalOutput").ap()
    scr = [nc.dram_tensor(f"scr{i}", [NTOK, D], F32).ap() for i in range(2)]
    with ExitStack() as st:
        S = Sched(nc, st)
        cur = Dm["x"]
        for i, ph in enumerate(phases):
            dst = out if i == len(phases) - 1 else scr[i % 2]
            if ph in ("F0", "F1"):
                phase_ffn(nc, S, cur, dst, Dm, int(ph[1]), ph == "F1")
            elif ph == "L1":
                phase_dsa(nc, S, cur, dst, Dm)
            elif ph == "L0":
                phase_l0(nc, S, cur, dst, Dm)
            cur = dst
        S.barrier()
        S.prog["sp"].append(([], None, None))
        S.flush(final=True)
    return nc


def host_inputs(inputs, phases):
    x = np.asarray(inputs["x"], np.float32)
    shared = {}
    for l in range(2):
        if f"F{l}" in phases:
            shared[f"ffn_gate{l}"] = np.ascontiguousarray(inputs["ffn_gate"][l], dtype=np.float32)
            shared[f"ffn_up{l}"] = np.ascontiguousarray(inputs["ffn_up"][l], dtype=np.float32)
            shared[f"ffn_down{l}"] = np.ascontiguousarray(inputs["ffn_down"][l], dtype=np.float32)
            shared[f"g_ffn{l}"] = fm(inputs["norm_ffn"][l])
    if "F1" in phases:
        shared["g_final"] = np.asarray(inputs["norm_final"], np.float32).reshape(1, D)
    if "L0" in phases:
        shared.update(l0_host(inputs))
    if "L1" in phases:
        w = np.asarray(inputs["w_in1"][0], np.float32)
        q, k, v, iq, ik, iw = np.split(w, np.cumsum([1024, 64, 64, 512, 64])[:].tolist(), axis=1)
        shared["w1"] = np.ascontiguousarray(np.concatenate([q, iq, k, k, ik, ik, v, iw], axis=1))
        shared["wo1"] = np.ascontiguousarray(np.asarray(inputs["w_out1"][0], np.float32).reshape(16, 64, D).transpose(1, 0, 2))
        shared["g_mix1"] = fm(inputs["norm_mix"][1])
        shared.update(dsa_consts())
    maps = []
    for c in range(8):
        m = dict(shared)
        m["x"] = np.ascontiguousarray(x[c * NSEQ:(c + 1) * NSEQ].reshape(NTOK, D))
        maps.append(m)
    return maps


PHASES = ("L0", "F0", "L1", "F1")
DBG = {"nseq": NSEQ, "qbs": None, "d1_tiles": None, "l0_tiles": None, "cores": 8}


def kernel(phases=PHASES, **inputs):
    nc = build(phases)
    maps = host_inputs(inputs, phases)
    ncores = DBG["cores"]
    res = run_bass_kernel_spmd(nc, maps[:ncores], core_ids=list(range(ncores)))
    outs = [np.asarray(r["out"], np.float32).reshape(NSEQ, SEQ, D) for r in res.results]
    outs += [np.zeros((NSEQ, SEQ, D), np.float32)] * (8 - ncores)
    return np.concatenate(outs, axis=0)


W1C = 1864
NIT = 24
TOPK = 256


def phase_dsa(nc, S, src, dst, Dm):
    with ExitStack() as st:
        C = Ctx(nc, S, st)
        S.barrier()
        w1, _ = C.sb([128, 8, W1C], BF16, "w1")
        Rw1 = [S.res() for _ in range(8)]
        load_weight(S, "pool", w1, Dm["w1"], 8, Rw1)
        wo, Rwo = C.sb([64, 16, D], BF16, "wo")
        for hh_ in range(16):
            S.dma("pool", lambda e, hh_=hh_: e.dma_start(out=wo[:, hh_, :], in_=Dm["wo1"][:, hh_, :]), writes=[Rwo])
        gain, Rg = C.sb([128, 8], F32, "gain")
        S.dma("sp", lambda e: e.dma_start(out=gain[:], in_=Dm["g_mix1"]), writes=[Rg])
        pm_, Rpm = C.sb([128, 128], BF16, "Pm")
        S.dma("pool", lambda e: e.dma_start(out=pm_[:], in_=Dm["c_pm"]), writes=[Rpm])
        negm, Rnegm = C.sb([128, 128], F32, "negm")
        S.dma("sp", lambda e: e.dma_start(out=negm[:], in_=Dm["c_negmask"]), writes=[Rnegm])
        pow2, Rpow2 = C.sb([128, NIT + 1], F32, "pow2")
        S.dma("sp", lambda e: e.dma_start(out=pow2[:], in_=Dm["c_pow2"]), writes=[Rpow2])
        idf, Ridf, idb, Ridb = make_ident(S, C)
        onesf, Rones = C.sb([128, 64], F32, "onesf")
        S.op("pool", lambda e: e.memset(onesf[:], 1.0), writes=[Rones])
        thrc, Rthrc = C.sb([128, 1], F32, "thrc")
        S.op("pool", lambda e: e.memset(thrc[:], -1e29), writes=[Rthrc])

        qT, RqT = C.sb([128, 8, SEQ], BF16, "qT")
        iqT, RiqT = C.sb([128, 4, SEQ], BF16, "iqT")
        kT2, RkT2 = C.sb([128, SEQ], BF16, "kT2")
        ikT2, RikT2 = C.sb([128, SEQ], BF16, "ikT2")
        Vaug, RV = C.sb([128, 16, 65], BF16, "Vaug")
        iw, Riw = C.sb([128, 16, 8], F32, "iw")
        S.op("pool", lambda e: e.memset(Vaug[:], 1.0), writes=[RV])

        T1 = 256
        xts = [C.sb([128, 2, D], F32) for _ in range(1)]
        hT, RhT = C.sb([128, 8, T1], BF16, "hT")
        junk, Rjunk = C.sb([128, D], BF16, "junk")
        ss, Rss = C.sb([128, 1], F32, "ss")
        xn, Rxn = C.sb([128, D], BF16, "xn")
        pbs = [C.sb([128, T1], BF16) for _ in range(2)]
        t1s = [C.sb([128, T1], F32) for _ in range(2)]
        t2s = [C.sb([128, T1], F32) for _ in range(2)]
        css = [C.sb([128, 2, T1], F32) for _ in range(2)]
        score, Rscore = C.sb([128, SEQ], F32, "score")
        jcnt, Rjcnt = C.sb([128, SEQ], BF16, "jcnt")
        maskTs = [C.sb([128, 16, 128], BF16) for _ in range(2)]
        maskb, Rmaskb = C.sb([128, SEQ], BF16, "maskb")
        rls = [C.sb([128, 512], F32) for _ in range(2)]
        Es = [C.sb([128, 4, 128], BF16) for _ in range(2)]
        PTs = [C.sb([128, 4, 128], BF16) for _ in range(2)]
        rs, Rrs = C.sb([128, 512], F32, "rs")
        bc, Rbc = C.sb([64, 512], F32, "bc")
        attnT = [C.sb([64, 4, 128], BF16) for _ in range(4)]
        xres, Rxres = C.sb([128, D], F32, "xres")
        bis, Rbis = C.sb([128, 8], F32, "bis")
        steps, Rsteps = C.sb([128, NIT + 1], F32, "steps")
        cnts, Rcnts = C.sb([128, NIT], F32, "cnts")
        pT, RpT = C.ps([128, 8, 128], BF16, "pT")
        B = [C.ps([128, 4, 128], F32) for _ in range(7)]

        for sq in range(DBG["nseq"]):
            base = sq * SEQ
            for tt in range(DBG["d1_tiles"] or SEQ // T1):
                t0 = tt * T1
                xt, Rxt = xts[0]
                cs, Rcs = css[tt % 2]
                S.dma("sp", lambda e, xt=xt, a=base + t0: e.dma_start(
                    out=xt[:], in_=src[a:a + T1, :].rearrange("(s p) d -> p s d", p=128)), writes=[Rxt])
                S.dma("sp", lambda e, cs=cs, t0=t0: e.dma_start(out=cs[:, 0, :], in_=Dm["c_cos"][:, t0:t0 + T1]),
                      writes=[Rcs])
                S.dma("sp", lambda e, cs=cs, t0=t0: e.dma_start(out=cs[:, 1, :], in_=Dm["c_sin"][:, t0:t0 + T1]),
                      writes=[Rcs])
                for s in range(T1 // 128):
                    norm_T(S, xt[:, s, :], Rxt, gain[:], Rg, hT[:, :, s * 128:(s + 1) * 128], RhT,
                           (junk, Rjunk, ss, Rss, xn, Rxn, pT, RpT, idb, Ridb))
                for nt in range(DBG.get("d1_nt", 14)):
                    pp, Rpp = B[nt % 2]
                    pr, Rpr = B[2 + nt % 2]
                    pb, Rpb = pbs[nt % 2]
                    ta, Rta = t1s[nt % 2]
                    tb, Rtb = t2s[nt % 2]
                    ppv = pp[:].rearrange("p a b -> p (a b)")[:, 0:T1]
                    prv = pr[:].rearrange("p a b -> p (a b)")[:, 0:T1]
                    for k in range(8):
                        S.op("pe", lambda e, k=k, nt=nt, ppv=ppv: e.matmul(
                            ppv, lhsT=w1[:, k, nt * 128:(nt + 1) * 128], rhs=hT[:, k, :], start=(k == 0), stop=(k == 7)),
                            reads=[Rw1[k], RhT], writes=[Rpp])
                    lvl = DBG.get("d1_ops", 9)
                    if lvl < 2:
                        continue
                    S.op("act", lambda e, pb=pb, ppv=ppv: e.copy(out=pb[:], in_=ppv), reads=[Rpp], writes=[Rpb])
                    if lvl < 3:
                        continue
                    S.op("pe", lambda e, pb=pb, prv=prv: e.matmul(prv, lhsT=pm_[:], rhs=pb[:], start=True, stop=True),
                         reads=[Rpm, Rpb], writes=[Rpr])
                    if lvl < 4:
                        continue
                    if DBG.get("tt", 3) & 1:
                        S.op("dve", lambda e, ta=ta, ppv=ppv, cs=cs: e.tensor_tensor(out=ta[:], in0=ppv, in1=cs[:, 0, :],
                                                                                    op=ALU.mult),
                             reads=[Rpp, Rcs, Rpb], writes=[Rta])
                    if DBG.get("tt", 3) & 2:
                        S.op("dve", lambda e, tb=tb, prv=prv, cs=cs: e.tensor_tensor(out=tb[:], in0=prv, in1=cs[:, 1, :],
                                                                                    op=ALU.mult),
                             reads=[Rpr, Rcs], writes=[Rtb])
                    if DBG.get("tt", 3) & 4:
                        S.op("dve", lambda e, ta=ta, ppv=ppv, cs=cs: e.tensor_tensor(out=ta[:], in0=ppv, in1=ta[:],
                                                                                    op=ALU.mult),
                             reads=[Rpp, Rcs], writes=[Rta])
                    if lvl < 5:
                        continue
                    if nt < 8:
                        dest, Rd = qT[:, nt, t0:t0 + T1], RqT
                    elif nt < 12:
                        dest, Rd = iqT[:, nt - 8, t0:t0 + T1], RiqT
                    elif nt == 12:
                        dest, Rd = kT2[:, t0:t0 + T1], RkT2
                    else:
                        dest, Rd = ikT2[:, t0:t0 + T1], RikT2
                    S.op(DBG.get("addeng", "pool"), lambda e, dest=dest, ta=ta, tb=tb: e.tensor_tensor(out=dest, in0=ta[:], in1=tb[:],
                                                                                   op=ALU.add),
                         reads=[Rta, Rtb], writes=[Rd])
                for s in range(DBG.get("d1_v", T1 // 128)):
                    blk = t0 // 128 + s
                    pv, Rpv = B[4]
                    pvv = pv[:].rearrange("p a b -> p (a b)")[:, 0:72]
                    for k in range(8):
                        S.op("pe", lambda e, k=k, s=s, pvv=pvv: e.matmul(
                            pvv, lhsT=hT[:, k, s * 128:(s + 1) * 128], rhs=w1[:, k, 1792:1864], start=(k == 0),
                            stop=(k == 7)), reads=[Rw1[k], RhT], writes=[Rpv])
                    S.op("act", lambda e, blk=blk, pvv=pvv: e.copy(out=Vaug[:, blk, 0:64], in_=pvv[:, 0:64]),
                         reads=[Rpv], writes=[RV])
                    S.op("dve", lambda e, blk=blk, pvv=pvv: e.tensor_scalar(
                        out=iw[:, blk, :], in0=pvv[:, 64:72], scalar1=1.0 / (8.0 * 8.0 ** 0.5), scalar2=None,
                        op0=ALU.mult), reads=[Rpv], writes=[Riw])

            if DBG.get("dump_d1"):
                dd = [(qT[:, 0, 0:512], 0, 0, 512), (kT2[:, 0:512], 0, 512, 512), (iqT[:, 0, 0:512], 128, 0, 512),
                      (ikT2[:, 0:512], 128, 512, 512), (Vaug[:, 0:4, :].rearrange("p a b -> p (a b)"), 256, 0, 260),
                      (iw[:, 0:4, :].rearrange("p a b -> p (a b)"), 256, 512, 32)]
                for ap_, r0, c0_, w_ in dd:
                    S.dma("pool", lambda e, ap_=ap_, r0=r0, c0_=c0_, w_=w_: e.dma_start(
                        out=dst[r0:r0 + 128, c0_:c0_ + w_], in_=ap_), reads=[RqT, RkT2, RiqT, RikT2, RV, Riw], writes=[])
            def stage_index(qb):
                nk = (qb + 1) * 128
                q0 = qb * 128
                pi, Rpi = B[6]
                n = 0
                for h in range(8):
                    pr_ = (h % 2) * 64
                    for c0 in range(0, nk, 512):
                        w = min(512, nk - c0)
                        rl, Rrl = rls[n % 2]
                        n += 1
                        piv = pi[:].rearrange("p a b -> p (a b)")[:, 0:w]
                        S.op("pe", lambda e: e.matmul(
                            piv, lhsT=iqT[pr_:pr_ + 64, h // 2, q0:q0 + 128], rhs=ikT2[pr_:pr_ + 64, c0:c0 + w],
                            start=True, stop=True), reads=[RiqT, RikT2], writes=[Rpi])
                        S.op("act", lambda e: e.activation(out=rl[:, 0:w], in_=piv, func=AF.Relu),
                             reads=[Rpi], writes=[Rrl])
                        fe = DBG.get("fmaeng", "dve")
                        if h == 0:
                            S.op(fe, lambda e: e.tensor_scalar(
                                out=score[:, c0:c0 + w], in0=rl[:, 0:w], scalar1=iw[:, qb, 0:1], scalar2=None,
                                op0=ALU.mult), reads=[Rrl, Riw], writes=[Rscore])
                        else:
                            S.op(fe, lambda e: e.scalar_tensor_tensor(
                                out=score[:, c0:c0 + w], in0=rl[:, 0:w], scalar=iw[:, qb, h:h + 1],
                                in1=score[:, c0:c0 + w], op0=ALU.mult, op1=ALU.add), reads=[Rrl, Riw, Rscore],
                                writes=[Rscore])
                S.op("dve", lambda e: e.tensor_tensor(out=score[:, q0:nk], in0=score[:, q0:nk], in1=negm[:],
                                                      op=ALU.add), reads=[Rscore, Rnegm], writes=[Rscore])
                if qb >= 2:
                    S.op("dve", lambda e: e.tensor_reduce(out=bis[:, 0:1], in_=score[:, 0:nk], axis=AX.X, op=ALU.max),
                         reads=[Rscore], writes=[Rbis])
                    S.op("dve", lambda e: e.tensor_reduce(out=bis[:, 1:2], in_=score[:, 0:nk - 128], axis=AX.X,
                                                          op=ALU.min), reads=[Rscore], writes=[Rbis])
                    S.op("dve", lambda e: e.tensor_tensor(out=bis[:, 2:3], in0=bis[:, 0:1], in1=bis[:, 1:2],
                                                          op=ALU.subtract), reads=[Rbis], writes=[Rbis])
                    S.op("dve", lambda e: e.tensor_scalar(out=steps[:], in0=pow2[:], scalar1=bis[:, 2:3], scalar2=None,
                                                          op0=ALU.mult), reads=[Rbis, Rpow2], writes=[Rsteps])
                    S.op("pool", lambda e: e.memset(cnts[:], 0.0), writes=[Rcnts])
                    S.op("dve", lambda e: e.tensor_tensor(out=bis[:, 3:4], in0=bis[:, 1:2], in1=steps[:, 0:1], op=ALU.add),
                         reads=[Rbis, Rsteps], writes=[Rbis])
                    for k in range(NIT):
                        S.op("dve", lambda e: e.tensor_scalar(
                            out=jcnt[:, 0:nk], in0=score[:, 0:nk], scalar1=bis[:, 3:4], scalar2=0.0, op0=ALU.is_ge,
                            op1=ALU.add, accum_out=cnts[:, k:k + 1]), reads=[Rscore, Rbis, Rcnts],
                            writes=[Rjcnt, Rcnts])
                        S.op("dve", lambda e: e.scalar_tensor_tensor(
                            out=bis[:, 4:5], in0=cnts[:, k:k + 1], scalar=float(TOPK), in1=steps[:, k:k + 1],
                            op0=ALU.is_ge, op1=ALU.mult), reads=[Rcnts, Rsteps], writes=[Rbis])
                        S.op("dve", lambda e: e.scalar_tensor_tensor(
                            out=bis[:, 3:4], in0=bis[:, 3:4], scalar=steps[:, k + 1:k + 2], in1=bis[:, 4:5],
                            op0=ALU.subtract, op1=ALU.add), reads=[Rbis, Rsteps], writes=[Rbis])
                    S.op("dve", lambda e: e.tensor_tensor(out=bis[:, 1:2], in0=bis[:, 3:4], in1=steps[:, NIT:NIT + 1],
                                                          op=ALU.subtract), reads=[Rbis, Rsteps], writes=[Rbis])
                    thr, Rthr = bis[:, 1:2], Rbis
                else:
                    thr, Rthr = thrc[:, 0:1], Rthrc
                S.op("dve", lambda e: e.tensor_scalar(out=maskb[:, 0:nk], in0=score[:, 0:nk], scalar1=thr, scalar2=None,
                                                      op0=ALU.is_ge), reads=[Rscore, Rthr], writes=[Rmaskb])

            def stage_maskT(qb):
                mT, RmT = maskTs[qb % 2]
                for jb in range(0, qb + 1, 8):
                    nb = min(8, qb + 1 - jb)
                    for i in range(nb):
                        S.op("pe", lambda e: e.transpose(pT[:, i, :], maskb[:, (jb + i) * 128:(jb + i + 1) * 128], idb[:]),
                             reads=[Rmaskb, Ridb], writes=[RpT])
                    S.op("act", lambda e: e.copy(out=mT[:, jb:jb + nb, :], in_=pT[:, 0:nb, :]), reads=[RpT], writes=[RmT])

            def stage_attn(qb):
                q0 = qb * 128
                mT, RmT = maskTs[qb % 2]
                n = 0
                for j in range(qb + 1):
                    for g in range(4):
                        par, half = g % 2, g // 2
                        pst, Rpst = B[4 + n % 2]
                        E, RE = Es[n % 2]
                        PT, RPT = PTs[n % 2]
                        n += 1
                        S.op("pe", lambda e: e.matmul(
                            pst[:], lhsT=kT2[par * 64:par * 64 + 64, j * 128:(j + 1) * 128],
                            rhs=qT[par * 64:par * 64 + 64, 4 * half:4 * half + 4, q0:q0 + 128], start=True, stop=True),
                            reads=[RkT2, RqT], writes=[Rpst])
                        S.op("act", lambda e: e.activation(out=E[:], in_=pst[:], func=AF.Exp, scale=0.125),
                             reads=[Rpst], writes=[RE])
                        S.op("pool", lambda e: e.tensor_tensor(
                            out=PT[:], in0=E[:], in1=mT[:, j:j + 1, :].to_broadcast([128, 4, 128]), op=ALU.mult),
                            reads=[RE, RmT], writes=[RPT])
                        ot, Rot = B[g]
                        S.op("pe", lambda e: e.matmul(
                            ot[0:65], lhsT=Vaug[:, j, :], rhs=PT[:], start=(j == 0), stop=(j == qb)),
                            reads=[RV, RPT], writes=[Rot])

            def stage_out(qb):
                q0 = qb * 128
                S.dma("sp", lambda e: e.dma_start(out=xres[:], in_=src[base + q0:base + q0 + 128, :]), writes=[Rxres])
                for g in range(4):
                    ot, Rot = B[g]
                    otv = ot[:].rearrange("p a b -> p (a b)")
                    pbc, Rpbc = B[6]
                    pbcv = pbc[:].rearrange("p a b -> p (a b)")
                    at, Rat = attnT[g]
                    S.op("dve", lambda e: e.reciprocal(out=rs[64:65, :], in_=otv[64:65, :]), reads=[Rot], writes=[Rrs])
                    S.op("pe", lambda e: e.matmul(pbcv[0:64, :], lhsT=onesf[64:65, 0:64], rhs=rs[64:65, :],
                                                  start=True, stop=True), reads=[Rones, Rrs], writes=[Rpbc])
                    S.op("act", lambda e: e.copy(out=bc[:], in_=pbcv[0:64, :]), reads=[Rpbc], writes=[Rbc])
                    S.op("dve", lambda e: e.tensor_tensor(
                        out=at[:].rearrange("p a b -> p (a b)"), in0=otv[0:64, :], in1=bc[:], op=ALU.mult),
                        reads=[Rot, Rbc], writes=[Rat])
                for dh in range(2):
                    po, Rpo = B[6]
                    pov = po[:].rearrange("p a b -> p (a b)")
                    n2 = 0
                    for g in range(4):
                        par, half = g % 2, g // 2
                        at, Rat = attnT[g]
                        for i in range(4):
                            hh = 2 * (4 * half + i) + par
                            S.op("pe", lambda e: e.matmul(
                                pov, lhsT=at[:, i, :], rhs=wo[:, hh, dh * 512:(dh + 1) * 512], start=(n2 == 0),
                                stop=(n2 == 15)), reads=[Rat, Rwo], writes=[Rpo])
                            n2 += 1
                    S.op("dve", lambda e: e.tensor_tensor(
                        out=xres[:, dh * 512:(dh + 1) * 512], in0=pov, in1=xres[:, dh * 512:(dh + 1) * 512], op=ALU.add),
                        reads=[Rpo, Rxres], writes=[Rxres])
                S.dma("pool", lambda e: e.dma_start(out=dst[base + q0:base + q0 + 128, :], in_=xres[:]), reads=[Rxres],
                      writes=[])

            qbl = list(DBG["qbs"]) if DBG["qbs"] is not None else list(range(SEQ // 128))
            if qbl:
                stage_index(qbl[0])
                stage_maskT(qbl[0])
            for ii, qb in enumerate(qbl):
                nxt = qbl[ii + 1] if ii + 1 < len(qbl) else None
                if nxt is not None:
                    stage_index(nxt)
                stage_attn(qb)
                if nxt is not None:
                    stage_maskT(nxt)
                stage_out(qb)
        S.flush()


def dsa_consts():
    inv = (10000.0 ** (-(np.arange(0, 64, 2, dtype=np.float32)) / np.float32(64))).astype(np.float32)
    ang = (np.arange(SEQ, dtype=np.float32)[:, None] * inv[None, :]).astype(np.float32)
    cos, sin = np.cos(ang).astype(np.float32), np.sin(ang).astype(np.float32)
    p = np.arange(128)
    d = p % 64
    c_cos = np.ascontiguousarray(cos[:, d % 32].T)
    sgn = np.where(d < 32, -1.0, 1.0).astype(np.float32)
    c_sin = np.ascontiguousarray((sin[:, d % 32] * sgn[None, :]).T.astype(np.float32))
    pm = np.zeros((128, 128), np.float32)
    partner = (d + 32) % 64 + 64 * (p // 64)
    pm[p, partner] = 1.0
    negmask = np.zeros((128, 128), np.float32)
    negmask[:64, 64:] = -1e30
    pow2 = np.tile((2.0 ** -(1.0 + np.arange(NIT + 1))).astype(np.float32)[None, :], (128, 1))
    return {"c_cos": c_cos, "c_sin": c_sin, "c_pm": pm, "c_negmask": negmask, "c_pow2": np.ascontiguousarray(pow2)}


TL = 256
NCH = TL // 128
CDEC = float(np.exp(-0.5))
P_MU, P_W0, P_AB, P_KK, P_KA, P_RK, P_CB, P_BRG, P_BIG, P_LAM, P_CW, P_G = 0, 14, 18, 22, 26, 30, 34, 38, 42, 46, 50, 66
NP0 = 74


def phase_l0(nc, S, src, dst, Dm):
    with ExitStack() as st:
        C = Ctx(nc, S, st)
        S.barrier()
        win, _ = C.sb([128, 8, 2816], BF16, "win")
        Rwin = [S.res() for _ in range(8)]
        load_weight(S, "pool", win, Dm["w_in0"], 8, Rwin)
        wo, _ = C.sb([128, 8, D], BF16, "wo0")
        Rwo = [S.res() for _ in range(8)]
        load_weight(S, "pool", wo, Dm["w_out0"], 8, Rwo)
        P0, RP0 = C.sb([128, NP0], F32, "P0")
        S.dma("sp", lambda e: e.dma_start(out=P0[:], in_=Dm["p0"]), writes=[RP0])
        lora, Rlora = C.sb([128, 512], BF16, "lora")
        S.dma("pool", lambda e: e.dma_start(out=lora[:], in_=Dm["lora0"]), writes=[Rlora])
        wgu, Rwgu = C.sb([128, 512], BF16, "wgu")
        S.dma("pool", lambda e: e.dma_start(out=wgu[:], in_=Dm["w_g_up0"]), writes=[Rwgu])
        gbd, Rgbd = C.sb([128, 8, 128], BF16, "gbd")
        S.dma("pool", lambda e: e.dma_start(out=gbd[:], in_=Dm["gates_bd0"]), writes=[Rgbd])
        lnw, Rlnw = C.sb([128, 2, 512], F32, "lnw")
        S.dma("sp", lambda e: e.dma_start(out=lnw[:, 0, :], in_=Dm["lnx0"][0, :].partition_broadcast(128)), writes=[Rlnw])
        S.dma("sp", lambda e: e.dma_start(out=lnw[:, 1, :], in_=Dm["lnx0"][1, :].partition_broadcast(128)), writes=[Rlnw])
        cm, Rcm = C.sb([128, 128 + 256 + 256 + 128 + 2], BF16, "cm")
        S.dma("pool", lambda e: e.dma_start(out=cm[:], in_=Dm["c_masks0"]), writes=[Rcm])
        maskL, maskU2, maskU3, BD, IND = cm[:, 0:128], cm[:, 128:384], cm[:, 384:640], cm[:, 640:768], cm[:, 768:770]
        idf, Ridf, idb, Ridb = make_ident(S, C)
        onesT, Rones = C.sb([128, 128], F32, "onesT")
        S.op("pool", lambda e: e.memset(onesT[:], 1.0), writes=[Rones])
        spt, Rspt = C.sb([128, 16], F32, "spt")
        S.op("act", lambda e: e.activation(out=spt[:, 0:4], in_=P0[:, P_LAM:P_LAM + 4], func=AF.Exp, scale=-1.0),
             reads=[RP0], writes=[Rspt])
        S.op("act", lambda e: e.activation(out=spt[:, 0:4], in_=spt[:, 0:4], func=AF.Ln, bias=1.0), reads=[Rspt],
             writes=[Rspt])
        for i, m in ((1, 8.0), (2, -8.0), (3, -16.0)):
            S.op("dve", lambda e, i=i, m=m: e.tensor_scalar(out=spt[:, 4 * i:4 * i + 4], in0=spt[:, 0:4], scalar1=m,
                                                            scalar2=None, op0=ALU.mult), reads=[Rspt], writes=[Rspt])

        xts = [C.sb([128, NCH, D], F32) for _ in range(2)]
        hT, RhT = C.sb([128, 8, TL], BF16, "hT")
        junk, Rjunk = C.sb([128, D], BF16, "junk")
        ss, Rss = C.sb([128, 1], F32, "ss")
        xn, Rxn = C.sb([128, D], BF16, "xn")
        Pb = [C.sb([128, TL + 1], F32) for _ in range(14)]
        LX = [C.sb([128, TL + 3], F32) for _ in range(4)]
        hprev, Rhprev = C.sb([128, 4], F32, "hprev")
        tmps = {}

        def tmp(name, dt=F32, shape=None):
            if name not in tmps:
                tmps[name] = C.sb(shape or [128, TL], dt, "tmp_" + name)
            return tmps[name]

        ART = [C.sb([128, NCH, 2, 128], BF16) for _ in range(4)]
        BTt = [C.sb([128, TL], BF16) for _ in range(4)]
        KTt = [C.sb([128, TL], BF16) for _ in range(4)]
        BTm = [C.sb([128, 2, TL], BF16) for _ in range(4)]
        KTm = [C.sb([128, 2, TL], BF16) for _ in range(4)]
        Hm = [C.sb([128, 2, 64], BF16) for _ in range(4)]
        hmk, Rhmk = C.sb([128, 2], F32, "hmk")
        S.op("pool", lambda e: e.memset(hmk[:], 0.0), writes=[Rhmk])
        S.op("pool", lambda e: e.memset(hmk[0:64, 0:1], 1.0), writes=[Rhmk])
        S.op("pool", lambda e: e.memset(hmk[64:128, 1:2], 1.0), writes=[Rhmk])
        TM = [C.sb([128, 3, 4, 128], BF16) for _ in range(NCH)]
        WL, RWL = C.sb([128, 4, NCH], F32, "WL")
        Hs = [C.sb([128, 64], F32) for _ in range(4)]
        Hb = [C.sb([128, 64], BF16) for _ in range(4)]
        mixT, RmixT = C.sb([128, 8, TL], BF16, "mixT")
        bons, Rbons = C.sb([128, NCH, 8], F32, "bons")
        pT, RpT = C.ps([128, 8, 128], BF16, "pT")
        PP, RPP_ = C.ps([128, 2, TL], F32, "PP")
        RPP = [S.res(bank=RPP_.bank), S.res(bank=RPP_.bank)]
        BKA, RBKA_ = C.ps([128, 2, 256], F32, "BKA")
        BKB, RBKB_ = C.ps([128, 2, 256], F32, "BKB")
        RBK = [S.res(bank=RBKA_.bank), S.res(bank=RBKA_.bank), S.res(bank=RBKB_.bank), S.res(bank=RBKB_.bank)]
        slots = [BKA[:, 0, :], BKA[:, 1, :], BKB[:, 0, :], BKB[:, 1, :]]
        BKC, RBKC_ = C.ps([128, 4, 128], F32, "BKC")
        RBKC = [S.res(bank=RBKC_.bank), S.res(bank=RBKC_.bank)]
        BKD, RBKD_ = C.ps([128, 512], F32, "BKD")
        RX, RU, RdH, Rbon = [S.res(bank=RBKD_.bank) for _ in range(4)]
        YT, RYT = C.ps([128, 8, 64], F32, "YT")
        GP, RGP = C.ps([128, 512], F32, "GP")
        nslot = [0]

        def slot():
            i = nslot[0] % 4
            nslot[0] += 1
            return slots[i], RBK[i]

        def PV(col, n=4):
            return P0[:, col:col + 1] if n == 1 else P0[:, col:col + n]

        for sq in range(DBG["nseq"]):
            base = sq * SEQ
            for i in range(14):
                S.op("pool", lambda e, i=i: e.memset(Pb[i][0][:, 0:1], 0.0), writes=[Pb[i][1]])
            for i in range(4):
                S.op("pool", lambda e, i=i: e.memset(LX[i][0][:, 0:3], 0.0), writes=[LX[i][1]])
                S.op("pool", lambda e, i=i: e.memset(Hs[i][0][:], 0.0), writes=[Hs[i][1]])
                S.op("pool", lambda e, i=i: e.memset(Hb[i][0][:], 0.0), writes=[Hb[i][1]])
                S.op("pool", lambda e, i=i: e.memset(Hm[i][0][:], 0.0), writes=[Hm[i][1]])
            S.op("pool", lambda e: e.memset(hprev[:], 0.0), writes=[Rhprev])
            for tt in range(DBG["l0_tiles"] or SEQ // TL):
                t0 = tt * TL
                xt, Rxt = xts[tt % 2]
                S.dma("sp", lambda e, xt=xt, a=base + t0: e.dma_start(
                    out=xt[:], in_=src[a:a + TL, :].rearrange("(s p) d -> p s d", p=128)), writes=[Rxt])
                for s in range(NCH):
                    norm_T(S, xt[:, s, :], Rxt, P0[:, P_G:P_G + 8], RP0, hT[:, :, s * 128:(s + 1) * 128], RhT,
                           (junk, Rjunk, ss, Rss, xn, Rxn, pT, RpT, idb, Ridb))
                npp = [0]

                def proj(nt):
                    i = npp[0] % 2
                    npp[0] += 1
                    for k in range(8):
                        S.op("pe", lambda e, k=k, nt=nt, i=i: e.matmul(
                            PP[:, i, :], lhsT=win[:, k, nt * 128:(nt + 1) * 128], rhs=hT[:, k, :], start=(k == 0),
                            stop=(k == 7)), reads=[Rwin[k], RhT], writes=[RPP[i]])
                    return PP[:, i, :], RPP[i]

                def shift(nt, name):
                    pp, Rpp = proj(nt)
                    Pt, RPt = Pb[nt]
                    o, Ro = tmp(name)
                    d_, Rd_ = tmp("shd")
                    S.op("act", lambda e: e.copy(out=Pt[:, 1:TL + 1], in_=pp), reads=[Rpp], writes=[RPt])
                    S.op("dve", lambda e: e.tensor_tensor(out=d_[:], in0=Pt[:, 0:TL], in1=Pt[:, 1:TL + 1], op=ALU.subtract),
                         reads=[RPt], writes=[Rd_])
                    S.op("dve", lambda e: e.scalar_tensor_tensor(out=o[:], in0=d_[:], scalar=P0[:, P_MU + nt:P_MU + nt + 1],
                                                                 in1=Pt[:, 1:TL + 1], op0=ALU.mult, op1=ALU.add),
                         reads=[Rd_, RPt, RP0], writes=[Ro])
                    S.op("act", lambda e: e.copy(out=Pt[:, 0:1], in_=Pt[:, TL:TL + 1]), reads=[RPt], writes=[RPt])
                    return o, Ro

                LV = DBG.get('l0_lvl', 9)
                if LV < 2:
                    continue
                swa, Rswa = shift(12, "s_wa")
                lor, Rlor = tmp("lor", BF16)
                S.op("act", lambda e: e.activation(out=lor[0:64, :], in_=swa[0:64, :], func=AF.Tanh), reads=[Rswa],
                     writes=[Rlor])
                S.op("act", lambda e: e.copy(out=lor[64:128, :], in_=swa[64:128, :]), reads=[Rswa], writes=[Rlor])
                sgd_, Rsgd_ = shift(13, "s_gd")
                sgd, Rsgd = tmp("sgd", BF16)
                S.op("act", lambda e: e.activation(out=sgd[:], in_=sgd_[:], func=AF.Sigmoid), reads=[Rsgd_], writes=[Rsgd])

                if LV < 3:
                    continue
                for j in range(4):
                    r_, Rr = shift(j, "s_r")
                    k_, Rk = shift(4 + j, "s_k")
                    v_, Rv = shift(8 + j, "s_v")
                    art, Rart = ART[j]
                    bt, Rbt = BTt[j]
                    kt, Rkt = KTt[j]
                    zw, Rzw = slot()
                    S.op("pe", lambda e, j=j, zw=zw: e.matmul(zw, lhsT=lora[0:64, j * 128:(j + 1) * 128], rhs=lor[0:64, :],
                                                              start=True, stop=True), reads=[Rlora, Rlor], writes=[Rzw])
                    ee, Ree = tmp("ee")
                    S.op("act", lambda e, j=j, zw=zw: e.activation(out=ee[:], in_=zw, func=AF.Sigmoid,
                                                                  bias=P0[:, P_W0 + j:P_W0 + j + 1]),
                         reads=[Rzw, RP0], writes=[Ree])
                    za, Rza = slot()
                    S.op("pe", lambda e, j=j, za=za: e.matmul(za, lhsT=lora[64:128, j * 128:(j + 1) * 128],
                                                              rhs=lor[64:128, :], start=True, stop=True),
                         reads=[Rlora, Rlor], writes=[Rza])
                    aa, Raa = tmp("aa")
                    S.op("act", lambda e, j=j, za=za: e.activation(out=aa[:], in_=za, func=AF.Sigmoid,
                                                                  bias=P0[:, P_AB + j:P_AB + j + 1]),
                         reads=[Rza, RP0], writes=[Raa])
                    cs, Rcs = tmp("cs")
                    for c in range(NCH):
                        S.op("dve", lambda e, c=c: e.tensor_tensor_scan(
                            out=cs[:, c * 128:(c + 1) * 128], data0=onesT[:], data1=ee[:, c * 128:(c + 1) * 128],
                            initial=0.0, op0=ALU.mult, op1=ALU.add), reads=[Rones, Ree], writes=[Rcs])
                    csm, Rcsm = tmp("csm")
                    S.op("dve", lambda e: e.tensor_tensor(out=csm[:], in0=cs[:], in1=ee[:], op=ALU.subtract),
                         reads=[Rcs, Ree], writes=[Rcsm])
                    einv, Reinv = tmp("einv")
                    edec, Redec = tmp("edec")
                    eprev, Reprev = tmp("eprev")
                    S.op("act", lambda e: e.activation(out=einv[:], in_=cs[:], func=AF.Exp, scale=CDEC), reads=[Rcs],
                         writes=[Reinv])
                    S.op("act", lambda e: e.activation(out=edec[:], in_=cs[:], func=AF.Exp, scale=-CDEC), reads=[Rcs],
                         writes=[Redec])
                    S.op("act", lambda e: e.activation(out=eprev[:], in_=csm[:], func=AF.Exp, scale=-CDEC), reads=[Rcsm],
                         writes=[Reprev])
                    S.op("dve", lambda e, j=j: e.tensor_copy(
                        out=WL[:, j, :], in_=edec[:].rearrange("p (c t) -> p c t", t=128)[:, :, 127]), reads=[Redec],
                        writes=[RWL])
                    kk, Rkk = tmp("kk")
                    S.op("dve", lambda e, j=j: e.tensor_scalar(out=kk[:], in0=k_[:], scalar1=P0[:, P_KK + j:P_KK + j + 1],
                                                               scalar2=None, op0=ALU.mult), reads=[Rk, RP0], writes=[Rkk])
                    kk2, Rkk2 = tmp("kk2", BF16)
                    S.op("pool", lambda e: e.tensor_tensor(out=kk2[:], in0=kk[:], in1=kk[:], op=ALU.mult), reads=[Rkk],
                         writes=[Rkk2])
                    zs, Rzs = slot()
                    S.op("pe", lambda e, zs=zs: e.matmul(zs, lhsT=BD, rhs=kk2[:], start=True, stop=True),
                         reads=[Rcm, Rkk2], writes=[Rzs])
                    rn, Rrn = tmp("rn")
                    S.op("act", lambda e, zs=zs: e.activation(out=rn[:], in_=zs, func=AF.Ln, bias=1e-24), reads=[Rzs],
                         writes=[Rrn])
                    S.op("act", lambda e: e.activation(out=rn[:], in_=rn[:], func=AF.Exp, scale=-0.5), reads=[Rrn],
                         writes=[Rrn])
                    S.op("dve", lambda e: e.tensor_tensor(out=kk[:], in0=kk[:], in1=rn[:], op=ALU.mult), reads=[Rkk, Rrn],
                         writes=[Rkk])
                    km, Rkm = tmp("km")
                    S.op("dve", lambda e, j=j: e.tensor_scalar(out=km[:], in0=aa[:], scalar1=-1.0,
                                                               scalar2=P0[:, P_KA + j:P_KA + j + 1], op0=ALU.add,
                                                               op1=ALU.mult), reads=[Raa, RP0], writes=[Rkm])
                    S.op("dve", lambda e: e.scalar_tensor_tensor(out=km[:], in0=km[:], scalar=1.0, in1=k_[:], op0=ALU.add,
                                                                 op1=ALU.mult), reads=[Rkm, Rk], writes=[Rkm])
                    S.op("dve", lambda e, art=art: e.tensor_tensor(
                        out=art[:, :, 0, :], in0=kk[:].rearrange("p (c t) -> p c t", t=128),
                        in1=eprev[:].rearrange("p (c t) -> p c t", t=128), op=ALU.mult), reads=[Rkk, Reprev], writes=[Rart])
                    S.op("pool", lambda e, art=art: e.tensor_tensor(
                        out=art[:, :, 1, :], in0=r_[:].rearrange("p (c t) -> p c t", t=128),
                        in1=edec[:].rearrange("p (c t) -> p c t", t=128), op=ALU.mult), reads=[Rr, Redec], writes=[Rart])
                    tb, Rtb = tmp("tb")
                    S.op("pool", lambda e: e.tensor_tensor(out=tb[:], in0=kk[:], in1=aa[:], op=ALU.mult), reads=[Rkk, Raa],
                         writes=[Rtb])
                    S.op("dve", lambda e, bt=bt: e.tensor_tensor(out=bt[:], in0=tb[:], in1=einv[:], op=ALU.mult),
                         reads=[Rtb, Reinv], writes=[Rbt])
                    S.op("pool", lambda e, kt=kt: e.tensor_tensor(out=kt[:], in0=km[:], in1=einv[:], op=ALU.mult),
                         reads=[Rkm, Reinv], writes=[Rkt])
                    btm, Rbtm = BTm[j]
                    ktm, Rktm = KTm[j]
                    for h in range(2):
                        S.op("dve", lambda e, h=h, btm=btm, bt=bt: e.tensor_scalar(
                            out=btm[:, h, :], in0=bt[:], scalar1=hmk[:, h:h + 1], scalar2=None, op0=ALU.mult),
                            reads=[Rbt, Rhmk], writes=[Rbtm])
                        S.op("pool", lambda e, h=h, ktm=ktm, kt=kt: e.tensor_scalar(
                            out=ktm[:, h, :], in0=kt[:], scalar1=hmk[:, h:h + 1], scalar2=None, op0=ALU.mult),
                            reads=[Rkt, Rhmk], writes=[Rktm])
                    rkr, Rrkr = tmp("rkr", BF16)
                    S.op("dve", lambda e, j=j: e.scalar_tensor_tensor(out=rkr[:], in0=r_[:],
                                                                      scalar=P0[:, P_RK + j:P_RK + j + 1], in1=km[:],
                                                                      op0=ALU.mult, op1=ALU.mult), reads=[Rr, Rkm, RP0],
                         writes=[Rrkr])
                    for c in range(NCH):
                        S.op("pe", lambda e, c=c, j=j: e.matmul(BKD[:, 384 + c * 8 + 2 * j:384 + c * 8 + 2 * j + 2],
                                                                lhsT=rkr[:, c * 128:(c + 1) * 128], rhs=IND, start=True,
                                                                stop=True), reads=[Rrkr, Rcm], writes=[Rbon])
                    vb, Rvb = tmp("vb", BF16)
                    S.op("act", lambda e: e.copy(out=vb[:], in_=v_[:]), reads=[Rv], writes=[Rvb])
                    for c in range(NCH):
                        tm, Rtm = TM[c]
                        for i, (srct, Rs) in enumerate(((kt, Rkt), (bt, Rbt), (vb, Rvb))):
                            S.op("pe", lambda e, i=i, c=c, srct=srct: e.transpose(
                                pT[:, i, :], srct[:, c * 128:(c + 1) * 128], idb[:]), reads=[Rs, Ridb], writes=[RpT])
                        S.op("act", lambda e, tm=tm, j=j: e.copy(out=tm[:, :, j, :], in_=pT[:, 0:3, :]), reads=[RpT],
                             writes=[Rtm])
                S.op("act", lambda e: e.copy(out=bons[:].rearrange("p c h -> p (c h)"), in_=BKD[:, 384:384 + NCH * 8]),
                     reads=[Rbon], writes=[Rbons])

                if DBG.get("dummy_alloc"):
                    for j_ in range(4):
                        tmp(f"M0_{j_}", BF16, [128, 2, 128]); tmp(f"NB_{j_}", BF16, [128, 2, 256]); tmp(f"NK_{j_}", BF16, [128, 2, 256])
                if LV < 4:
                    continue
                for c in range(NCH):
                    tm, Rtm = TM[c]
                    for j in range(4):
                        art, Rart = ART[j]
                        bt, Rbt = BTt[j]
                        kt, Rkt = KTt[j]
                        Hf, RHf = Hs[j]
                        Hh, RHh = Hm[j]
                        btm, Rbtm = BTm[j]
                        ktm, Rktm = KTm[j]
                        M0, RM0 = tmp(f"M0_{j}", BF16, [128, 2, 128])
                        NB, RNB = tmp(f"NB_{j}", BF16, [128, 2, 256])
                        NK, RNK = tmp(f"NK_{j}", BF16, [128, 2, 256])
                        pa, Rpa = BKC[:, 0:2, :], RBKC[0]
                        for h in range(2):
                            po = h * 64
                            v_ = DBG.get("p1var", 0)
                            if v_ == 3:
                                if c == 0 and j == 0 and h == 0:
                                    S.op("pe", lambda e, h=h, po=po, art=art, bt=bt, c=c: e.matmul(
                                        GP[:, 0:128], lhsT=bt[0:64, 0:128], rhs=bt[0:64, 0:128],
                                        start=True, stop=True), reads=[Rbt], writes=[RGP])
                                continue
                            if v_ in (5, 6, 7):
                                if (v_ == 5 and c == 0) or (v_ == 6 and j == 0) or (v_ == 7 and h == 0):
                                    S.op("pe", lambda e, h=h, po=po, art=art, bt=bt, c=c: e.matmul(
                                        GP[:, 0:128], lhsT=bt[po:po + 64, 0:128], rhs=bt[po:po + 64, 0:128],
                                        start=True, stop=True), reads=[Rbt], writes=[RGP])
                                continue
                            if v_ == 4:
                                if c == 0 and j == 0 and h == 0:
                                    S.op("pe", lambda e, h=h, po=po, art=art, bt=bt, c=c: e.matmul(
                                        GP[:, 0:128], lhsT=idb[:], rhs=idb[:],
                                        start=True, stop=True), reads=[Ridb], writes=[RGP])
                                continue
                            if v_ == 1:
                                S.op("pe", lambda e, h=h, po=po, art=art, bt=bt, c=c: e.matmul(
                                    BKC[:, h, :], lhsT=bt[po:po + 64, c * 128:(c + 1) * 128], rhs=bt[po:po + 64, c * 128:(c + 1) * 128],
                                    start=True, stop=True), reads=[Rbt], writes=[Rpa])
                                continue
                            if v_ == 2:
                                S.op("pe", lambda e, h=h, po=po, art=art, bt=bt, c=c: e.matmul(
                                    GP[:, h * 128:(h + 1) * 128], lhsT=art[po:po + 64, c, 0, :], rhs=bt[po:po + 64, c * 128:(c + 1) * 128],
                                    start=True, stop=True), reads=[Rart, Rbt], writes=[RGP])
                                continue
                            S.op("pe", lambda e, h=h, po=po, art=art, btm=btm, c=c: e.matmul(
                                BKC[:, h, :], lhsT=art[:, c, 0, :], rhs=btm[:, h, c * 128:(c + 1) * 128],
                                start=True, stop=True), reads=[Rart, Rbtm], writes=[Rpa])
                        if DBG.get('l0_n', 9) < 1:
                            continue
                        S.op("dve", lambda e, M0=M0: e.scalar_tensor_tensor(
                            out=M0[:], in0=BKC[:, 0:2, :], scalar=-1.0, in1=maskL.unsqueeze(1).to_broadcast([128, 2, 128]),
                            op0=ALU.mult, op1=ALU.mult), reads=[Rpa, Rcm], writes=[RM0])
                        if DBG.get('l0_n', 9) < 2:
                            continue
                        for h in range(2):
                            po = h * 64
                            S.op("pe", lambda e, h=h, po=po, art=art, btm=btm, c=c: e.matmul(
                                BKA[:, h, :], lhsT=btm[:, h, c * 128:(c + 1) * 128],
                                rhs=art[:, c, :, :].rearrange("p a t -> p (a t)"), start=True, stop=True),
                                reads=[Rart, Rbtm], writes=[RBK[0], RBK[1]])
                        if DBG.get('l0_n', 9) < 3:
                            continue
                        S.op("dve", lambda e, NB=NB: e.tensor_tensor(
                            out=NB[:], in0=BKA[:], in1=maskU2.unsqueeze(1).to_broadcast([128, 2, 256]), op=ALU.mult),
                            reads=[RBK[0], RBK[1], Rcm], writes=[RNB])
                        for h in range(2):
                            po = h * 64
                            S.op("pe", lambda e, h=h, po=po, art=art, ktm=ktm, c=c: e.matmul(
                                BKB[:, h, :], lhsT=ktm[:, h, c * 128:(c + 1) * 128],
                                rhs=art[:, c, :, :].rearrange("p a t -> p (a t)"), start=True, stop=True),
                                reads=[Rart, Rktm], writes=[RBK[2], RBK[3]])
                        S.op("dve", lambda e, NK=NK: e.tensor_tensor(
                            out=NK[:], in0=BKB[:], in1=maskU3.unsqueeze(1).to_broadcast([128, 2, 256]), op=ALU.mult),
                            reads=[RBK[2], RBK[3], Rcm], writes=[RNK])
                        SL = DBG.get('l0_sub', 9)
                        if SL < 2:
                            continue
                        Q, RQ = tmp(f"Q_{j}", BF16, [128, 2, 128])
                        S.op("pool", lambda e, Q=Q, NB=NB: e.tensor_tensor(
                            out=Q[:], in0=NB[:, :, 0:128], in1=idb[:].unsqueeze(1).to_broadcast([128, 2, 128]), op=ALU.add),
                            reads=[RNB, Ridb], writes=[RQ])
                        Mc, RMc = M0, RM0
                        McT, RMcT = NB[:, :, 0:128], RNB
                        for i in range(1, 7):
                            Mn, RMn = tmp(f"M{i % 2}_{j}", BF16, [128, 2, 128])
                            for h in range(2):
                                S.op("pe", lambda e, h=h, Mc=Mc, McT=McT: e.matmul(
                                    BKC[:, 2 + h, :], lhsT=McT[:, h, :], rhs=Mc[:, h, :], start=True, stop=True),
                                    reads=[RMc, RMcT], writes=[RBKC[1]])
                            S.op("act", lambda e, Mn=Mn: e.copy(out=Mn[:], in_=BKC[:, 2:4, :]), reads=[RBKC[1]],
                                 writes=[RMn])
                            if i < 6:
                                MnT, RMnT = tmp(f"MT{i % 2}_{j}", BF16, [128, 2, 128])
                                for h in range(2):
                                    S.op("pe", lambda e, h=h, Mc=Mc, McT=McT: e.matmul(
                                        BKC[:, h, :], lhsT=Mc[:, h, :], rhs=McT[:, h, :], start=True, stop=True),
                                        reads=[RMc, RMcT], writes=[RBKC[0]])
                                S.op("act", lambda e, MnT=MnT: e.copy(out=MnT[:], in_=BKC[:, 0:2, :]), reads=[RBKC[0]],
                                     writes=[RMnT])
                            qs, Rqs = slot()
                            qsv = qs.rearrange("p (a t) -> p a t", t=128)
                            for h in range(2):
                                S.op("pe", lambda e, h=h, Mn=Mn, Q=Q, qsv=qsv: e.matmul(
                                    qsv[:, h, :], lhsT=Mn[:, h, :], rhs=Q[:, h, :], start=True, stop=True),
                                    reads=[RMn, RQ], writes=[Rqs])
                            S.op("dve", lambda e, Q=Q, qsv=qsv: e.tensor_tensor(out=Q[:], in0=qsv, in1=Q[:], op=ALU.add),
                                 reads=[Rqs, RQ], writes=[RQ])
                            Mc, RMc = Mn, RMn
                            if i < 6:
                                McT, RMcT = MnT[:], RMnT
                        if SL < 3:
                            continue
                        Xb, RXb = tmp(f"Xb_{j}", BF16, [128, 2, 64])
                        Un, RUn = tmp(f"Un_{j}", BF16, [128, 2, 64])
                        for h in range(2):
                            po = h * 64
                            S.op("pe", lambda e, h=h, po=po, art=art, Hh=Hh, c=c: e.matmul(
                                BKD[:, h * 64:(h + 1) * 64], lhsT=art[:, c, 0, :], rhs=Hh[:, h, :],
                                start=True, stop=False), reads=[Rart, RHh], writes=[RX])
                            S.op("pe", lambda e, h=h, po=po, NK=NK, tm=tm, j=j: e.matmul(
                                BKD[:, h * 64:(h + 1) * 64], lhsT=NK[:, h, 0:128], rhs=tm[:, 2, j, po:po + 64],
                                start=False, stop=True), reads=[RNK, Rtm], writes=[RX])
                        S.op("act", lambda e, Xb=Xb: e.copy(out=Xb[:].rearrange("p a v -> p (a v)"), in_=BKD[:, 0:128]),
                             reads=[RX], writes=[RXb])
                        for h in range(2):
                            S.op("pe", lambda e, h=h, Q=Q, Xb=Xb: e.matmul(
                                BKD[:, 128 + h * 64:128 + (h + 1) * 64], lhsT=Q[:, h, :], rhs=Xb[:, h, :], start=True,
                                stop=True), reads=[RQ, RXb], writes=[RU])
                        S.op("act", lambda e, Un=Un: e.mul(out=Un[:].rearrange("p a v -> p (a v)"), in_=BKD[:, 128:256],
                                                           mul=-1.0), reads=[RU], writes=[RUn])
                        if SL < 4:
                            continue
                        for h in range(2):
                            po = h * 64
                            hd = 2 * j + h
                            S.op("pe", lambda e, h=h, po=po, hd=hd, art=art, Hh=Hh, c=c: e.matmul(
                                YT[:, hd, :], lhsT=art[:, c, 1, :], rhs=Hh[:, h, :], start=True, stop=False),
                                reads=[Rart, RHh], writes=[RYT])
                            S.op("pe", lambda e, h=h, po=po, hd=hd, NK=NK, tm=tm, j=j: e.matmul(
                                YT[:, hd, :], lhsT=NK[:, h, 128:256], rhs=tm[:, 2, j, po:po + 64], start=False, stop=False),
                                reads=[RNK, Rtm], writes=[RYT])
                            S.op("pe", lambda e, h=h, hd=hd, NB=NB, Un=Un: e.matmul(
                                YT[:, hd, :], lhsT=NB[:, h, 128:256], rhs=Un[:, h, :], start=False, stop=True),
                                reads=[RNB, RUn], writes=[RYT])
                        if SL < 5:
                            continue
                        for h in range(2):
                            po = h * 64
                            S.op("pe", lambda e, h=h, po=po, tm=tm, j=j: e.matmul(
                                BKD[:, 256 + h * 64:256 + (h + 1) * 64], lhsT=tm[:, 0, j, :], rhs=tm[:, 2, j, po:po + 64],
                                start=True, stop=False), reads=[Rtm], writes=[RdH])
                            S.op("pe", lambda e, h=h, tm=tm, j=j, Un=Un: e.matmul(
                                BKD[:, 256 + h * 64:256 + (h + 1) * 64], lhsT=tm[:, 1, j, :], rhs=Un[:, h, :], start=False,
                                stop=True), reads=[Rtm, RUn], writes=[RdH])
                        for h in range(2):
                            po = h * 64
                            S.op("dve", lambda e, h=h, po=po, Hf=Hf: e.tensor_tensor(
                                out=Hf[po:po + 64, :], in0=BKD[po:po + 64, 256 + h * 64:256 + (h + 1) * 64],
                                in1=Hf[po:po + 64, :], op=ALU.add), reads=[RdH, RHf], writes=[RHf])
                        S.op("dve", lambda e, Hf=Hf, j=j, c=c: e.tensor_scalar(out=Hf[:], in0=Hf[:], scalar1=WL[:, j, c:c + 1],
                                                                               scalar2=None, op0=ALU.mult),
                             reads=[RHf, RWL], writes=[RHf])
                        for h in range(2):
                            S.op("pool", lambda e, h=h, Hf=Hf, Hh=Hh: e.tensor_scalar(
                                out=Hh[:, h, :], in0=Hf[:], scalar1=hmk[:, h:h + 1], scalar2=None, op0=ALU.mult),
                                reads=[RHf, Rhmk], writes=[RHh])

                    if LV < 5:
                        continue
                    ysb, Rysb = tmp("ysb", F32, [128, 8, 64])
                    st8, Rst8 = tmp("st8", F32, [128, 16])
                    S.op("act", lambda e: e.copy(out=ysb[:], in_=YT[:]), reads=[RYT], writes=[Rysb])
                    S.op("dve", lambda e: e.tensor_reduce(out=st8[:, 0:8], in_=ysb[:], axis=AX.X, op=ALU.add), reads=[Rysb],
                         writes=[Rst8])
                    S.op("dve", lambda e: e.tensor_scalar(out=st8[:, 0:8], in0=st8[:, 0:8], scalar1=1.0 / 64, scalar2=None,
                                                          op0=ALU.mult), reads=[Rst8], writes=[Rst8])
                    S.op("dve", lambda e: e.tensor_tensor(out=ysb[:], in0=ysb[:],
                                                          in1=st8[:, 0:8].unsqueeze(2).to_broadcast([128, 8, 64]),
                                                          op=ALU.subtract), reads=[Rysb, Rst8], writes=[Rysb])
                    ysq, Rysq = tmp("ysq", F32, [128, 8, 64])
                    S.op("pool", lambda e: e.tensor_tensor(out=ysq[:], in0=ysb[:], in1=ysb[:], op=ALU.mult), reads=[Rysb],
                         writes=[Rysq])
                    S.op("dve", lambda e: e.tensor_reduce(out=st8[:, 8:16], in_=ysq[:], axis=AX.X, op=ALU.add), reads=[Rysq],
                         writes=[Rst8])
                    sv, Rsv = tmp("sv", F32, [128, 8])
                    S.op("dve", lambda e: e.tensor_copy(out=sv[:], in_=st8[:, 8:16]), reads=[Rst8], writes=[Rsv])
                    rstd_ops(S, sv, Rsv, n=64, eps=64e-5)
                    S.op("dve", lambda e: e.tensor_tensor(out=ysb[:], in0=ysb[:],
                                                          in1=sv[:].unsqueeze(2).to_broadcast([128, 8, 64]), op=ALU.mult),
                         reads=[Rysb, Rsv], writes=[Rysb])
                    yf = ysb[:].rearrange("p h v -> p (h v)")
                    S.op("dve", lambda e: e.tensor_tensor(out=yf, in0=yf, in1=lnw[:, 0, :], op=ALU.mult), reads=[Rysb, Rlnw],
                         writes=[Rysb])
                    S.op("pool", lambda e: e.tensor_tensor(out=yf, in0=yf, in1=lnw[:, 1, :], op=ALU.add), reads=[Rysb, Rlnw],
                         writes=[Rysb])
                    bv, Rbv = tmp("bv", F32, [128, 8, 64])
                    S.op("pool", lambda e, tm=tm, c=c: e.tensor_tensor(
                        out=bv[:], in0=tm[:, 2, :, :].rearrange("p j (h v) -> p (j h) v", v=64),
                        in1=bons[:, c, :].unsqueeze(2).to_broadcast([128, 8, 64]), op=ALU.mult), reads=[Rtm, Rbons],
                        writes=[Rbv])
                    S.op("dve", lambda e: e.tensor_tensor(out=ysb[:], in0=ysb[:], in1=bv[:], op=ALU.add), reads=[Rysb, Rbv],
                         writes=[Rysb])
                    S.op("pe", lambda e, c=c: e.matmul(GP[:], lhsT=sgd[:, c * 128:(c + 1) * 128], rhs=wgu[:], start=True,
                                                       stop=True), reads=[Rsgd, Rwgu], writes=[RGP])
                    rwb, Rrwb = tmp("rwb", BF16, [128, 512])
                    S.op("dve", lambda e: e.tensor_tensor(out=rwb[:], in0=GP[:], in1=yf, op=ALU.mult), reads=[RGP, Rysb],
                         writes=[Rrwb])
                    for jj in range(4):
                        S.op("pe", lambda e, jj=jj: e.transpose(pT[:, 4 + jj, :], rwb[:, jj * 128:(jj + 1) * 128], idb[:]),
                             reads=[Rrwb, Ridb], writes=[RpT])
                    S.op("act", lambda e, c=c: e.copy(out=mixT[:, 0:4, c * 128:(c + 1) * 128], in_=pT[:, 4:8, :]),
                         reads=[RpT], writes=[RmixT])

                if LV < 6:
                    continue
                for jt in range(4):
                    lx, Rlx = LX[jt]
                    pp, Rpp = proj(14 + jt)
                    S.op("act", lambda e, lx=lx, pp=pp: e.copy(out=lx[:, 3:TL + 3], in_=pp), reads=[Rpp], writes=[Rlx])
                    xc, Rxc = tmp("ee")
                    cw = P_CW + 4 * jt
                    S.op("dve", lambda e, lx=lx, cw=cw, jt=jt: e.tensor_scalar(
                        out=xc[:], in0=lx[:, 3:TL + 3], scalar1=P0[:, cw + 3:cw + 4], scalar2=P0[:, P_CB + jt:P_CB + jt + 1],
                        op0=ALU.mult, op1=ALU.add), reads=[Rlx, RP0], writes=[Rxc])
                    for i in range(3):
                        S.op("dve", lambda e, lx=lx, cw=cw, i=i: e.scalar_tensor_tensor(
                            out=xc[:], in0=lx[:, i:TL + i], scalar=P0[:, cw + i:cw + i + 1], in1=xc[:], op0=ALU.mult,
                            op1=ALU.add), reads=[Rlx, RP0, Rxc], writes=[Rxc])
                    S.op("act", lambda e, lx=lx: e.copy(out=lx[:, 0:3], in_=lx[:, TL:TL + 3]), reads=[Rlx], writes=[Rlx])
                    xcb, Rxcb = tmp("kk2", BF16)
                    S.op("act", lambda e: e.copy(out=xcb[:], in_=xc[:]), reads=[Rxc], writes=[Rxcb])
                    zr, Rzr = slot()
                    S.op("pe", lambda e, zr=zr, jt=jt: e.matmul(zr, lhsT=gbd[:, jt, :], rhs=xcb[:], start=True, stop=True),
                         reads=[Rgbd, Rxcb], writes=[Rzr])
                    zi, Rzi = slot()
                    S.op("pe", lambda e, zi=zi, jt=jt: e.matmul(zi, lhsT=gbd[:, 4 + jt, :], rhs=xcb[:], start=True,
                                                                stop=True), reads=[Rgbd, Rxcb], writes=[Rzi])
                    rg, Rrg = tmp("aa")
                    ig, Rig = tmp("cs")
                    S.op("act", lambda e, zr=zr, jt=jt: e.activation(out=rg[:], in_=zr, func=AF.Sigmoid,
                                                                    bias=P0[:, P_BRG + jt:P_BRG + jt + 1]),
                         reads=[Rzr, RP0], writes=[Rrg])
                    S.op("act", lambda e, zi=zi, jt=jt: e.activation(out=ig[:], in_=zi, func=AF.Sigmoid,
                                                                    bias=P0[:, P_BIG + jt:P_BIG + jt + 1]),
                         reads=[Rzi, RP0], writes=[Rig])
                    th, Rth = tmp("csm")
                    a2, Ra2 = tmp("einv")
                    at_, Rat_ = tmp("edec")
                    S.op("act", lambda e, jt=jt: e.activation(out=th[:], in_=rg[:], func=AF.Tanh,
                                                             scale=spt[:, 4 + jt:5 + jt]), reads=[Rrg, Rspt], writes=[Rth])
                    S.op("act", lambda e, jt=jt: e.activation(out=a2[:], in_=rg[:], func=AF.Exp,
                                                             scale=spt[:, 12 + jt:13 + jt]), reads=[Rrg, Rspt], writes=[Ra2])
                    S.op("act", lambda e, jt=jt: e.activation(out=at_[:], in_=rg[:], func=AF.Exp,
                                                             scale=spt[:, 8 + jt:9 + jt]), reads=[Rrg, Rspt], writes=[Rat_])
                    S.op("dve", lambda e: e.scalar_tensor_tensor(out=a2[:], in0=a2[:], scalar=1.0, in1=th[:], op0=ALU.add,
                                                                 op1=ALU.mult), reads=[Ra2, Rth], writes=[Ra2])
                    S.op("act", lambda e: e.activation(out=a2[:], in_=a2[:], func=AF.Sqrt), reads=[Ra2], writes=[Ra2])
                    S.op("pool", lambda e: e.tensor_tensor(out=xc[:], in0=xc[:], in1=ig[:], op=ALU.mult), reads=[Rxc, Rig],
                         writes=[Rxc])
                    S.op("dve", lambda e: e.tensor_tensor(out=xc[:], in0=xc[:], in1=a2[:], op=ALU.mult), reads=[Rxc, Ra2],
                         writes=[Rxc])
                    hs, Rhs = tmp("eprev")
                    S.op("dve", lambda e, jt=jt: e.tensor_tensor_scan(out=hs[:], data0=at_[:], data1=xc[:],
                                                                      initial=hprev[:, jt:jt + 1], op0=ALU.mult,
                                                                      op1=ALU.add), reads=[Rat_, Rxc, Rhprev], writes=[Rhs])
                    S.op("dve", lambda e, jt=jt: e.tensor_copy(out=hprev[:, jt:jt + 1], in_=hs[:, TL - 1:TL]), reads=[Rhs],
                         writes=[Rhprev])
                    pg, Rpg = proj(18 + jt)
                    lg, Rlg = tmp("kk")
                    sqg, Rsqg = tmp("rn")
                    S.op("act", lambda e, pg=pg: e.copy(out=lg[:], in_=pg), reads=[Rpg], writes=[Rlg])
                    S.op("act", lambda e, pg=pg: e.activation(out=sqg[:], in_=pg, func=AF.Square), reads=[Rpg], writes=[Rsqg])
                    S.op("dve", lambda e: e.tensor_scalar(out=sqg[:], in0=sqg[:], scalar1=0.044715, scalar2=1.0, op0=ALU.mult,
                                                          op1=ALU.add), reads=[Rsqg], writes=[Rsqg])
                    S.op("pool", lambda e: e.tensor_tensor(out=sqg[:], in0=sqg[:], in1=lg[:], op=ALU.mult), reads=[Rsqg, Rlg],
                         writes=[Rsqg])
                    S.op("act", lambda e: e.activation(out=sqg[:], in_=sqg[:], func=AF.Sigmoid, scale=1.5957691216),
                         reads=[Rsqg], writes=[Rsqg])
                    S.op("pool", lambda e: e.tensor_tensor(out=sqg[:], in0=sqg[:], in1=lg[:], op=ALU.mult), reads=[Rsqg, Rlg],
                         writes=[Rsqg])
                    S.op("dve", lambda e, jt=jt: e.tensor_tensor(out=mixT[:, 4 + jt, :], in0=sqg[:], in1=hs[:], op=ALU.mult),
                         reads=[Rsqg, Rhs], writes=[RmixT])

                if LV < 7:
                    continue
                n = 0
                for s in range(NCH):
                    for dh in range(2):
                        po_, Rpo_ = slot()
                        pov = po_
                        for kc in range(8):
                            S.op("pe", lambda e, kc=kc, s=s, dh=dh: e.matmul(
                                GP[:], lhsT=mixT[:, kc, s * 128:(s + 1) * 128], rhs=wo[:, kc, dh * 512:(dh + 1) * 512],
                                start=(kc == 0), stop=(kc == 7)), reads=[RmixT, Rwo[kc]], writes=[RGP])
                        S.op("dve", lambda e, s=s, dh=dh, xt=xt: e.tensor_tensor(
                            out=xt[:, s, dh * 512:(dh + 1) * 512], in0=GP[:], in1=xt[:, s, dh * 512:(dh + 1) * 512],
                            op=ALU.add), reads=[RGP, Rxt], writes=[Rxt])
                S.dma("pool", lambda e, xt=xt, a=base + t0: e.dma_start(
                    out=dst[a:a + TL, :].rearrange("(s p) d -> p s d", p=128), in_=xt[:]), reads=[Rxt], writes=[])
        if DBG.get("verbose"):
            print("L0 sbuf bytes remaining", nc.sbuf_bytes_remaining, "ops", S.n_ops, {e: len(v) for e, v in S.prog.items()})
        S.flush()


def l0_host(inputs):
    f = lambda a: np.asarray(a, np.float32)
    p0 = np.zeros((128, NP0), np.float32)
    p0[:, P_MU:P_MU + 14] = fm(inputs["mu_shift"][0])
    p0[:, P_W0:P_W0 + 4] = fm(inputs["w_decay0"][0])
    p0[:, P_AB:P_AB + 4] = fm(inputs["a_bias"][0])
    p0[:, P_KK:P_KK + 4] = fm(inputs["k_k"][0])
    p0[:, P_KA:P_KA + 4] = fm(inputs["k_a"][0])
    p0[:, P_RK:P_RK + 4] = fm(f(inputs["r_k"][0]).reshape(-1))
    p0[:, P_CB:P_CB + 4] = fm(inputs["conv_b"][0])
    p0[:, P_BRG:P_BRG + 4] = fm(f(inputs["b_rgate"][0]).reshape(-1))
    p0[:, P_BIG:P_BIG + 4] = fm(f(inputs["b_igate"][0]).reshape(-1))
    p0[:, P_LAM:P_LAM + 4] = fm(inputs["lru_lambda"][0])
    cw = f(inputs["conv_w"][0])
    for jt in range(4):
        p0[:, P_CW + 4 * jt:P_CW + 4 * jt + 4] = cw[:, jt * 128:(jt + 1) * 128].T
    p0[:, P_G:P_G + 8] = fm(inputs["norm_mix"][0])
    lora = np.concatenate([f(inputs["w_decay_up"][0]), f(inputs["w_a_up"][0])], axis=0)
    gbd = np.zeros((128, 8, 128), np.float32)
    for gi, w in enumerate((f(inputs["w_rgate"][0]), f(inputs["w_igate"][0]))):
        for jt in range(4):
            gbd[0:64, gi * 4 + jt, 0:64] = w[2 * jt]
            gbd[64:128, gi * 4 + jt, 64:128] = w[2 * jt + 1]
    lnx = np.stack([f(inputs["lnx_w"][0]), f(inputs["lnx_b"][0])], axis=0)
    i = np.arange(128)
    low = (i[:, None] > i[None, :]).astype(np.float32)
    up_s = (i[:, None] < i[None, :]).astype(np.float32)
    up_i = (i[:, None] <= i[None, :]).astype(np.float32)
    bd = np.zeros((128, 128), np.float32)
    bd[:64, :64] = 1.0
    bd[64:, 64:] = 1.0
    ind = np.zeros((128, 2), np.float32)
    ind[:64, 0] = 1.0
    ind[64:, 1] = 1.0
    masks = np.concatenate([low, -up_s, up_i, up_s, up_i, bd, ind], axis=1)
    return {"w_in0": np.ascontiguousarray(f(inputs["w_in0"][0])), "w_out0": np.ascontiguousarray(f(inputs["w_out0"][0])),
            "p0": p0, "lora0": np.ascontiguousarray(lora), "w_g_up0": np.ascontiguousarray(f(inputs["w_g_up"][0])),
            "gates_bd0": gbd, "lnx0": np.ascontiguousarray(lnx), "c_masks0": np.ascontiguousarray(masks)}
```

```python
import numpy as np
from contextlib import ExitStack
import concourse.bass as bass
import concourse.mybir as mybir
from concourse.bass_utils import run_bass_kernel_spmd

F32 = mybir.dt.float32
BF16 = mybir.dt.bfloat16
AF = mybir.ActivationFunctionType
ALU = mybir.AluOpType
AX = mybir.AxisListType


class Res:
    __slots__ = ("name", "w", "readers", "bank")

    def __init__(self, name="", bank=None):
        self.name = name
        self.w = None
        self.readers = {}
        self.bank = bank


class _Rec:
    def __init__(self):
        self.call = None

    def __getattr__(self, name):
        def f(*a, **k):
            self.call = (name, a, k)
            return self
        return f


def _eager(fn):
    rec = _Rec()
    fn(rec)
    name, a, k = rec.call
    return lambda e: getattr(e, name)(*a, **k)


class Sched:
    COMPUTE = ("pe", "act", "dve", "pool")
    NDSEM = 6

    def __init__(self, nc, stack, dma_queues=("sp", "pool")):
        self.nc = nc
        self.sems = {}
        self.ecnt = {}
        self.prog = {e: [] for e in ("pe", "act", "dve", "pool", "sp")}
        self.known = {e: {} for e in self.prog}
        for e in self.COMPUTE:
            self.sems["e_" + e] = stack.enter_context(nc.semaphore("e_" + e))
            self.ecnt[e] = 0
        self.dq = {}
        for q in dma_queues:
            keys = []
            for i in range(self.NDSEM):
                k = f"d_{q}{i}"
                self.sems[k] = stack.enter_context(nc.semaphore(k))
                keys.append(k)
            self.dq[q] = {"keys": keys, "cnt": [0] * self.NDSEM, "next": 0}
        self.n_ops = 0

    def res(self, name="", bank=None):
        return Res(name, bank)

    def _deps(self, eng, is_dma, reads, writes):
        deps = {}

        def add(t, raw):
            key, val, peng, pdma = t
            if not pdma and peng == eng and not is_dma:
                if not raw or eng == "pe":
                    return
            if deps.get(key, 0) < val:
                deps[key] = val

        for r in reads:
            if r.w is not None:
                add(r.w, True)
        for r in list(reads) + list(writes):
            if r.bank is not None:
                for key, (val, peng, pdma) in r.bank.readers.items():
                    if peng != eng:
                        add((key, val, peng, pdma), False)
        for w in writes:
            if w.w is not None:
                add(w.w, False)
            for key, (val, peng, pdma) in w.readers.items():
                add((key, val, peng, pdma), False)
        kn = self.known[eng]
        out = []
        for key, val in deps.items():
            if kn.get(key, 0) < val:
                kn[key] = val
                out.append((key, val))
        return out

    def _commit(self, tick, reads, writes):
        key, val, eng, is_dma = tick
        for w in writes:
            w.w = tick
            w.readers = {}
        for r in list(reads) + list(writes):
            if r.bank is not None:
                r.bank.readers[key] = (val, eng, is_dma)
        for r in reads:
            if r in writes:
                continue
            r.readers[key] = (val, eng, is_dma)

    def op(self, eng, fn, reads=(), writes=()):
        reads = [r for r in reads if r is not None]
        writes = [w for w in writes if w is not None]
        waits = self._deps(eng, False, reads, writes)
        self.ecnt[eng] += 1
        tick = ("e_" + eng, self.ecnt[eng], eng, False)
        self.prog[eng].append((waits, _eager(fn), ("e_" + eng, 1)))
        self._commit(tick, reads, writes)
        self.n_ops += 1

    def dma(self, q, fn, reads=(), writes=()):
        reads = [r for r in reads if r is not None]
        writes = [w for w in writes if w is not None]
        waits = self._deps(q, True, reads, writes)
        d = self.dq[q]
        slot = d["next"] % self.NDSEM
        d["next"] += 1
        key = d["keys"][slot]
        if d["cnt"][slot] > 0 and self.known[q].get(key, 0) < d["cnt"][slot]:
            self.known[q][key] = d["cnt"][slot]
            waits.append((key, d["cnt"][slot]))
        d["cnt"][slot] += 16
        tick = (key, d["cnt"][slot], q, True)
        self.prog[q].append((waits, _eager(fn), (key, 16)))
        self._commit(tick, reads, writes)
        self.n_ops += 1

    def barrier(self):
        allv = {}
        for e in self.COMPUTE:
            if self.ecnt[e] > 0:
                allv["e_" + e] = self.ecnt[e]
        for q, d in self.dq.items():
            for k, c in zip(d["keys"], d["cnt"]):
                if c > 0:
                    allv[k] = c
        for e in self.prog:
            waits = []
            for k, v in allv.items():
                if self.known[e].get(k, 0) < v:
                    self.known[e][k] = v
                    waits.append((k, v))
            if waits:
                self.prog[e].append((waits, None, None))

    def flush(self, final=False):
        nc = self.nc
        sems = self.sems
        fin = {}
        if final:
            for q, d in self.dq.items():
                for k, c in zip(d["keys"], d["cnt"]):
                    if c > 0:
                        fin[k] = c

        def run(eng_obj, items, extra=None):
            for waits, fn, inc in items:
                for key, val in waits:
                    eng_obj.wait_ge(sems[key], val)
                if fn is not None:
                    ins = fn(eng_obj)
                    ins.then_inc(sems[inc[0]], inc[1])
            if extra:
                for key, val in extra.items():
                    eng_obj.wait_ge(sems[key], val)

        prog = self.prog
        with nc.Block() as block:
            @block.tensor
            def _(e):
                run(e, prog["pe"])

            @block.scalar
            def _(e):
                run(e, prog["act"])

            @block.vector
            def _(e):
                run(e, prog["dve"])

            @block.gpsimd
            def _(e):
                run(e, prog["pool"])

            @block.sync
            def _(e):
                run(e, prog["sp"], extra=fin)
        self.prog = {e: [] for e in prog}


D = 1024
SEQ = 2048
NSEQ = 2
NTOK = NSEQ * SEQ
FF = 2816
EPS = 1e-6


class Ctx:
    _phase = [0]

    def __init__(self, nc, S, st):
        self.nc, self.S, self.st = nc, S, st
        self.n = 0
        Ctx._phase[0] += 1
        self.pfx = f"ph{Ctx._phase[0]}_"

    def sb(self, shape, dt, name=None):
        self.n += 1
        t = self.st.enter_context(self.nc.sbuf_tensor(self.pfx + (name or f"t{self.n}"), list(shape), dt))
        return t, self.S.res(name or f"t{self.n}")

    def ps(self, shape, dt, name=None):
        self.n += 1
        t = self.st.enter_context(self.nc.psum_tensor(self.pfx + (name or f"p{self.n}"), list(shape), dt))
        r = self.S.res(name or f"p{self.n}")
        r.bank = Res("bank")
        return t, r


def load_weight(S, q, dst, src, nk, res_list):
    for k in range(nk):
        S.dma(q, lambda e, k=k: e.dma_start(out=dst[:, k, :], in_=src[k * 128:(k + 1) * 128, :]),
              writes=[res_list[k]])


def rstd_ops(S, ss, Rss, n=D, eps=EPS):
    S.op("dve", lambda e: e.tensor_scalar(out=ss[:], in0=ss[:], scalar1=1.0 / n, scalar2=eps,
                                          op0=ALU.mult, op1=ALU.add), reads=[Rss], writes=[Rss])
    S.op("act", lambda e: e.activation(out=ss[:], in_=ss[:], func=AF.Sqrt), reads=[Rss], writes=[Rss])
    S.op("dve", lambda e: e.reciprocal(out=ss[:], in_=ss[:]), reads=[Rss], writes=[Rss])


def norm_T(S, xt, Rxt, gain, Rg, hT, RhT, tmp):
    junk, Rjunk, ss, Rss, xn, Rxn, pT, RpT, ident, Rid = tmp
    S.op("act", lambda e: e.activation(out=junk[:], in_=xt, func=AF.Square, accum_out=ss[:]),
         reads=[Rxt], writes=[Rjunk, Rss])
    rstd_ops(S, ss, Rss)
    S.op("act", lambda e: e.activation(out=xn[:], in_=xt, func=AF.Copy, scale=ss[:, 0:1]),
         reads=[Rxt, Rss], writes=[Rxn])
    for k in range(8):
        S.op("pe", lambda e, k=k: e.transpose(pT[:, k, :], xn[:, k * 128:(k + 1) * 128], ident[:]),
             reads=[Rxn, Rid], writes=[RpT])
    S.op("dve", lambda e: e.tensor_tensor(out=hT, in0=pT[:], in1=gain.unsqueeze(2).to_broadcast([128, 8, 128]),
                                          op=ALU.mult), reads=[RpT, Rg], writes=[RhT])


def make_ident(S, C, dt=BF16):
    idf, Ridf = C.sb([128, 128], F32)
    S.op("pool", lambda e: e.memset(idf[:], 0.0), writes=[Ridf])
    S.op("pool", lambda e: e.affine_select(out=idf[:], in_=idf[:], pattern=[[-1, 128]], compare_op=ALU.not_equal,
                                           fill=1.0, base=0, channel_multiplier=1), reads=[Ridf], writes=[Ridf])
    idb, Ridb = C.sb([128, 128], BF16)
    S.op("dve", lambda e: e.tensor_copy(out=idb[:], in_=idf[:]), reads=[Ridf], writes=[Ridb])
    return idf, Ridf, idb, Ridb


def phase_ffn(nc, S, src, dst, Dm, l, final):
    with ExitStack() as st:
        C = Ctx(nc, S, st)
        S.barrier()
        wg, _ = C.sb([128, 8, FF], BF16, "wg")
        wu, _ = C.sb([128, 8, FF], BF16, "wu")
        wd, _ = C.sb([128, 22, D], BF16, "wd")
        Rwg = [S.res() for _ in range(8)]
        Rwu = [S.res() for _ in range(8)]
        Rwd = [S.res() for _ in range(22)]
        gain, Rg = C.sb([128, 8], F32, "gain")
        S.dma("sp", lambda e: e.dma_start(out=gain[:], in_=Dm[f"g_ffn{l}"]), writes=[Rg])
        load_weight(S, "pool", wg, Dm[f"ffn_gate{l}"], 8, Rwg)
        load_weight(S, "pool", wu, Dm[f"ffn_up{l}"], 8, Rwu)
        load_weight(S, "pool", wd, Dm[f"ffn_down{l}"], 22, Rwd)
        if final:
            gfin, Rgfin = C.sb([128, D], F32, "gfin")
            S.dma("sp", lambda e: e.dma_start(out=gfin[:], in_=Dm["g_final"][0, :].partition_broadcast(128)),
                  writes=[Rgfin])
        idf, Ridf, idb, Ridb = make_ident(S, C)
        TT = 256
        xts = [C.sb([128, 2, D], F32) for _ in range(2)]
        hTs = [C.sb([128, 8, TT], BF16) for _ in range(2)]
        actT, RactT = C.sb([128, 22, TT], BF16, "actT")
        junk, Rjunk = C.sb([128, D], BF16, "junk")
        ss, Rss = C.sb([128, 1], F32, "ss")
        xn, Rxn = C.sb([128, D], BF16, "xn")
        sgs = [C.sb([128, TT], F32) for _ in range(2)]
        pTs = [C.ps([128, 8, 128], BF16) for _ in range(2)]
        pgu = [C.ps([128, 2, TT], F32) for _ in range(2)]
        pso = [C.ps([128, 512], F32) for _ in range(2)]
        npt = 0
        for it in range(NTOK // TT):
            tok0 = it * TT
            xt, Rxt = xts[it % 2]
            hT, RhT = hTs[it % 2]
            S.dma("sp", lambda e, xt=xt, tok0=tok0: e.dma_start(
                out=xt[:], in_=src[tok0:tok0 + TT, :].rearrange("(s p) d -> p s d", p=128)), writes=[Rxt])
            for s in range(2):
                pT, RpT = pTs[npt % 2]
                npt += 1
                norm_T(S, xt[:, s, :], Rxt, gain[:], Rg, hT[:, :, s * 128:(s + 1) * 128], RhT,
                       (junk, Rjunk, ss, Rss, xn, Rxn, pT, RpT, idb, Ridb))
            for j in range(22):
                pg, Rpg = pgu[j % 2]
                sg, Rsg = sgs[j % 2]
                for k in range(8):
                    S.op("pe", lambda e, k=k, j=j, pg=pg, hT=hT: e.matmul(
                        pg[:, 0, :], lhsT=wg[:, k, j * 128:(j + 1) * 128], rhs=hT[:, k, :], start=(k == 0), stop=(k == 7)),
                        reads=[Rwg[k], RhT], writes=[Rpg])
                for k in range(8):
                    S.op("pe", lambda e, k=k, j=j, pg=pg, hT=hT: e.matmul(
                        pg[:, 1, :], lhsT=wu[:, k, j * 128:(j + 1) * 128], rhs=hT[:, k, :], start=(k == 0), stop=(k == 7)),
                        reads=[Rwu[k], RhT], writes=[Rpg])
                S.op("act", lambda e, pg=pg, sg=sg: e.activation(out=sg[:], in_=pg[:, 0, :], func=AF.Silu),
                     reads=[Rpg], writes=[Rsg])
                S.op("dve", lambda e, pg=pg, sg=sg, j=j: e.tensor_tensor(out=actT[:, j, :], in0=pg[:, 1, :], in1=sg[:],
                                                                        op=ALU.mult),
                     reads=[Rpg, Rsg], writes=[RactT])
            n = 0
            for s in range(2):
                for dh in range(2):
                    po, Rpo = pso[n % 2]
                    n += 1
                    for j in range(22):
                        S.op("pe", lambda e, j=j, s=s, dh=dh, po=po: e.matmul(
                            po[:], lhsT=actT[:, j, s * 128:(s + 1) * 128], rhs=wd[:, j, dh * 512:(dh + 1) * 512],
                            start=(j == 0), stop=(j == 21)), reads=[RactT, Rwd[j]], writes=[Rpo])
                    S.op("dve", lambda e, s=s, dh=dh, po=po, xt=xt: e.tensor_tensor(
                        out=xt[:, s, dh * 512:(dh + 1) * 512], in0=po[:], in1=xt[:, s, dh * 512:(dh + 1) * 512],
                        op=ALU.add), reads=[Rpo, Rxt], writes=[Rxt])
            if final:
                for s in range(2):
                    S.op("act", lambda e, s=s, xt=xt: e.activation(out=junk[:], in_=xt[:, s, :], func=AF.Square,
                                                                   accum_out=ss[:]), reads=[Rxt], writes=[Rjunk, Rss])
                    rstd_ops(S, ss, Rss)
                    S.op("act", lambda e, s=s, xt=xt: e.activation(out=xt[:, s, :], in_=xt[:, s, :], func=AF.Copy,
                                                                   scale=ss[:, 0:1]), reads=[Rxt, Rss], writes=[Rxt])
                    S.op("dve", lambda e, s=s, xt=xt: e.tensor_tensor(out=xt[:, s, :], in0=xt[:, s, :], in1=gfin[:],
                                                                      op=ALU.mult), reads=[Rxt, Rgfin], writes=[Rxt])
            S.dma("pool", lambda e, xt=xt, tok0=tok0: e.dma_start(
                out=dst[tok0:tok0 + TT, :].rearrange("(s p) d -> p s d", p=128), in_=xt[:]), reads=[Rxt], writes=[])
        S.flush()


def fm(v):
    v = np.asarray(v, np.float32).reshape(-1, 128)
    return np.ascontiguousarray(v.T)


def build(phases):
    nc = bass.Bass("TRN2", target_bir_lowering=False)
    Dm = {}

    def inp(name, shape):
        Dm[name] = nc.dram_tensor(name, list(shape), F32, kind="ExternalInput").ap()

    inp("x", [NTOK, D])
    for l in range(2):
        if f"F{l}" in phases:
            inp(f"ffn_gate{l}", [D, FF]); inp(f"ffn_up{l}", [D, FF]); inp(f"ffn_down{l}", [FF, D])
            inp(f"g_ffn{l}", [128, 8])
    if "F1" in phases:
        inp("g_final", [1, D])
    if "L0" in phases:
        inp("w_in0", [D, 2816]); inp("w_out0", [D, D]); inp("p0", [128, NP0]); inp("lora0", [128, 512])
        inp("w_g_up0", [128, 512]); inp("gates_bd0", [128, 8, 128]); inp("lnx0", [2, 512]); inp("c_masks0", [128, 770])
    if "L1" in phases:
        inp("w1", [D, W1C]); inp("wo1", [64, 16, D]); inp("g_mix1", [128, 8])
        inp("c_cos", [128, SEQ]); inp("c_sin", [128, SEQ]); inp("c_pm", [128, 128])
        inp("c_negmask", [128, 128]); inp("c_pow2", [128, NIT + 1])
    out = nc.dram_tensor("out", [NTOK, D], F32, kind="ExternalOutput").ap()
    scr = [nc.dram_tensor(f"scr{i}", [NTOK, D], F32).ap() for i in range(2)]
    with ExitStack() as st:
        S = Sched(nc, st)
        cur = Dm["x"]
        for i, ph in enumerate(phases):
            dst = out if i == len(phases) - 1 else scr[i % 2]
            if ph in ("F0", "F1"):
                phase_ffn(nc, S, cur, dst, Dm, int(ph[1]), ph == "F1")
            elif ph == "L1":
                phase_dsa(nc, S, cur, dst, Dm)
            elif ph == "L0":
                phase_l0(nc, S, cur, dst, Dm)
            cur = dst
        S.barrier()
        S.prog["sp"].append(([], None, None))
        S.flush(final=True)
    return nc


def host_inputs(inputs, phases):
    x = np.asarray(inputs["x"], np.float32)
    shared = {}
    for l in range(2):
        if f"F{l}" in phases:
            shared[f"ffn_gate{l}"] = np.ascontiguousarray(inputs["ffn_gate"][l], dtype=np.float32)
            shared[f"ffn_up{l}"] = np.ascontiguousarray(inputs["ffn_up"][l], dtype=np.float32)
            shared[f"ffn_down{l}"] = np.ascontiguousarray(inputs["ffn_down"][l], dtype=np.float32)
            shared[f"g_ffn{l}"] = fm(inputs["norm_ffn"][l])
    if "F1" in phases:
        shared["g_final"] = np.asarray(inputs["norm_final"], np.float32).reshape(1, D)
    if "L0" in phases:
        shared.update(l0_host(inputs))
    if "L1" in phases:
        w = np.asarray(inputs["w_in1"][0], np.float32)
        q, k, v, iq, ik, iw = np.split(w, np.cumsum([1024, 64, 64, 512, 64])[:].tolist(), axis=1)
        shared["w1"] = np.ascontiguousarray(np.concatenate([q, iq, k, k, ik, ik, v, iw], axis=1))
        shared["wo1"] = np.ascontiguousarray(np.asarray(inputs["w_out1"][0], np.float32).reshape(16, 64, D).transpose(1, 0, 2))
        shared["g_mix1"] = fm(inputs["norm_mix"][1])
        shared.update(dsa_consts())
    maps = []
    for c in range(8):
        m = dict(shared)
        m["x"] = np.ascontiguousarray(x[c * NSEQ:(c + 1) * NSEQ].reshape(NTOK, D))
        maps.append(m)
    return maps


PHASES = ("L0", "F0", "L1", "F1")
DBG = {"nseq": NSEQ, "qbs": None, "d1_tiles": None, "l0_tiles": None, "cores": 8}


def kernel(phases=PHASES, **inputs):
    nc = build(phases)
    maps = host_inputs(inputs, phases)
    ncores = DBG["cores"]
    res = run_bass_kernel_spmd(nc, maps[:ncores], core_ids=list(range(ncores)))
    outs = [np.asarray(r["out"], np.float32).reshape(NSEQ, SEQ, D) for r in res.results]
    outs += [np.zeros((NSEQ, SEQ, D), np.float32)] * (8 - ncores)
    return np.concatenate(outs, axis=0)


W1C = 1864
NIT = 24
TOPK = 256


def phase_dsa(nc, S, src, dst, Dm):
    with ExitStack() as st:
        C = Ctx(nc, S, st)
        S.barrier()
        w1, _ = C.sb([128, 8, W1C], BF16, "w1")
        Rw1 = [S.res() for _ in range(8)]
        load_weight(S, "pool", w1, Dm["w1"], 8, Rw1)
        wo, Rwo = C.sb([64, 16, D], BF16, "wo")
        for hh_ in range(16):
            S.dma("pool", lambda e, hh_=hh_: e.dma_start(out=wo[:, hh_, :], in_=Dm["wo1"][:, hh_, :]), writes=[Rwo])
        gain, Rg = C.sb([128, 8], F32, "gain")
        S.dma("sp", lambda e: e.dma_start(out=gain[:], in_=Dm["g_mix1"]), writes=[Rg])
        pm_, Rpm = C.sb([128, 128], BF16, "Pm")
        S.dma("pool", lambda e: e.dma_start(out=pm_[:], in_=Dm["c_pm"]), writes=[Rpm])
        negm, Rnegm = C.sb([128, 128], F32, "negm")
        S.dma("sp", lambda e: e.dma_start(out=negm[:], in_=Dm["c_negmask"]), writes=[Rnegm])
        pow2, Rpow2 = C.sb([128, NIT + 1], F32, "pow2")
        S.dma("sp", lambda e: e.dma_start(out=pow2[:], in_=Dm["c_pow2"]), writes=[Rpow2])
        idf, Ridf, idb, Ridb = make_ident(S, C)
        onesf, Rones = C.sb([128, 64], F32, "onesf")
        S.op("pool", lambda e: e.memset(onesf[:], 1.0), writes=[Rones])
        thrc, Rthrc = C.sb([128, 1], F32, "thrc")
        S.op("pool", lambda e: e.memset(thrc[:], -1e29), writes=[Rthrc])

        qT, RqT = C.sb([128, 8, SEQ], BF16, "qT")
        iqT, RiqT = C.sb([128, 4, SEQ], BF16, "iqT")
        kT2, RkT2 = C.sb([128, SEQ], BF16, "kT2")
        ikT2, RikT2 = C.sb([128, SEQ], BF16, "ikT2")
        Vaug, RV = C.sb([128, 16, 65], BF16, "Vaug")
        iw, Riw = C.sb([128, 16, 8], F32, "iw")
        S.op("pool", lambda e: e.memset(Vaug[:], 1.0), writes=[RV])

        T1 = 256
        xts = [C.sb([128, 2, D], F32) for _ in range(1)]
        hT, RhT = C.sb([128, 8, T1], BF16, "hT")
        junk, Rjunk = C.sb([128, D], BF16, "junk")
        ss, Rss = C.sb([128, 1], F32, "ss")
        xn, Rxn = C.sb([128, D], BF16, "xn")
        pbs = [C.sb([128, T1], BF16) for _ in range(2)]
        t1s = [C.sb([128, T1], F32) for _ in range(2)]
        t2s = [C.sb([128, T1], F32) for _ in range(2)]
        css = [C.sb([128, 2, T1], F32) for _ in range(2)]
        scs = [C.sb([128, SEQ], F32) for _ in range(2)]
        score, Rscore = scs[0]
        jcnt, Rjcnt = C.sb([128, SEQ], BF16, "jcnt")
        maskTs = [C.sb([128, 16, 128], BF16) for _ in range(2)]
        maskb, Rmaskb = C.sb([128, SEQ], BF16, "maskb")
        rls = [C.sb([128, 512], F32) for _ in range(2)]
        Es = [C.sb([128, 4, 128], BF16) for _ in range(2)]
        PTs = [C.sb([128, 4, 128], BF16) for _ in range(2)]
        rs, Rrs = C.sb([128, 512], F32, "rs")
        bc, Rbc = C.sb([64, 512], F32, "bc")
        attnT = [C.sb([64, 4, 128], BF16) for _ in range(4)]
        xres, Rxres = C.sb([128, D], F32, "xres")
        bis, Rbis = C.sb([128, 8], F32, "bis")
        steps, Rsteps = C.sb([128, NIT + 1], F32, "steps")
        cnts, Rcnts = C.sb([128, NIT], F32, "cnts")
        pT, RpT = C.ps([128, 8, 128], BF16, "pT")
        B = [C.ps([128, 4, 128], F32) for _ in range(7)]

        for sq in range(DBG["nseq"]):
            base = sq * SEQ
            for tt in range(DBG["d1_tiles"] or SEQ // T1):
                t0 = tt * T1
                xt, Rxt = xts[0]
                cs, Rcs = css[tt % 2]
                S.dma("sp", lambda e, xt=xt, a=base + t0: e.dma_start(
                    out=xt[:], in_=src[a:a + T1, :].rearrange("(s p) d -> p s d", p=128)), writes=[Rxt])
                S.dma("sp", lambda e, cs=cs, t0=t0: e.dma_start(out=cs[:, 0, :], in_=Dm["c_cos"][:, t0:t0 + T1]),
                      writes=[Rcs])
                S.dma("sp", lambda e, cs=cs, t0=t0: e.dma_start(out=cs[:, 1, :], in_=Dm["c_sin"][:, t0:t0 + T1]),
                      writes=[Rcs])
                for s in range(T1 // 128):
                    norm_T(S, xt[:, s, :], Rxt, gain[:], Rg, hT[:, :, s * 128:(s + 1) * 128], RhT,
                           (junk, Rjunk, ss, Rss, xn, Rxn, pT, RpT, idb, Ridb))
                for nt in range(DBG.get("d1_nt", 14)):
                    pp, Rpp = B[nt % 2]
                    pr, Rpr = B[2 + nt % 2]
                    pb, Rpb = pbs[nt % 2]
                    ta, Rta = t1s[nt % 2]
                    tb, Rtb = t2s[nt % 2]
                    ppv = pp[:].rearrange("p a b -> p (a b)")[:, 0:T1]
                    prv = pr[:].rearrange("p a b -> p (a b)")[:, 0:T1]
                    for k in range(8):
                        S.op("pe", lambda e, k=k, nt=nt, ppv=ppv: e.matmul(
                            ppv, lhsT=w1[:, k, nt * 128:(nt + 1) * 128], rhs=hT[:, k, :], start=(k == 0), stop=(k == 7)),
                            reads=[Rw1[k], RhT], writes=[Rpp])
                    lvl = DBG.get("d1_ops", 9)
                    if lvl < 2:
                        continue
                    S.op("act", lambda e, pb=pb, ppv=ppv: e.copy(out=pb[:], in_=ppv), reads=[Rpp], writes=[Rpb])
                    if lvl < 3:
                        continue
                    S.op("pe", lambda e, pb=pb, prv=prv: e.matmul(prv, lhsT=pm_[:], rhs=pb[:], start=True, stop=True),
                         reads=[Rpm, Rpb], writes=[Rpr])
                    if lvl < 4:
                        continue
                    if DBG.get("tt", 3) & 1:
                        S.op("dve", lambda e, ta=ta, ppv=ppv, cs=cs: e.tensor_tensor(out=ta[:], in0=ppv, in1=cs[:, 0, :],
                                                                                    op=ALU.mult),
                             reads=[Rpp, Rcs, Rpb], writes=[Rta])
                    if DBG.get("tt", 3) & 2:
                        S.op("dve", lambda e, tb=tb, prv=prv, cs=cs: e.tensor_tensor(out=tb[:], in0=prv, in1=cs[:, 1, :],
                                                                                    op=ALU.mult),
                             reads=[Rpr, Rcs], writes=[Rtb])
                    if DBG.get("tt", 3) & 4:
                        S.op("dve", lambda e, ta=ta, ppv=ppv, cs=cs: e.tensor_tensor(out=ta[:], in0=ppv, in1=ta[:],
                                                                                    op=ALU.mult),
                             reads=[Rpp, Rcs], writes=[Rta])
                    if lvl < 5:
                        continue
                    if nt < 8:
                        dest, Rd = qT[:, nt, t0:t0 + T1], RqT
                    elif nt < 12:
                        dest, Rd = iqT[:, nt - 8, t0:t0 + T1], RiqT
                    elif nt == 12:
                        dest, Rd = kT2[:, t0:t0 + T1], RkT2
                    else:
                        dest, Rd = ikT2[:, t0:t0 + T1], RikT2
                    S.op(DBG.get("addeng", "pool"), lambda e, dest=dest, ta=ta, tb=tb: e.tensor_tensor(out=dest, in0=ta[:], in1=tb[:],
                                                                                   op=ALU.add),
                         reads=[Rta, Rtb], writes=[Rd])
                for s in range(DBG.get("d1_v", T1 // 128)):
                    blk = t0 // 128 + s
                    pv, Rpv = B[4]
                    pvv = pv[:].rearrange("p a b -> p (a b)")[:, 0:72]
                    for k in range(8):
                        S.op("pe", lambda e, k=k, s=s, pvv=pvv: e.matmul(
                            pvv, lhsT=hT[:, k, s * 128:(s + 1) * 128], rhs=w1[:, k, 1792:1864], start=(k == 0),
                            stop=(k == 7)), reads=[Rw1[k], RhT], writes=[Rpv])
                    S.op("act", lambda e, blk=blk, pvv=pvv: e.copy(out=Vaug[:, blk, 0:64], in_=pvv[:, 0:64]),
                         reads=[Rpv], writes=[RV])
                    S.op("dve", lambda e, blk=blk, pvv=pvv: e.tensor_scalar(
                        out=iw[:, blk, :], in0=pvv[:, 64:72], scalar1=1.0 / (8.0 * 8.0 ** 0.5), scalar2=None,
                        op0=ALU.mult), reads=[Rpv], writes=[Riw])

            if DBG.get("dump_d1"):
                dd = [(qT[:, 0, 0:512], 0, 0, 512), (kT2[:, 0:512], 0, 512, 512), (iqT[:, 0, 0:512], 128, 0, 512),
                      (ikT2[:, 0:512], 128, 512, 512), (Vaug[:, 0:4, :].rearrange("p a b -> p (a b)"), 256, 0, 260),
                      (iw[:, 0:4, :].rearrange("p a b -> p (a b)"), 256, 512, 32)]
                for ap_, r0, c0_, w_ in dd:
                    S.dma("pool", lambda e, ap_=ap_, r0=r0, c0_=c0_, w_=w_: e.dma_start(
                        out=dst[r0:r0 + 128, c0_:c0_ + w_], in_=ap_), reads=[RqT, RkT2, RiqT, RikT2, RV, Riw], writes=[])
            def idx_items(qb, pos):
                nk = (qb + 1) * 128
                q0 = qb * 128
                sc, Rsc = scs[pos % 2]
                pi, Rpi = B[6]
                items = []
                n = 0
                for h in range(8):
                    pr_ = (h % 2) * 64
                    for c0 in range(0, nk, 512):
                        w = min(512, nk - c0)
                        rl, Rrl = rls[n % 2]
                        n += 1

                        def it(h=h, pr_=pr_, c0=c0, w=w, rl=rl, Rrl=Rrl):
                            piv = pi[:].rearrange("p a b -> p (a b)")[:, 0:w]
                            S.op("pe", lambda e: e.matmul(
                                piv, lhsT=iqT[pr_:pr_ + 64, h // 2, q0:q0 + 128], rhs=ikT2[pr_:pr_ + 64, c0:c0 + w],
                                start=True, stop=True), reads=[RiqT, RikT2], writes=[Rpi])
                            S.op("act", lambda e: e.activation(out=rl[:, 0:w], in_=piv, func=AF.Relu),
                                 reads=[Rpi], writes=[Rrl])
                            if h == 0:
                                S.op("dve", lambda e: e.tensor_scalar(
                                    out=sc[:, c0:c0 + w], in0=rl[:, 0:w], scalar1=iw[:, qb, 0:1], scalar2=None,
                                    op0=ALU.mult), reads=[Rrl, Riw], writes=[Rsc])
                            else:
                                S.op("dve", lambda e: e.scalar_tensor_tensor(
                                    out=sc[:, c0:c0 + w], in0=rl[:, 0:w], scalar=iw[:, qb, h:h + 1],
                                    in1=sc[:, c0:c0 + w], op0=ALU.mult, op1=ALU.add), reads=[Rrl, Riw, Rsc],
                                    writes=[Rsc])
                        items.append(it)
                return items

            def bis_items(qb, pos):
                nk = (qb + 1) * 128
                q0 = qb * 128
                sc, Rsc = scs[pos % 2]
                items = []

                def pre():
                    S.op("dve", lambda e: e.tensor_tensor(out=sc[:, q0:nk], in0=sc[:, q0:nk], in1=negm[:], op=ALU.add),
                         reads=[Rsc, Rnegm], writes=[Rsc])
                    if qb >= 2:
                        S.op("dve", lambda e: e.tensor_reduce(out=bis[:, 0:1], in_=sc[:, 0:nk], axis=AX.X, op=ALU.max),
                             reads=[Rsc], writes=[Rbis])
                        S.op("dve", lambda e: e.tensor_reduce(out=bis[:, 1:2], in_=sc[:, 0:nk - 128], axis=AX.X,
                                                              op=ALU.min), reads=[Rsc], writes=[Rbis])
                        S.op("dve", lambda e: e.tensor_tensor(out=bis[:, 2:3], in0=bis[:, 0:1], in1=bis[:, 1:2],
                                                              op=ALU.subtract), reads=[Rbis], writes=[Rbis])
                        S.op("dve", lambda e: e.tensor_scalar(out=steps[:], in0=pow2[:], scalar1=bis[:, 2:3],
                                                              scalar2=None, op0=ALU.mult), reads=[Rbis, Rpow2],
                             writes=[Rsteps])
                        S.op("pool", lambda e: e.memset(cnts[:], 0.0), writes=[Rcnts])
                        S.op("dve", lambda e: e.tensor_tensor(out=bis[:, 3:4], in0=bis[:, 1:2], in1=steps[:, 0:1],
                                                              op=ALU.add), reads=[Rbis, Rsteps], writes=[Rbis])
                items.append(pre)
                if qb >= 2:
                    for k in range(NIT):
                        def itk(k=k):
                            S.op("dve", lambda e: e.tensor_scalar(
                                out=jcnt[:, 0:nk], in0=sc[:, 0:nk], scalar1=bis[:, 3:4], scalar2=0.0, op0=ALU.is_ge,
                                op1=ALU.add, accum_out=cnts[:, k:k + 1]), reads=[Rsc, Rbis, Rcnts],
                                writes=[Rjcnt, Rcnts])
                            S.op("dve", lambda e: e.scalar_tensor_tensor(
                                out=bis[:, 4:5], in0=cnts[:, k:k + 1], scalar=float(TOPK), in1=steps[:, k:k + 1],
                                op0=ALU.is_ge, op1=ALU.mult), reads=[Rcnts, Rsteps], writes=[Rbis])
                            S.op("dve", lambda e: e.scalar_tensor_tensor(
                                out=bis[:, 3:4], in0=bis[:, 3:4], scalar=steps[:, k + 1:k + 2], in1=bis[:, 4:5],
                                op0=ALU.subtract, op1=ALU.add), reads=[Rbis, Rsteps], writes=[Rbis])
                        items.append(itk)

                def post():
                    if qb >= 2:
                        S.op("dve", lambda e: e.tensor_tensor(out=bis[:, 1:2], in0=bis[:, 3:4],
                                                              in1=steps[:, NIT:NIT + 1], op=ALU.subtract),
                             reads=[Rbis, Rsteps], writes=[Rbis])
                        thr, Rthr = bis[:, 1:2], Rbis
                    else:
                        thr, Rthr = thrc[:, 0:1], Rthrc
                    S.op("dve", lambda e: e.tensor_scalar(out=maskb[:, 0:nk], in0=sc[:, 0:nk], scalar1=thr, scalar2=None,
                                                          op0=ALU.is_ge), reads=[Rsc, Rthr], writes=[Rmaskb])
                items.append(post)
                return items

            def stage_maskT(qb, pos):
                mT, RmT = maskTs[pos % 2]
                for jb in range(0, qb + 1, 8):
                    nb = min(8, qb + 1 - jb)
                    for i in range(nb):
                        S.op("pe", lambda e: e.transpose(pT[:, i, :], maskb[:, (jb + i) * 128:(jb + i + 1) * 128], idb[:]),
                             reads=[Rmaskb, Ridb], writes=[RpT])
                    S.op("act", lambda e: e.copy(out=mT[:, jb:jb + nb, :], in_=pT[:, 0:nb, :]), reads=[RpT], writes=[RmT])

            def attn_items(qb, pos):
                q0 = qb * 128
                mT, RmT = maskTs[pos % 2]
                items = []
                n = 0
                for j in range(qb + 1):
                    for g in range(4):
                        items.append((j, g, n))
                        n += 1

                def mk(j, g, n):
                    def it():
                        par, half = g % 2, g // 2
                        pst, Rpst = B[4 + n % 2]
                        E, RE = Es[n % 2]
                        PT, RPT = PTs[n % 2]
                        S.op("pe", lambda e: e.matmul(
                            pst[:], lhsT=kT2[par * 64:par * 64 + 64, j * 128:(j + 1) * 128],
                            rhs=qT[par * 64:par * 64 + 64, 4 * half:4 * half + 4, q0:q0 + 128], start=True, stop=True),
                            reads=[RkT2, RqT], writes=[Rpst])
                        S.op("act", lambda e: e.activation(out=E[:], in_=pst[:], func=AF.Exp, scale=0.125),
                             reads=[Rpst], writes=[RE])
                        S.op("pool", lambda e: e.tensor_tensor(
                            out=PT[:], in0=E[:], in1=mT[:, j:j + 1, :].to_broadcast([128, 4, 128]), op=ALU.mult),
                            reads=[RE, RmT], writes=[RPT])
                        ot, Rot = B[g]
                        S.op("pe", lambda e: e.matmul(
                            ot[0:65], lhsT=Vaug[:, j, :], rhs=PT[:], start=(j == 0), stop=(j == qb)),
                            reads=[RV, RPT], writes=[Rot])
                    return it
                return [mk(j, g, n) for (j, g, n) in items]

            def interleave(lists):
                lists = [l for l in lists if l]
                if not lists:
                    return
                L = max(len(l) for l in lists)
                pos = [0] * len(lists)
                for s_ in range(L):
                    for li, l in enumerate(lists):
                        tgt = ((s_ + 1) * len(l)) // L
                        while pos[li] < tgt:
                            l[pos[li]]()
                            pos[li] += 1

            def stage_out(qb):
                q0 = qb * 128
                S.dma("sp", lambda e: e.dma_start(out=xres[:], in_=src[base + q0:base + q0 + 128, :]), writes=[Rxres])
                for g in range(4):
                    ot, Rot = B[g]
                    otv = ot[:].rearrange("p a b -> p (a b)")
                    pbc, Rpbc = B[6]
                    pbcv = pbc[:].rearrange("p a b -> p (a b)")
                    at, Rat = attnT[g]
                    S.op("dve", lambda e: e.reciprocal(out=rs[64:65, :], in_=otv[64:65, :]), reads=[Rot], writes=[Rrs])
                    S.op("pe", lambda e: e.matmul(pbcv[0:64, :], lhsT=onesf[64:65, 0:64], rhs=rs[64:65, :],
                                                  start=True, stop=True), reads=[Rones, Rrs], writes=[Rpbc])
                    S.op("act", lambda e: e.copy(out=bc[:], in_=pbcv[0:64, :]), reads=[Rpbc], writes=[Rbc])
                    S.op("dve", lambda e: e.tensor_tensor(
                        out=at[:].rearrange("p a b -> p (a b)"), in0=otv[0:64, :], in1=bc[:], op=ALU.mult),
                        reads=[Rot, Rbc], writes=[Rat])
                for dh in range(2):
                    po, Rpo = B[6]
                    pov = po[:].rearrange("p a b -> p (a b)")
                    n2 = 0
                    for g in range(4):
                        par, half = g % 2, g // 2
                        at, Rat = attnT[g]
                        for i in range(4):
                            hh = 2 * (4 * half + i) + par
                            S.op("pe", lambda e: e.matmul(
                                pov, lhsT=at[:, i, :], rhs=wo[:, hh, dh * 512:(dh + 1) * 512], start=(n2 == 0),
                                stop=(n2 == 15)), reads=[Rat, Rwo], writes=[Rpo])
                            n2 += 1
                    S.op("dve", lambda e: e.tensor_tensor(
                        out=xres[:, dh * 512:(dh + 1) * 512], in0=pov, in1=xres[:, dh * 512:(dh + 1) * 512], op=ALU.add),
                        reads=[Rpo, Rxres], writes=[Rxres])
                S.dma("pool", lambda e: e.dma_start(out=dst[base + q0:base + q0 + 128, :], in_=xres[:]), reads=[Rxres],
                      writes=[])

            qbl = list(DBG["qbs"]) if DBG["qbs"] is not None else list(range(SEQ // 128))
            nq = len(qbl)
            if nq:
                interleave([idx_items(qbl[0], 0)])
                interleave([bis_items(qbl[0], 0)])
                stage_maskT(qbl[0], 0)
            if nq > 1:
                interleave([idx_items(qbl[1], 1)])
            for ii, qb in enumerate(qbl):
                interleave([attn_items(qb, ii),
                            idx_items(qbl[ii + 2], ii + 2) if ii + 2 < nq else [],
                            bis_items(qbl[ii + 1], ii + 1) if ii + 1 < nq else []])
                if ii + 1 < nq:
                    stage_maskT(qbl[ii + 1], ii + 1)
                stage_out(qb)
        S.flush()


def dsa_consts():
    inv = (10000.0 ** (-(np.arange(0, 64, 2, dtype=np.float32)) / np.float32(64))).astype(np.float32)
    ang = (np.arange(SEQ, dtype=np.float32)[:, None] * inv[None, :]).astype(np.float32)
    cos, sin = np.cos(ang).astype(np.float32), np.sin(ang).astype(np.float32)
    p = np.arange(128)
    d = p % 64
    c_cos = np.ascontiguousarray(cos[:, d % 32].T)
    sgn = np.where(d < 32, -1.0, 1.0).astype(np.float32)
    c_sin = np.ascontiguousarray((sin[:, d % 32] * sgn[None, :]).T.astype(np.float32))
    pm = np.zeros((128, 128), np.float32)
    partner = (d + 32) % 64 + 64 * (p // 64)
    pm[p, partner] = 1.0
    negmask = np.zeros((128, 128), np.float32)
    negmask[:64, 64:] = -1e30
    pow2 = np.tile((2.0 ** -(1.0 + np.arange(NIT + 1))).astype(np.float32)[None, :], (128, 1))
    return {"c_cos": c_cos, "c_sin": c_sin, "c_pm": pm, "c_negmask": negmask, "c_pow2": np.ascontiguousarray(pow2)}


TL = 256
NCH = TL // 128
CDEC = float(np.exp(-0.5))
P_MU, P_W0, P_AB, P_KK, P_KA, P_RK, P_CB, P_BRG, P_BIG, P_LAM, P_CW, P_G = 0, 14, 18, 22, 26, 30, 34, 38, 42, 46, 50, 66
NP0 = 74


def phase_l0(nc, S, src, dst, Dm):
    with ExitStack() as st:
        C = Ctx(nc, S, st)
        S.barrier()
        win, _ = C.sb([128, 8, 2816], BF16, "win")
        Rwin = [S.res() for _ in range(8)]
        load_weight(S, "pool", win, Dm["w_in0"], 8, Rwin)
        wo, _ = C.sb([128, 8, D], BF16, "wo0")
        Rwo = [S.res() for _ in range(8)]
        load_weight(S, "pool", wo, Dm["w_out0"], 8, Rwo)
        P0, RP0 = C.sb([128, NP0], F32, "P0")
        S.dma("sp", lambda e: e.dma_start(out=P0[:], in_=Dm["p0"]), writes=[RP0])
        lora, Rlora = C.sb([128, 512], BF16, "lora")
        S.dma("pool", lambda e: e.dma_start(out=lora[:], in_=Dm["lora0"]), writes=[Rlora])
        wgu, Rwgu = C.sb([128, 512], BF16, "wgu")
        S.dma("pool", lambda e: e.dma_start(out=wgu[:], in_=Dm["w_g_up0"]), writes=[Rwgu])
        gbd, Rgbd = C.sb([128, 8, 128], BF16, "gbd")
        S.dma("pool", lambda e: e.dma_start(out=gbd[:], in_=Dm["gates_bd0"]), writes=[Rgbd])
        lnw, Rlnw = C.sb([128, 2, 512], F32, "lnw")
        S.dma("sp", lambda e: e.dma_start(out=lnw[:, 0, :], in_=Dm["lnx0"][0, :].partition_broadcast(128)), writes=[Rlnw])
        S.dma("sp", lambda e: e.dma_start(out=lnw[:, 1, :], in_=Dm["lnx0"][1, :].partition_broadcast(128)), writes=[Rlnw])
        cm, Rcm = C.sb([128, 128 + 256 + 256 + 128 + 2], BF16, "cm")
        S.dma("pool", lambda e: e.dma_start(out=cm[:], in_=Dm["c_masks0"]), writes=[Rcm])
        maskL, maskU2, maskU3, BD, IND = cm[:, 0:128], cm[:, 128:384], cm[:, 384:640], cm[:, 640:768], cm[:, 768:770]
        idf, Ridf, idb, Ridb = make_ident(S, C)
        onesT, Rones = C.sb([128, 128], F32, "onesT")
        S.op("pool", lambda e: e.memset(onesT[:], 1.0), writes=[Rones])
        spt, Rspt = C.sb([128, 16], F32, "spt")
        S.op("act", lambda e: e.activation(out=spt[:, 0:4], in_=P0[:, P_LAM:P_LAM + 4], func=AF.Exp, scale=-1.0),
             reads=[RP0], writes=[Rspt])
        S.op("act", lambda e: e.activation(out=spt[:, 0:4], in_=spt[:, 0:4], func=AF.Ln, bias=1.0), reads=[Rspt],
             writes=[Rspt])
        for i, m in ((1, 8.0), (2, -8.0), (3, -16.0)):
            S.op("dve", lambda e, i=i, m=m: e.tensor_scalar(out=spt[:, 4 * i:4 * i + 4], in0=spt[:, 0:4], scalar1=m,
                                                            scalar2=None, op0=ALU.mult), reads=[Rspt], writes=[Rspt])

        xts = [C.sb([128, NCH, D], F32) for _ in range(2)]
        hT, RhT = C.sb([128, 8, TL], BF16, "hT")
        junk, Rjunk = C.sb([128, D], BF16, "junk")
        ss, Rss = C.sb([128, 1], F32, "ss")
        xn, Rxn = C.sb([128, D], BF16, "xn")
        Pb = [C.sb([128, TL + 1], F32) for _ in range(14)]
        LX = [C.sb([128, TL + 3], F32) for _ in range(4)]
        hprev, Rhprev = C.sb([128, 4], F32, "hprev")
        tmps = {}

        def tmp(name, dt=F32, shape=None):
            if name not in tmps:
                tmps[name] = C.sb(shape or [128, TL], dt, "tmp_" + name)
            return tmps[name]

        ART = [C.sb([128, NCH, 2, 128], BF16) for _ in range(4)]
        BTt = [C.sb([128, TL], BF16) for _ in range(4)]
        KTt = [C.sb([128, TL], BF16) for _ in range(4)]
        BTm = [C.sb([128, 2, TL], BF16) for _ in range(4)]
        KTm = [C.sb([128, 2, TL], BF16) for _ in range(4)]
        Hm = [C.sb([128, 2, 64], BF16) for _ in range(4)]
        hmk, Rhmk = C.sb([128, 2], F32, "hmk")
        S.op("pool", lambda e: e.memset(hmk[:], 0.0), writes=[Rhmk])
        S.op("pool", lambda e: e.memset(hmk[0:64, 0:1], 1.0), writes=[Rhmk])
        S.op("pool", lambda e: e.memset(hmk[64:128, 1:2], 1.0), writes=[Rhmk])
        TM = [C.sb([128, 3, 4, 128], BF16) for _ in range(NCH)]
        WL, RWL = C.sb([128, 4, NCH], F32, "WL")
        Hs = [C.sb([128, 64], F32) for _ in range(4)]
        Hb = [C.sb([128, 64], BF16) for _ in range(4)]
        mixT, RmixT = C.sb([128, 8, TL], BF16, "mixT")
        bons, Rbons = C.sb([128, NCH, 8], F32, "bons")
        pT, RpT = C.ps([128, 8, 128], BF16, "pT")
        PP, RPP_ = C.ps([128, 2, TL], F32, "PP")
        RPP = [S.res(bank=RPP_.bank), S.res(bank=RPP_.bank)]
        BKA, RBKA_ = C.ps([128, 2, 256], F32, "BKA")
        BKB, RBKB_ = C.ps([128, 2, 256], F32, "BKB")
        RBK = [S.res(bank=RBKA_.bank), S.res(bank=RBKA_.bank), S.res(bank=RBKB_.bank), S.res(bank=RBKB_.bank)]
        slots = [BKA[:, 0, :], BKA[:, 1, :], BKB[:, 0, :], BKB[:, 1, :]]
        BKC, RBKC_ = C.ps([128, 4, 128], F32, "BKC")
        RBKC = [S.res(bank=RBKC_.bank), S.res(bank=RBKC_.bank)]
        BKD, RBKD_ = C.ps([128, 512], F32, "BKD")
        RX, RU, RdH, Rbon = [S.res(bank=RBKD_.bank) for _ in range(4)]
        YT, RYT = C.ps([128, 8, 64], F32, "YT")
        GP, RGP = C.ps([128, 512], F32, "GP")
        nslot = [0]

        def slot():
            i = nslot[0] % 4
            nslot[0] += 1
            return slots[i], RBK[i]

        def PV(col, n=4):
            return P0[:, col:col + 1] if n == 1 else P0[:, col:col + n]

        for sq in range(DBG["nseq"]):
            base = sq * SEQ
            for i in range(14):
                S.op("pool", lambda e, i=i: e.memset(Pb[i][0][:, 0:1], 0.0), writes=[Pb[i][1]])
            for i in range(4):
                S.op("pool", lambda e, i=i: e.memset(LX[i][0][:, 0:3], 0.0), writes=[LX[i][1]])
                S.op("pool", lambda e, i=i: e.memset(Hs[i][0][:], 0.0), writes=[Hs[i][1]])
                S.op("pool", lambda e, i=i: e.memset(Hb[i][0][:], 0.0), writes=[Hb[i][1]])
                S.op("pool", lambda e, i=i: e.memset(Hm[i][0][:], 0.0), writes=[Hm[i][1]])
            S.op("pool", lambda e: e.memset(hprev[:], 0.0), writes=[Rhprev])
            for tt in range(DBG["l0_tiles"] or SEQ // TL):
                t0 = tt * TL
                xt, Rxt = xts[tt % 2]
                S.dma("sp", lambda e, xt=xt, a=base + t0: e.dma_start(
                    out=xt[:], in_=src[a:a + TL, :].rearrange("(s p) d -> p s d", p=128)), writes=[Rxt])
                for s in range(NCH):
                    norm_T(S, xt[:, s, :], Rxt, P0[:, P_G:P_G + 8], RP0, hT[:, :, s * 128:(s + 1) * 128], RhT,
                           (junk, Rjunk, ss, Rss, xn, Rxn, pT, RpT, idb, Ridb))
                npp = [0]

                def proj(nt):
                    i = npp[0] % 2
                    npp[0] += 1
                    for k in range(8):
                        S.op("pe", lambda e, k=k, nt=nt, i=i: e.matmul(
                            PP[:, i, :], lhsT=win[:, k, nt * 128:(nt + 1) * 128], rhs=hT[:, k, :], start=(k == 0),
                            stop=(k == 7)), reads=[Rwin[k], RhT], writes=[RPP[i]])
                    return PP[:, i, :], RPP[i]

                def shift(nt, name):
                    pp, Rpp = proj(nt)
                    Pt, RPt = Pb[nt]
                    o, Ro = tmp(name)
                    d_, Rd_ = tmp("shd")
                    S.op("act", lambda e: e.copy(out=Pt[:, 1:TL + 1], in_=pp), reads=[Rpp], writes=[RPt])
                    S.op("dve", lambda e: e.tensor_tensor(out=d_[:], in0=Pt[:, 0:TL], in1=Pt[:, 1:TL + 1], op=ALU.subtract),
                         reads=[RPt], writes=[Rd_])
                    S.op("dve", lambda e: e.scalar_tensor_tensor(out=o[:], in0=d_[:], scalar=P0[:, P_MU + nt:P_MU + nt + 1],
                                                                 in1=Pt[:, 1:TL + 1], op0=ALU.mult, op1=ALU.add),
                         reads=[Rd_, RPt, RP0], writes=[Ro])
                    S.op("act", lambda e: e.copy(out=Pt[:, 0:1], in_=Pt[:, TL:TL + 1]), reads=[RPt], writes=[RPt])
                    return o, Ro

                LV = DBG.get('l0_lvl', 9)
                if LV < 2:
                    continue
                swa, Rswa = shift(12, "s_wa")
                lor, Rlor = tmp("lor", BF16)
                S.op("act", lambda e: e.activation(out=lor[0:64, :], in_=swa[0:64, :], func=AF.Tanh), reads=[Rswa],
                     writes=[Rlor])
                S.op("act", lambda e: e.copy(out=lor[64:128, :], in_=swa[64:128, :]), reads=[Rswa], writes=[Rlor])
                sgd_, Rsgd_ = shift(13, "s_gd")
                sgd, Rsgd = tmp("sgd", BF16)
                S.op("act", lambda e: e.activation(out=sgd[:], in_=sgd_[:], func=AF.Sigmoid), reads=[Rsgd_], writes=[Rsgd])

                if LV < 3:
                    continue
                for j in range(4):
                    r_, Rr = shift(j, "s_r")
                    k_, Rk = shift(4 + j, "s_k")
                    v_, Rv = shift(8 + j, "s_v")
                    art, Rart = ART[j]
                    bt, Rbt = BTt[j]
                    kt, Rkt = KTt[j]
                    zw, Rzw = slot()
                    S.op("pe", lambda e, j=j, zw=zw: e.matmul(zw, lhsT=lora[0:64, j * 128:(j + 1) * 128], rhs=lor[0:64, :],
                                                              start=True, stop=True), reads=[Rlora, Rlor], writes=[Rzw])
                    ee, Ree = tmp("ee")
                    S.op("act", lambda e, j=j, zw=zw: e.activation(out=ee[:], in_=zw, func=AF.Sigmoid,
                                                                  bias=P0[:, P_W0 + j:P_W0 + j + 1]),
                         reads=[Rzw, RP0], writes=[Ree])
                    za, Rza = slot()
                    S.op("pe", lambda e, j=j, za=za: e.matmul(za, lhsT=lora[64:128, j * 128:(j + 1) * 128],
                                                              rhs=lor[64:128, :], start=True, stop=True),
                         reads=[Rlora, Rlor], writes=[Rza])
                    aa, Raa = tmp("aa")
                    S.op("act", lambda e, j=j, za=za: e.activation(out=aa[:], in_=za, func=AF.Sigmoid,
                                                                  bias=P0[:, P_AB + j:P_AB + j + 1]),
                         reads=[Rza, RP0], writes=[Raa])
                    cs, Rcs = tmp("cs")
                    for c in range(NCH):
                        S.op("dve", lambda e, c=c: e.tensor_tensor_scan(
                            out=cs[:, c * 128:(c + 1) * 128], data0=onesT[:], data1=ee[:, c * 128:(c + 1) * 128],
                            initial=0.0, op0=ALU.mult, op1=ALU.add), reads=[Rones, Ree], writes=[Rcs])
                    csm, Rcsm = tmp("csm")
                    S.op("dve", lambda e: e.tensor_tensor(out=csm[:], in0=cs[:], in1=ee[:], op=ALU.subtract),
                         reads=[Rcs, Ree], writes=[Rcsm])
                    einv, Reinv = tmp("einv")
                    edec, Redec = tmp("edec")
                    eprev, Reprev = tmp("eprev")
                    S.op("act", lambda e: e.activation(out=einv[:], in_=cs[:], func=AF.Exp, scale=CDEC), reads=[Rcs],
                         writes=[Reinv])
                    S.op("act", lambda e: e.activation(out=edec[:], in_=cs[:], func=AF.Exp, scale=-CDEC), reads=[Rcs],
                         writes=[Redec])
                    S.op("act", lambda e: e.activation(out=eprev[:], in_=csm[:], func=AF.Exp, scale=-CDEC), reads=[Rcsm],
                         writes=[Reprev])
                    S.op("dve", lambda e, j=j: e.tensor_copy(
                        out=WL[:, j, :], in_=edec[:].rearrange("p (c t) -> p c t", t=128)[:, :, 127]), reads=[Redec],
                        writes=[RWL])
                    kk, Rkk = tmp("kk")
                    S.op("dve", lambda e, j=j: e.tensor_scalar(out=kk[:], in0=k_[:], scalar1=P0[:, P_KK + j:P_KK + j + 1],
                                                               scalar2=None, op0=ALU.mult), reads=[Rk, RP0], writes=[Rkk])
                    kk2, Rkk2 = tmp("kk2", BF16)
                    S.op("pool", lambda e: e.tensor_tensor(out=kk2[:], in0=kk[:], in1=kk[:], op=ALU.mult), reads=[Rkk],
                         writes=[Rkk2])
                    zs, Rzs = slot()
                    S.op("pe", lambda e, zs=zs: e.matmul(zs, lhsT=BD, rhs=kk2[:], start=True, stop=True),
                         reads=[Rcm, Rkk2], writes=[Rzs])
                    rn, Rrn = tmp("rn")
                    S.op("act", lambda e, zs=zs: e.activation(out=rn[:], in_=zs, func=AF.Ln, bias=1e-24), reads=[Rzs],
                         writes=[Rrn])
                    S.op("act", lambda e: e.activation(out=rn[:], in_=rn[:], func=AF.Exp, scale=-0.5), reads=[Rrn],
                         writes=[Rrn])
                    S.op("dve", lambda e: e.tensor_tensor(out=kk[:], in0=kk[:], in1=rn[:], op=ALU.mult), reads=[Rkk, Rrn],
                         writes=[Rkk])
                    km, Rkm = tmp("km")
                    S.op("dve", lambda e, j=j: e.tensor_scalar(out=km[:], in0=aa[:], scalar1=-1.0,
                                                               scalar2=P0[:, P_KA + j:P_KA + j + 1], op0=ALU.add,
                                                               op1=ALU.mult), reads=[Raa, RP0], writes=[Rkm])
                    S.op("dve", lambda e: e.scalar_tensor_tensor(out=km[:], in0=km[:], scalar=1.0, in1=k_[:], op0=ALU.add,
                                                                 op1=ALU.mult), reads=[Rkm, Rk], writes=[Rkm])
                    S.op("dve", lambda e, art=art: e.tensor_tensor(
                        out=art[:, :, 0, :], in0=kk[:].rearrange("p (c t) -> p c t", t=128),
                        in1=eprev[:].rearrange("p (c t) -> p c t", t=128), op=ALU.mult), reads=[Rkk, Reprev], writes=[Rart])
                    S.op("pool", lambda e, art=art: e.tensor_tensor(
                        out=art[:, :, 1, :], in0=r_[:].rearrange("p (c t) -> p c t", t=128),
                        in1=edec[:].rearrange("p (c t) -> p c t", t=128), op=ALU.mult), reads=[Rr, Redec], writes=[Rart])
                    tb, Rtb = tmp("tb")
                    S.op("pool", lambda e: e.tensor_tensor(out=tb[:], in0=kk[:], in1=aa[:], op=ALU.mult), reads=[Rkk, Raa],
                         writes=[Rtb])
                    S.op("dve", lambda e, bt=bt: e.tensor_tensor(out=bt[:], in0=tb[:], in1=einv[:], op=ALU.mult),
                         reads=[Rtb, Reinv], writes=[Rbt])
                    S.op("pool", lambda e, kt=kt: e.tensor_tensor(out=kt[:], in0=km[:], in1=einv[:], op=ALU.mult),
                         reads=[Rkm, Reinv], writes=[Rkt])
                    btm, Rbtm = BTm[j]
                    ktm, Rktm = KTm[j]
                    for h in range(2):
                        S.op("dve", lambda e, h=h, btm=btm, bt=bt: e.tensor_scalar(
                            out=btm[:, h, :], in0=bt[:], scalar1=hmk[:, h:h + 1], scalar2=None, op0=ALU.mult),
                            reads=[Rbt, Rhmk], writes=[Rbtm])
                        S.op("pool", lambda e, h=h, ktm=ktm, kt=kt: e.tensor_scalar(
                            out=ktm[:, h, :], in0=kt[:], scalar1=hmk[:, h:h + 1], scalar2=None, op0=ALU.mult),
                            reads=[Rkt, Rhmk], writes=[Rktm])
                    rkr, Rrkr = tmp("rkr", BF16)
                    S.op("dve", lambda e, j=j: e.scalar_tensor_tensor(out=rkr[:], in0=r_[:],
                                                                      scalar=P0[:, P_RK + j:P_RK + j + 1], in1=km[:],
                                                                      op0=ALU.mult, op1=ALU.mult), reads=[Rr, Rkm, RP0],
                         writes=[Rrkr])
                    for c in range(NCH):
                        S.op("pe", lambda e, c=c, j=j: e.matmul(BKD[:, 384 + c * 8 + 2 * j:384 + c * 8 + 2 * j + 2],
                                                                lhsT=rkr[:, c * 128:(c + 1) * 128], rhs=IND, start=True,
                                                                stop=True), reads=[Rrkr, Rcm], writes=[Rbon])
                    vb, Rvb = tmp("vb", BF16)
                    S.op("act", lambda e: e.copy(out=vb[:], in_=v_[:]), reads=[Rv], writes=[Rvb])
                    for c in range(NCH):
                        tm, Rtm = TM[c]
                        for i, (srct, Rs) in enumerate(((kt, Rkt), (bt, Rbt), (vb, Rvb))):
                            S.op("pe", lambda e, i=i, c=c, srct=srct: e.transpose(
                                pT[:, i, :], srct[:, c * 128:(c + 1) * 128], idb[:]), reads=[Rs, Ridb], writes=[RpT])
                        S.op("act", lambda e, tm=tm, j=j: e.copy(out=tm[:, :, j, :], in_=pT[:, 0:3, :]), reads=[RpT],
                             writes=[Rtm])
                S.op("act", lambda e: e.copy(out=bons[:].rearrange("p c h -> p (c h)"), in_=BKD[:, 384:384 + NCH * 8]),
                     reads=[Rbon], writes=[Rbons])

                if DBG.get("dummy_alloc"):
                    for j_ in range(4):
                        tmp(f"M0_{j_}", BF16, [128, 2, 128]); tmp(f"NB_{j_}", BF16, [128, 2, 256]); tmp(f"NK_{j_}", BF16, [128, 2, 256])
                if LV < 4:
                    continue
                for c in range(NCH):
                    tm, Rtm = TM[c]
                    for j in range(4):
                        art, Rart = ART[j]
                        bt, Rbt = BTt[j]
                        kt, Rkt = KTt[j]
                        Hf, RHf = Hs[j]
                        Hh, RHh = Hm[j]
                        btm, Rbtm = BTm[j]
                        ktm, Rktm = KTm[j]
                        M0, RM0 = tmp(f"M0_{j}", BF16, [128, 2, 128])
                        NB, RNB = tmp(f"NB_{j}", BF16, [128, 2, 256])
                        NK, RNK = tmp(f"NK_{j}", BF16, [128, 2, 256])
                        pa, Rpa = BKC[:, 0:2, :], RBKC[0]
                        for h in range(2):
                            po = h * 64
                            v_ = DBG.get("p1var", 0)
                            if v_ == 3:
                                if c == 0 and j == 0 and h == 0:
                                    S.op("pe", lambda e, h=h, po=po, art=art, bt=bt, c=c: e.matmul(
                                        GP[:, 0:128], lhsT=bt[0:64, 0:128], rhs=bt[0:64, 0:128],
                                        start=True, stop=True), reads=[Rbt], writes=[RGP])
                                continue
                            if v_ in (5, 6, 7):
                                if (v_ == 5 and c == 0) or (v_ == 6 and j == 0) or (v_ == 7 and h == 0):
                                    S.op("pe", lambda e, h=h, po=po, art=art, bt=bt, c=c: e.matmul(
                                        GP[:, 0:128], lhsT=bt[po:po + 64, 0:128], rhs=bt[po:po + 64, 0:128],
                                        start=True, stop=True), reads=[Rbt], writes=[RGP])
                                continue
                            if v_ == 4:
                                if c == 0 and j == 0 and h == 0:
                                    S.op("pe", lambda e, h=h, po=po, art=art, bt=bt, c=c: e.matmul(
                                        GP[:, 0:128], lhsT=idb[:], rhs=idb[:],
                                        start=True, stop=True), reads=[Ridb], writes=[RGP])
                                continue
                            if v_ == 1:
                                S.op("pe", lambda e, h=h, po=po, art=art, bt=bt, c=c: e.matmul(
                                    BKC[:, h, :], lhsT=bt[po:po + 64, c * 128:(c + 1) * 128], rhs=bt[po:po + 64, c * 128:(c + 1) * 128],
                                    start=True, stop=True), reads=[Rbt], writes=[Rpa])
                                continue
                            if v_ == 2:
                                S.op("pe", lambda e, h=h, po=po, art=art, bt=bt, c=c: e.matmul(
                                    GP[:, h * 128:(h + 1) * 128], lhsT=art[po:po + 64, c, 0, :], rhs=bt[po:po + 64, c * 128:(c + 1) * 128],
                                    start=True, stop=True), reads=[Rart, Rbt], writes=[RGP])
                                continue
                            S.op("pe", lambda e, h=h, po=po, art=art, btm=btm, c=c: e.matmul(
                                BKC[:, h, :], lhsT=art[:, c, 0, :], rhs=btm[:, h, c * 128:(c + 1) * 128],
                                start=True, stop=True), reads=[Rart, Rbtm], writes=[Rpa])
                        if DBG.get('l0_n', 9) < 1:
                            continue
                        S.op("dve", lambda e, M0=M0: e.scalar_tensor_tensor(
                            out=M0[:], in0=BKC[:, 0:2, :], scalar=-1.0, in1=maskL.unsqueeze(1).to_broadcast([128, 2, 128]),
                            op0=ALU.mult, op1=ALU.mult), reads=[Rpa, Rcm], writes=[RM0])
                        if DBG.get('l0_n', 9) < 2:
                            continue
                        for h in range(2):
                            po = h * 64
                            S.op("pe", lambda e, h=h, po=po, art=art, btm=btm, c=c: e.matmul(
                                BKA[:, h, :], lhsT=btm[:, h, c * 128:(c + 1) * 128],
                                rhs=art[:, c, :, :].rearrange("p a t -> p (a t)"), start=True, stop=True),
                                reads=[Rart, Rbtm], writes=[RBK[0], RBK[1]])
                        if DBG.get('l0_n', 9) < 3:
                            continue
                        S.op("dve", lambda e, NB=NB: e.tensor_tensor(
                            out=NB[:], in0=BKA[:], in1=maskU2.unsqueeze(1).to_broadcast([128, 2, 256]), op=ALU.mult),
                            reads=[RBK[0], RBK[1], Rcm], writes=[RNB])
                        for h in range(2):
                            po = h * 64
                            S.op("pe", lambda e, h=h, po=po, art=art, ktm=ktm, c=c: e.matmul(
                                BKB[:, h, :], lhsT=ktm[:, h, c * 128:(c + 1) * 128],
                                rhs=art[:, c, :, :].rearrange("p a t -> p (a t)"), start=True, stop=True),
                                reads=[Rart, Rktm], writes=[RBK[2], RBK[3]])
                        S.op("dve", lambda e, NK=NK: e.tensor_tensor(
                            out=NK[:], in0=BKB[:], in1=maskU3.unsqueeze(1).to_broadcast([128, 2, 256]), op=ALU.mult),
                            reads=[RBK[2], RBK[3], Rcm], writes=[RNK])
                        SL = DBG.get('l0_sub', 9)
                        if SL < 2:
                            continue
                        Q, RQ = tmp(f"Q_{j}", BF16, [128, 2, 128])
                        S.op("pool", lambda e, Q=Q, NB=NB: e.tensor_tensor(
                            out=Q[:], in0=NB[:, :, 0:128], in1=idb[:].unsqueeze(1).to_broadcast([128, 2, 128]), op=ALU.add),
                            reads=[RNB, Ridb], writes=[RQ])
                        Mc, RMc = M0, RM0
                        McT, RMcT = NB[:, :, 0:128], RNB
                        for i in range(1, 7):
                            Mn, RMn = tmp(f"M{i % 2}_{j}", BF16, [128, 2, 128])
                            for h in range(2):
                                S.op("pe", lambda e, h=h, Mc=Mc, McT=McT: e.matmul(
                                    BKC[:, 2 + h, :], lhsT=McT[:, h, :], rhs=Mc[:, h, :], start=True, stop=True),
                                    reads=[RMc, RMcT], writes=[RBKC[1]])
                            S.op("act", lambda e, Mn=Mn: e.copy(out=Mn[:], in_=BKC[:, 2:4, :]), reads=[RBKC[1]],
                                 writes=[RMn])
                            if i < 6:
                                MnT, RMnT = tmp(f"MT{i % 2}_{j}", BF16, [128, 2, 128])
                                for h in range(2):
                                    S.op("pe", lambda e, h=h, Mc=Mc, McT=McT: e.matmul(
                                        BKC[:, h, :], lhsT=Mc[:, h, :], rhs=McT[:, h, :], start=True, stop=True),
                                        reads=[RMc, RMcT], writes=[RBKC[0]])
                                S.op("act", lambda e, MnT=MnT: e.copy(out=MnT[:], in_=BKC[:, 0:2, :]), reads=[RBKC[0]],
                                     writes=[RMnT])
                            qs, Rqs = slot()
                            qsv = qs.rearrange("p (a t) -> p a t", t=128)
                            for h in range(2):
                                S.op("pe", lambda e, h=h, Mn=Mn, Q=Q, qsv=qsv: e.matmul(
                                    qsv[:, h, :], lhsT=Mn[:, h, :], rhs=Q[:, h, :], start=True, stop=True),
                                    reads=[RMn, RQ], writes=[Rqs])
                            S.op("dve", lambda e, Q=Q, qsv=qsv: e.tensor_tensor(out=Q[:], in0=qsv, in1=Q[:], op=ALU.add),
                                 reads=[Rqs, RQ], writes=[RQ])
                            Mc, RMc = Mn, RMn
                            if i < 6:
                                McT, RMcT = MnT[:], RMnT
                        if SL < 3:
                            continue
                        Xb, RXb = tmp(f"Xb_{j}", BF16, [128, 2, 64])
                        Un, RUn = tmp(f"Un_{j}", BF16, [128, 2, 64])
                        for h in range(2):
                            po = h * 64
                            S.op("pe", lambda e, h=h, po=po, art=art, Hh=Hh, c=c: e.matmul(
                                BKD[:, h * 64:(h + 1) * 64], lhsT=art[:, c, 0, :], rhs=Hh[:, h, :],
                                start=True, stop=False), reads=[Rart, RHh], writes=[RX])
                            S.op("pe", lambda e, h=h, po=po, NK=NK, tm=tm, j=j: e.matmul(
                                BKD[:, h * 64:(h + 1) * 64], lhsT=NK[:, h, 0:128], rhs=tm[:, 2, j, po:po + 64],
                                start=False, stop=True), reads=[RNK, Rtm], writes=[RX])
                        S.op("act", lambda e, Xb=Xb: e.copy(out=Xb[:].rearrange("p a v -> p (a v)"), in_=BKD[:, 0:128]),
                             reads=[RX], writes=[RXb])
                        for h in range(2):
                            S.op("pe", lambda e, h=h, Q=Q, Xb=Xb: e.matmul(
                                BKD[:, 128 + h * 64:128 + (h + 1) * 64], lhsT=Q[:, h, :], rhs=Xb[:, h, :], start=True,
                                stop=True), reads=[RQ, RXb], writes=[RU])
                        S.op("act", lambda e, Un=Un: e.mul(out=Un[:].rearrange("p a v -> p (a v)"), in_=BKD[:, 128:256],
                                                           mul=-1.0), reads=[RU], writes=[RUn])
                        if SL < 4:
                            continue
                        for h in range(2):
                            po = h * 64
                            hd = 2 * j + h
                            S.op("pe", lambda e, h=h, po=po, hd=hd, art=art, Hh=Hh, c=c: e.matmul(
                                YT[:, hd, :], lhsT=art[:, c, 1, :], rhs=Hh[:, h, :], start=True, stop=False),
                                reads=[Rart, RHh], writes=[RYT])
                            S.op("pe", lambda e, h=h, po=po, hd=hd, NK=NK, tm=tm, j=j: e.matmul(
                                YT[:, hd, :], lhsT=NK[:, h, 128:256], rhs=tm[:, 2, j, po:po + 64], start=False, stop=False),
                                reads=[RNK, Rtm], writes=[RYT])
                            S.op("pe", lambda e, h=h, hd=hd, NB=NB, Un=Un: e.matmul(
                                YT[:, hd, :], lhsT=NB[:, h, 128:256], rhs=Un[:, h, :], start=False, stop=True),
                                reads=[RNB, RUn], writes=[RYT])
                        if SL < 5:
                            continue
                        for h in range(2):
                            po = h * 64
                            S.op("pe", lambda e, h=h, po=po, tm=tm, j=j: e.matmul(
                                BKD[:, 256 + h * 64:256 + (h + 1) * 64], lhsT=tm[:, 0, j, :], rhs=tm[:, 2, j, po:po + 64],
                                start=True, stop=False), reads=[Rtm], writes=[RdH])
                            S.op("pe", lambda e, h=h, tm=tm, j=j, Un=Un: e.matmul(
                                BKD[:, 256 + h * 64:256 + (h + 1) * 64], lhsT=tm[:, 1, j, :], rhs=Un[:, h, :], start=False,
                                stop=True), reads=[Rtm, RUn], writes=[RdH])
                        for h in range(2):
                            po = h * 64
                            S.op("dve", lambda e, h=h, po=po, Hf=Hf: e.tensor_tensor(
                                out=Hf[po:po + 64, :], in0=BKD[po:po + 64, 256 + h * 64:256 + (h + 1) * 64],
                                in1=Hf[po:po + 64, :], op=ALU.add), reads=[RdH, RHf], writes=[RHf])
                        S.op("dve", lambda e, Hf=Hf, j=j, c=c: e.tensor_scalar(out=Hf[:], in0=Hf[:], scalar1=WL[:, j, c:c + 1],
                                                                               scalar2=None, op0=ALU.mult),
                             reads=[RHf, RWL], writes=[RHf])
                        for h in range(2):
                            S.op("pool", lambda e, h=h, Hf=Hf, Hh=Hh: e.tensor_scalar(
                                out=Hh[:, h, :], in0=Hf[:], scalar1=hmk[:, h:h + 1], scalar2=None, op0=ALU.mult),
                                reads=[RHf, Rhmk], writes=[RHh])

                    if LV < 5:
                        continue
                    ysb, Rysb = tmp("ysb", F32, [128, 8, 64])
                    st8, Rst8 = tmp("st8", F32, [128, 16])
                    S.op("act", lambda e: e.copy(out=ysb[:], in_=YT[:]), reads=[RYT], writes=[Rysb])
                    S.op("dve", lambda e: e.tensor_reduce(out=st8[:, 0:8], in_=ysb[:], axis=AX.X, op=ALU.add), reads=[Rysb],
                         writes=[Rst8])
                    S.op("dve", lambda e: e.tensor_scalar(out=st8[:, 0:8], in0=st8[:, 0:8], scalar1=1.0 / 64, scalar2=None,
                                                          op0=ALU.mult), reads=[Rst8], writes=[Rst8])
                    S.op("dve", lambda e: e.tensor_tensor(out=ysb[:], in0=ysb[:],
                                                          in1=st8[:, 0:8].unsqueeze(2).to_broadcast([128, 8, 64]),
                                                          op=ALU.subtract), reads=[Rysb, Rst8], writes=[Rysb])
                    ysq, Rysq = tmp("ysq", F32, [128, 8, 64])
                    S.op("pool", lambda e: e.tensor_tensor(out=ysq[:], in0=ysb[:], in1=ysb[:], op=ALU.mult), reads=[Rysb],
                         writes=[Rysq])
                    S.op("dve", lambda e: e.tensor_reduce(out=st8[:, 8:16], in_=ysq[:], axis=AX.X, op=ALU.add), reads=[Rysq],
                         writes=[Rst8])
                    sv, Rsv = tmp("sv", F32, [128, 8])
                    S.op("dve", lambda e: e.tensor_copy(out=sv[:], in_=st8[:, 8:16]), reads=[Rst8], writes=[Rsv])
                    rstd_ops(S, sv, Rsv, n=64, eps=64e-5)
                    S.op("dve", lambda e: e.tensor_tensor(out=ysb[:], in0=ysb[:],
                                                          in1=sv[:].unsqueeze(2).to_broadcast([128, 8, 64]), op=ALU.mult),
                         reads=[Rysb, Rsv], writes=[Rysb])
                    yf = ysb[:].rearrange("p h v -> p (h v)")
                    S.op("dve", lambda e: e.tensor_tensor(out=yf, in0=yf, in1=lnw[:, 0, :], op=ALU.mult), reads=[Rysb, Rlnw],
                         writes=[Rysb])
                    S.op("pool", lambda e: e.tensor_tensor(out=yf, in0=yf, in1=lnw[:, 1, :], op=ALU.add), reads=[Rysb, Rlnw],
                         writes=[Rysb])
                    bv, Rbv = tmp("bv", F32, [128, 8, 64])
                    S.op("pool", lambda e, tm=tm, c=c: e.tensor_tensor(
                        out=bv[:], in0=tm[:, 2, :, :].rearrange("p j (h v) -> p (j h) v", v=64),
                        in1=bons[:, c, :].unsqueeze(2).to_broadcast([128, 8, 64]), op=ALU.mult), reads=[Rtm, Rbons],
                        writes=[Rbv])
                    S.op("dve", lambda e: e.tensor_tensor(out=ysb[:], in0=ysb[:], in1=bv[:], op=ALU.add), reads=[Rysb, Rbv],
                         writes=[Rysb])
                    S.op("pe", lambda e, c=c: e.matmul(GP[:], lhsT=sgd[:, c * 128:(c + 1) * 128], rhs=wgu[:], start=True,
                                                       stop=True), reads=[Rsgd, Rwgu], writes=[RGP])
                    rwb, Rrwb = tmp("rwb", BF16, [128, 512])
                    S.op("dve", lambda e: e.tensor_tensor(out=rwb[:], in0=GP[:], in1=yf, op=ALU.mult), reads=[RGP, Rysb],
                         writes=[Rrwb])
                    for jj in range(4):
                        S.op("pe", lambda e, jj=jj: e.transpose(pT[:, 4 + jj, :], rwb[:, jj * 128:(jj + 1) * 128], idb[:]),
                             reads=[Rrwb, Ridb], writes=[RpT])
                    S.op("act", lambda e, c=c: e.copy(out=mixT[:, 0:4, c * 128:(c + 1) * 128], in_=pT[:, 4:8, :]),
                         reads=[RpT], writes=[RmixT])

                if LV < 6:
                    continue
                for jt in range(4):
                    lx, Rlx = LX[jt]
                    pp, Rpp = proj(14 + jt)
                    S.op("act", lambda e, lx=lx, pp=pp: e.copy(out=lx[:, 3:TL + 3], in_=pp), reads=[Rpp], writes=[Rlx])
                    xc, Rxc = tmp("ee")
                    cw = P_CW + 4 * jt
                    S.op("dve", lambda e, lx=lx, cw=cw, jt=jt: e.tensor_scalar(
                        out=xc[:], in0=lx[:, 3:TL + 3], scalar1=P0[:, cw + 3:cw + 4], scalar2=P0[:, P_CB + jt:P_CB + jt + 1],
                        op0=ALU.mult, op1=ALU.add), reads=[Rlx, RP0], writes=[Rxc])
                    for i in range(3):
                        S.op("dve", lambda e, lx=lx, cw=cw, i=i: e.scalar_tensor_tensor(
                            out=xc[:], in0=lx[:, i:TL + i], scalar=P0[:, cw + i:cw + i + 1], in1=xc[:], op0=ALU.mult,
                            op1=ALU.add), reads=[Rlx, RP0, Rxc], writes=[Rxc])
                    S.op("act", lambda e, lx=lx: e.copy(out=lx[:, 0:3], in_=lx[:, TL:TL + 3]), reads=[Rlx], writes=[Rlx])
                    xcb, Rxcb = tmp("kk2", BF16)
                    S.op("act", lambda e: e.copy(out=xcb[:], in_=xc[:]), reads=[Rxc], writes=[Rxcb])
                    zr, Rzr = slot()
                    S.op("pe", lambda e, zr=zr, jt=jt: e.matmul(zr, lhsT=gbd[:, jt, :], rhs=xcb[:], start=True, stop=True),
                         reads=[Rgbd, Rxcb], writes=[Rzr])
                    zi, Rzi = slot()
                    S.op("pe", lambda e, zi=zi, jt=jt: e.matmul(zi, lhsT=gbd[:, 4 + jt, :], rhs=xcb[:], start=True,
                                                                stop=True), reads=[Rgbd, Rxcb], writes=[Rzi])
                    rg, Rrg = tmp("aa")
                    ig, Rig = tmp("cs")
                    S.op("act", lambda e, zr=zr, jt=jt: e.activation(out=rg[:], in_=zr, func=AF.Sigmoid,
                                                                    bias=P0[:, P_BRG + jt:P_BRG + jt + 1]),
                         reads=[Rzr, RP0], writes=[Rrg])
                    S.op("act", lambda e, zi=zi, jt=jt: e.activation(out=ig[:], in_=zi, func=AF.Sigmoid,
                                                                    bias=P0[:, P_BIG + jt:P_BIG + jt + 1]),
                         reads=[Rzi, RP0], writes=[Rig])
                    th, Rth = tmp("csm")
                    a2, Ra2 = tmp("einv")
                    at_, Rat_ = tmp("edec")
                    S.op("act", lambda e, jt=jt: e.activation(out=th[:], in_=rg[:], func=AF.Tanh,
                                                             scale=spt[:, 4 + jt:5 + jt]), reads=[Rrg, Rspt], writes=[Rth])
                    S.op("act", lambda e, jt=jt: e.activation(out=a2[:], in_=rg[:], func=AF.Exp,
                                                             scale=spt[:, 12 + jt:13 + jt]), reads=[Rrg, Rspt], writes=[Ra2])
                    S.op("act", lambda e, jt=jt: e.activation(out=at_[:], in_=rg[:], func=AF.Exp,
                                                             scale=spt[:, 8 + jt:9 + jt]), reads=[Rrg, Rspt], writes=[Rat_])
                    S.op("dve", lambda e: e.scalar_tensor_tensor(out=a2[:], in0=a2[:], scalar=1.0, in1=th[:], op0=ALU.add,
                                                                 op1=ALU.mult), reads=[Ra2, Rth], writes=[Ra2])
                    S.op("act", lambda e: e.activation(out=a2[:], in_=a2[:], func=AF.Sqrt), reads=[Ra2], writes=[Ra2])
                    S.op("pool", lambda e: e.tensor_tensor(out=xc[:], in0=xc[:], in1=ig[:], op=ALU.mult), reads=[Rxc, Rig],
                         writes=[Rxc])
                    S.op("dve", lambda e: e.tensor_tensor(out=xc[:], in0=xc[:], in1=a2[:], op=ALU.mult), reads=[Rxc, Ra2],
                         writes=[Rxc])
                    hs, Rhs = tmp("eprev")
                    S.op("dve", lambda e, jt=jt: e.tensor_tensor_scan(out=hs[:], data0=at_[:], data1=xc[:],
                                                                      initial=hprev[:, jt:jt + 1], op0=ALU.mult,
                                                                      op1=ALU.add), reads=[Rat_, Rxc, Rhprev], writes=[Rhs])
                    S.op("dve", lambda e, jt=jt: e.tensor_copy(out=hprev[:, jt:jt + 1], in_=hs[:, TL - 1:TL]), reads=[Rhs],
                         writes=[Rhprev])
                    pg, Rpg = proj(18 + jt)
                    lg, Rlg = tmp("kk")
                    sqg, Rsqg = tmp("rn")
                    S.op("act", lambda e, pg=pg: e.copy(out=lg[:], in_=pg), reads=[Rpg], writes=[Rlg])
                    S.op("act", lambda e, pg=pg: e.activation(out=sqg[:], in_=pg, func=AF.Square), reads=[Rpg], writes=[Rsqg])
                    S.op("dve", lambda e: e.tensor_scalar(out=sqg[:], in0=sqg[:], scalar1=0.044715, scalar2=1.0, op0=ALU.mult,
                                                          op1=ALU.add), reads=[Rsqg], writes=[Rsqg])
                    S.op("pool", lambda e: e.tensor_tensor(out=sqg[:], in0=sqg[:], in1=lg[:], op=ALU.mult), reads=[Rsqg, Rlg],
                         writes=[Rsqg])
                    S.op("act", lambda e: e.activation(out=sqg[:], in_=sqg[:], func=AF.Sigmoid, scale=1.5957691216),
                         reads=[Rsqg], writes=[Rsqg])
                    S.op("pool", lambda e: e.tensor_tensor(out=sqg[:], in0=sqg[:], in1=lg[:], op=ALU.mult), reads=[Rsqg, Rlg],
                         writes=[Rsqg])
                    S.op("dve", lambda e, jt=jt: e.tensor_tensor(out=mixT[:, 4 + jt, :], in0=sqg[:], in1=hs[:], op=ALU.mult),
                         reads=[Rsqg, Rhs], writes=[RmixT])

                if LV < 7:
                    continue
                n = 0
                for s in range(NCH):
                    for dh in range(2):
                        po_, Rpo_ = slot()
                        pov = po_
                        for kc in range(8):
                            S.op("pe", lambda e, kc=kc, s=s, dh=dh: e.matmul(
                                GP[:], lhsT=mixT[:, kc, s * 128:(s + 1) * 128], rhs=wo[:, kc, dh * 512:(dh + 1) * 512],
                                start=(kc == 0), stop=(kc == 7)), reads=[RmixT, Rwo[kc]], writes=[RGP])
                        S.op("dve", lambda e, s=s, dh=dh, xt=xt: e.tensor_tensor(
                            out=xt[:, s, dh * 512:(dh + 1) * 512], in0=GP[:], in1=xt[:, s, dh * 512:(dh + 1) * 512],
                            op=ALU.add), reads=[RGP, Rxt], writes=[Rxt])
                S.dma("pool", lambda e, xt=xt, a=base + t0: e.dma_start(
                    out=dst[a:a + TL, :].rearrange("(s p) d -> p s d", p=128), in_=xt[:]), reads=[Rxt], writes=[])
        if DBG.get("verbose"):
            print("L0 sbuf bytes remaining", nc.sbuf_bytes_remaining, "ops", S.n_ops, {e: len(v) for e, v in S.prog.items()})
        S.flush()


def l0_host(inputs):
    f = lambda a: np.asarray(a, np.float32)
    p0 = np.zeros((128, NP0), np.float32)
    p0[:, P_MU:P_MU + 14] = fm(inputs["mu_shift"][0])
    p0[:, P_W0:P_W0 + 4] = fm(inputs["w_decay0"][0])
    p0[:, P_AB:P_AB + 4] = fm(inputs["a_bias"][0])
    p0[:, P_KK:P_KK + 4] = fm(inputs["k_k"][0])
    p0[:, P_KA:P_KA + 4] = fm(inputs["k_a"][0])
    p0[:, P_RK:P_RK + 4] = fm(f(inputs["r_k"][0]).reshape(-1))
    p0[:, P_CB:P_CB + 4] = fm(inputs["conv_b"][0])
    p0[:, P_BRG:P_BRG + 4] = fm(f(inputs["b_rgate"][0]).reshape(-1))
    p0[:, P_BIG:P_BIG + 4] = fm(f(inputs["b_igate"][0]).reshape(-1))
    p0[:, P_LAM:P_LAM + 4] = fm(inputs["lru_lambda"][0])
    cw = f(inputs["conv_w"][0])
    for jt in range(4):
        p0[:, P_CW + 4 * jt:P_CW + 4 * jt + 4] = cw[:, jt * 128:(jt + 1) * 128].T
    p0[:, P_G:P_G + 8] = fm(inputs["norm_mix"][0])
    lora = np.concatenate([f(inputs["w_decay_up"][0]), f(inputs["w_a_up"][0])], axis=0)
    gbd = np.zeros((128, 8, 128), np.float32)
    for gi, w in enumerate((f(inputs["w_rgate"][0]), f(inputs["w_igate"][0]))):
        for jt in range(4):
            gbd[0:64, gi * 4 + jt, 0:64] = w[2 * jt]
            gbd[64:128, gi * 4 + jt, 64:128] = w[2 * jt + 1]
    lnx = np.stack([f(inputs["lnx_w"][0]), f(inputs["lnx_b"][0])], axis=0)
    i = np.arange(128)
    low = (i[:, None] > i[None, :]).astype(np.float32)
    up_s = (i[:, None] < i[None, :]).astype(np.float32)
    up_i = (i[:, None] <= i[None, :]).astype(np.float32)
    bd = np.zeros((128, 128), np.float32)
    bd[:64, :64] = 1.0
    bd[64:, 64:] = 1.0
    ind = np.zeros((128, 2), np.float32)
    ind[:64, 0] = 1.0
    ind[64:, 1] = 1.0
    masks = np.concatenate([low, -up_s, up_i, up_s, up_i, bd, ind], axis=1)
    return {"w_in0": np.ascontiguousarray(f(inputs["w_in0"][0])), "w_out0": np.ascontiguousarray(f(inputs["w_out0"][0])),
            "p0": p0, "lora0": np.ascontiguousarray(lora), "w_g_up0": np.ascontiguousarray(f(inputs["w_g_up"][0])),
            "gates_bd0": gbd, "lnx0": np.ascontiguousarray(lnx), "c_masks0": np.ascontiguousarray(masks)}
```

```python
import numpy as np
from contextlib import ExitStack
import concourse.bass as bass
import concourse.mybir as mybir
from concourse.bass_utils import run_bass_kernel_spmd

F32 = mybir.dt.float32
BF16 = mybir.dt.bfloat16
AF = mybir.ActivationFunctionType
ALU = mybir.AluOpType
AX = mybir.AxisListType


class Res:
    __slots__ = ("name", "w", "readers", "bank")

    def __init__(self, name="", bank=None):
        self.name = name
        self.w = None
        self.readers = {}
        self.bank = bank


class _Rec:
    def __init__(self):
        self.call = None

    def __getattr__(self, name):
        def f(*a, **k):
            self.call = (name, a, k)
            return self
        return f


def _eager(fn):
    rec = _Rec()
    fn(rec)
    name, a, k = rec.call
    return lambda e: getattr(e, name)(*a, **k)


class Sched:
    COMPUTE = ("pe", "act", "dve", "pool")
    NDSEM = 6

    def __init__(self, nc, stack, dma_queues=("sp", "pool")):
        self.nc = nc
        self.sems = {}
        self.ecnt = {}
        self.prog = {e: [] for e in ("pe", "act", "dve", "pool", "sp")}
        self.known = {e: {} for e in self.prog}
        for e in self.COMPUTE:
            self.sems["e_" + e] = stack.enter_context(nc.semaphore("e_" + e))
            self.ecnt[e] = 0
        self.dq = {}
        for q in dma_queues:
            keys = []
            for i in range(self.NDSEM):
                k = f"d_{q}{i}"
                self.sems[k] = stack.enter_context(nc.semaphore(k))
                keys.append(k)
            self.dq[q] = {"keys": keys, "cnt": [0] * self.NDSEM, "next": 0}
        self.n_ops = 0

    def res(self, name="", bank=None):
        return Res(name, bank)

    def _deps(self, eng, is_dma, reads, writes):
        deps = {}

        def add(t, raw):
            key, val, peng, pdma = t
            if not pdma and peng == eng and not is_dma:
                if not raw or eng == "pe":
                    return
            if deps.get(key, 0) < val:
                deps[key] = val

        for r in reads:
            if r.w is not None:
                add(r.w, True)
        for r in list(reads) + list(writes):
            if r.bank is not None:
                for key, (val, peng, pdma) in r.bank.readers.items():
                    if peng != eng:
                        add((key, val, peng, pdma), False)
        for w in writes:
            if w.w is not None:
                add(w.w, False)
            for key, (val, peng, pdma) in w.readers.items():
                add((key, val, peng, pdma), False)
        kn = self.known[eng]
        out = []
        for key, val in deps.items():
            if kn.get(key, 0) < val:
                kn[key] = val
                out.append((key, val))
        return out

    def _commit(self, tick, reads, writes):
        key, val, eng, is_dma = tick
        for w in writes:
            w.w = tick
            w.readers = {}
        for r in list(reads) + list(writes):
            if r.bank is not None:
                r.bank.readers[key] = (val, eng, is_dma)
        for r in reads:
            if r in writes:
                continue
            r.readers[key] = (val, eng, is_dma)

    def op(self, eng, fn, reads=(), writes=()):
        reads = [r for r in reads if r is not None]
        writes = [w for w in writes if w is not None]
        waits = self._deps(eng, False, reads, writes)
        self.ecnt[eng] += 1
        tick = ("e_" + eng, self.ecnt[eng], eng, False)
        self.prog[eng].append((waits, _eager(fn), ("e_" + eng, 1)))
        self._commit(tick, reads, writes)
        self.n_ops += 1

    def dma(self, q, fn, reads=(), writes=()):
        reads = [r for r in reads if r is not None]
        writes = [w for w in writes if w is not None]
        waits = self._deps(q, True, reads, writes)
        d = self.dq[q]
        slot = d["next"] % self.NDSEM
        d["next"] += 1
        key = d["keys"][slot]
        if d["cnt"][slot] > 0 and self.known[q].get(key, 0) < d["cnt"][slot]:
            self.known[q][key] = d["cnt"][slot]
            waits.append((key, d["cnt"][slot]))
        d["cnt"][slot] += 16
        tick = (key, d["cnt"][slot], q, True)
        self.prog[q].append((waits, _eager(fn), (key, 16)))
        self._commit(tick, reads, writes)
        self.n_ops += 1

    def barrier(self):
        allv = {}
        for e in self.COMPUTE:
            if self.ecnt[e] > 0:
                allv["e_" + e] = self.ecnt[e]
        for q, d in self.dq.items():
            for k, c in zip(d["keys"], d["cnt"]):
                if c > 0:
                    allv[k] = c
        for e in self.prog:
            waits = []
            for k, v in allv.items():
                if self.known[e].get(k, 0) < v:
                    self.known[e][k] = v
                    waits.append((k, v))
            if waits:
                self.prog[e].append((waits, None, None))

    def flush(self, final=False):
        nc = self.nc
        sems = self.sems
        fin = {}
        if final:
            for q, d in self.dq.items():
                for k, c in zip(d["keys"], d["cnt"]):
                    if c > 0:
                        fin[k] = c

        def run(eng_obj, items, extra=None):
            for waits, fn, inc in items:
                for key, val in waits:
                    eng_obj.wait_ge(sems[key], val)
                if fn is not None:
                    ins = fn(eng_obj)
                    ins.then_inc(sems[inc[0]], inc[1])
            if extra:
                for key, val in extra.items():
                    eng_obj.wait_ge(sems[key], val)

        prog = self.prog
        with nc.Block() as block:
            @block.tensor
            def _(e):
                run(e, prog["pe"])

            @block.scalar
            def _(e):
                run(e, prog["act"])

            @block.vector
            def _(e):
                run(e, prog["dve"])

            @block.gpsimd
            def _(e):
                run(e, prog["pool"])

            @block.sync
            def _(e):
                run(e, prog["sp"], extra=fin)
        self.prog = {e: [] for e in prog}


D = 1024
SEQ = 2048
NSEQ = 2
NTOK = NSEQ * SEQ
FF = 2816
EPS = 1e-6


class Ctx:
    _phase = [0]

    def __init__(self, nc, S, st):
        self.nc, self.S, self.st = nc, S, st
        self.n = 0
        Ctx._phase[0] += 1
        self.pfx = f"ph{Ctx._phase[0]}_"

    def sb(self, shape, dt, name=None):
        self.n += 1
        t = self.st.enter_context(self.nc.sbuf_tensor(self.pfx + (name or f"t{self.n}"), list(shape), dt))
        return t, self.S.res(name or f"t{self.n}")

    def ps(self, shape, dt, name=None):
        self.n += 1
        t = self.st.enter_context(self.nc.psum_tensor(self.pfx + (name or f"p{self.n}"), list(shape), dt))
        r = self.S.res(name or f"p{self.n}")
        r.bank = Res("bank")
        return t, r


def load_weight(S, q, dst, src, nk, res_list):
    for k in range(nk):
        S.dma(q, lambda e, k=k: e.dma_start(out=dst[:, k, :], in_=src[k * 128:(k + 1) * 128, :]),
              writes=[res_list[k]])


def rstd_ops(S, ss, Rss, n=D, eps=EPS):
    S.op("dve", lambda e: e.tensor_scalar(out=ss[:], in0=ss[:], scalar1=1.0 / n, scalar2=eps,
                                          op0=ALU.mult, op1=ALU.add), reads=[Rss], writes=[Rss])
    S.op("act", lambda e: e.activation(out=ss[:], in_=ss[:], func=AF.Sqrt), reads=[Rss], writes=[Rss])
    S.op("dve", lambda e: e.reciprocal(out=ss[:], in_=ss[:]), reads=[Rss], writes=[Rss])


def norm_T(S, xt, Rxt, gain, Rg, hT, RhT, tmp):
    junk, Rjunk, ss, Rss, xn, Rxn, pT, RpT, ident, Rid = tmp
    S.op("act", lambda e: e.activation(out=junk[:], in_=xt, func=AF.Square, accum_out=ss[:]),
         reads=[Rxt], writes=[Rjunk, Rss])
    rstd_ops(S, ss, Rss)
    S.op("act", lambda e: e.activation(out=xn[:], in_=xt, func=AF.Copy, scale=ss[:, 0:1]),
         reads=[Rxt, Rss], writes=[Rxn])
    for k in range(8):
        S.op("pe", lambda e, k=k: e.transpose(pT[:, k, :], xn[:, k * 128:(k + 1) * 128], ident[:]),
             reads=[Rxn, Rid], writes=[RpT])
    S.op("dve", lambda e: e.tensor_tensor(out=hT, in0=pT[:], in1=gain.unsqueeze(2).to_broadcast([128, 8, 128]),
                                          op=ALU.mult), reads=[RpT, Rg], writes=[RhT])


def make_ident(S, C, dt=BF16):
    idf, Ridf = C.sb([128, 128], F32)
    S.op("pool", lambda e: e.memset(idf[:], 0.0), writes=[Ridf])
    S.op("pool", lambda e: e.affine_select(out=idf[:], in_=idf[:], pattern=[[-1, 128]], compare_op=ALU.not_equal,
                                           fill=1.0, base=0, channel_multiplier=1), reads=[Ridf], writes=[Ridf])
    idb, Ridb = C.sb([128, 128], BF16)
    S.op("dve", lambda e: e.tensor_copy(out=idb[:], in_=idf[:]), reads=[Ridf], writes=[Ridb])
    return idf, Ridf, idb, Ridb


def phase_ffn(nc, S, src, dst, Dm, l, final):
    with ExitStack() as st:
        C = Ctx(nc, S, st)
        S.barrier()
        wg, _ = C.sb([128, 8, FF], BF16, "wg")
        wu, _ = C.sb([128, 8, FF], BF16, "wu")
        wd, _ = C.sb([128, 22, D], BF16, "wd")
        Rwg = [S.res() for _ in range(8)]
        Rwu = [S.res() for _ in range(8)]
        Rwd = [S.res() for _ in range(22)]
        gain, Rg = C.sb([128, 8], F32, "gain")
        S.dma("sp", lambda e: e.dma_start(out=gain[:], in_=Dm[f"g_ffn{l}"]), writes=[Rg])
        load_weight(S, "pool", wg, Dm[f"ffn_gate{l}"], 8, Rwg)
        load_weight(S, "pool", wu, Dm[f"ffn_up{l}"], 8, Rwu)
        load_weight(S, "pool", wd, Dm[f"ffn_down{l}"], 22, Rwd)
        if final:
            gfin, Rgfin = C.sb([128, D], F32, "gfin")
            S.dma("sp", lambda e: e.dma_start(out=gfin[:], in_=Dm["g_final"][0, :].partition_broadcast(128)),
                  writes=[Rgfin])
        idf, Ridf, idb, Ridb = make_ident(S, C)
        TT = 256
        xts = [C.sb([128, 2, D], F32) for _ in range(2)]
        hTs = [C.sb([128, 8, TT], BF16) for _ in range(2)]
        actT, RactT = C.sb([128, 22, TT], BF16, "actT")
        junk, Rjunk = C.sb([128, D], BF16, "junk")
        ss, Rss = C.sb([128, 1], F32, "ss")
        xn, Rxn = C.sb([128, D], BF16, "xn")
        sgs = [C.sb([128, TT], F32) for _ in range(2)]
        pTs = [C.ps([128, 8, 128], BF16) for _ in range(2)]
        pgu = [C.ps([128, 2, TT], F32) for _ in range(2)]
        pso = [C.ps([128, 512], F32) for _ in range(2)]
        npt = 0
        for it in range(DBG.get('ffn_tiles') or NTOK // TT):
            tok0 = it * TT
            xt, Rxt = xts[it % 2]
            hT, RhT = hTs[it % 2]
            S.dma("sp", lambda e, xt=xt, tok0=tok0: e.dma_start(
                out=xt[:], in_=src[tok0:tok0 + TT, :].rearrange("(s p) d -> p s d", p=128)), writes=[Rxt])
            for s in range(2):
                pT, RpT = pTs[npt % 2]
                npt += 1
                norm_T(S, xt[:, s, :], Rxt, gain[:], Rg, hT[:, :, s * 128:(s + 1) * 128], RhT,
                       (junk, Rjunk, ss, Rss, xn, Rxn, pT, RpT, idb, Ridb))
            for j in range(22):
                pg, Rpg = pgu[j % 2]
                sg, Rsg = sgs[j % 2]
                for k in range(8):
                    S.op("pe", lambda e, k=k, j=j, pg=pg, hT=hT: e.matmul(
                        pg[:, 0, :], lhsT=wg[:, k, j * 128:(j + 1) * 128], rhs=hT[:, k, :], start=(k == 0), stop=(k == 7)),
                        reads=[Rwg[k], RhT], writes=[Rpg])
                for k in range(8):
                    S.op("pe", lambda e, k=k, j=j, pg=pg, hT=hT: e.matmul(
                        pg[:, 1, :], lhsT=wu[:, k, j * 128:(j + 1) * 128], rhs=hT[:, k, :], start=(k == 0), stop=(k == 7)),
                        reads=[Rwu[k], RhT], writes=[Rpg])
                S.op("act", lambda e, pg=pg, sg=sg: e.activation(out=sg[:], in_=pg[:, 0, :], func=AF.Silu),
                     reads=[Rpg], writes=[Rsg])
                S.op("dve", lambda e, pg=pg, sg=sg, j=j: e.tensor_tensor(out=actT[:, j, :], in0=pg[:, 1, :], in1=sg[:],
                                                                        op=ALU.mult),
                     reads=[Rpg, Rsg], writes=[RactT])
            n = 0
            for s in range(2):
                for dh in range(2):
                    po, Rpo = pso[n % 2]
                    n += 1
                    for j in range(22):
                        S.op("pe", lambda e, j=j, s=s, dh=dh, po=po: e.matmul(
                            po[:], lhsT=actT[:, j, s * 128:(s + 1) * 128], rhs=wd[:, j, dh * 512:(dh + 1) * 512],
                            start=(j == 0), stop=(j == 21)), reads=[RactT, Rwd[j]], writes=[Rpo])
                    S.op("dve", lambda e, s=s, dh=dh, po=po, xt=xt: e.tensor_tensor(
                        out=xt[:, s, dh * 512:(dh + 1) * 512], in0=po[:], in1=xt[:, s, dh * 512:(dh + 1) * 512],
                        op=ALU.add), reads=[Rpo, Rxt], writes=[Rxt])
            if final:
                for s in range(2):
                    S.op("act", lambda e, s=s, xt=xt: e.activation(out=junk[:], in_=xt[:, s, :], func=AF.Square,
                                                                   accum_out=ss[:]), reads=[Rxt], writes=[Rjunk, Rss])
                    rstd_ops(S, ss, Rss)
                    S.op("act", lambda e, s=s, xt=xt: e.activation(out=xt[:, s, :], in_=xt[:, s, :], func=AF.Copy,
                                                                   scale=ss[:, 0:1]), reads=[Rxt, Rss], writes=[Rxt])
                    S.op("dve", lambda e, s=s, xt=xt: e.tensor_tensor(out=xt[:, s, :], in0=xt[:, s, :], in1=gfin[:],
                                                                      op=ALU.mult), reads=[Rxt, Rgfin], writes=[Rxt])
            S.dma("pool", lambda e, xt=xt, tok0=tok0: e.dma_start(
                out=dst[tok0:tok0 + TT, :].rearrange("(s p) d -> p s d", p=128), in_=xt[:]), reads=[Rxt], writes=[])
        S.flush()


def fm(v):
    v = np.asarray(v, np.float32).reshape(-1, 128)
    return np.ascontiguousarray(v.T)


def build(phases):
    nc = bass.Bass("TRN2", target_bir_lowering=False)
    Dm = {}

    def inp(name, shape):
        Dm[name] = nc.dram_tensor(name, list(shape), F32, kind="ExternalInput").ap()

    inp("x", [NTOK, D])
    for l in range(2):
        if f"F{l}" in phases:
            inp(f"ffn_gate{l}", [D, FF]); inp(f"ffn_up{l}", [D, FF]); inp(f"ffn_down{l}", [FF, D])
            inp(f"g_ffn{l}", [128, 8])
    if "F1" in phases:
        inp("g_final", [1, D])
    if "L0" in phases:
        inp("w_in0", [D, 2816]); inp("w_out0", [D, D]); inp("p0", [128, NP0]); inp("lora0", [128, 512])
        inp("w_g_up0", [128, 512]); inp("gates_bd0", [128, 8, 128]); inp("lnx0", [2, 512]); inp("c_masks0", [128, 770])
    if "L1" in phases:
        inp("w1", [D, W1C]); inp("wo1", [64, 16, D]); inp("g_mix1", [128, 8])
        inp("c_cos", [128, SEQ]); inp("c_sin", [128, SEQ]); inp("c_pm", [128, 128])
        inp("c_negmask", [128, 128]); inp("c_pow2", [128, NIT + 1])
    out = nc.dram_tensor("out", [NTOK, D], F32, kind="ExternalOutput").ap()
    scr = [nc.dram_tensor(f"scr{i}", [NTOK, D], F32).ap() for i in range(2)]
    with ExitStack() as st:
        S = Sched(nc, st)
        cur = Dm["x"]
        for i, ph in enumerate(phases):
            dst = out if i == len(phases) - 1 else scr[i % 2]
            if ph in ("F0", "F1"):
                phase_ffn(nc, S, cur, dst, Dm, int(ph[1]), ph == "F1")
            elif ph == "L1":
                phase_dsa(nc, S, cur, dst, Dm)
            elif ph == "L0":
                phase_l0(nc, S, cur, dst, Dm)
            cur = dst
        S.barrier()
        S.prog["sp"].append(([], None, None))
        S.flush(final=True)
    return nc


def host_inputs(inputs, phases):
    x = np.asarray(inputs["x"], np.float32)
    shared = {}
    for l in range(2):
        if f"F{l}" in phases:
            shared[f"ffn_gate{l}"] = np.ascontiguousarray(inputs["ffn_gate"][l], dtype=np.float32)
            shared[f"ffn_up{l}"] = np.ascontiguousarray(inputs["ffn_up"][l], dtype=np.float32)
            shared[f"ffn_down{l}"] = np.ascontiguousarray(inputs["ffn_down"][l], dtype=np.float32)
            shared[f"g_ffn{l}"] = fm(inputs["norm_ffn"][l])
    if "F1" in phases:
        shared["g_final"] = np.asarray(inputs["norm_final"], np.float32).reshape(1, D)
    if "L0" in phases:
        shared.update(l0_host(inputs))
    if "L1" in phases:
        w = np.asarray(inputs["w_in1"][0], np.float32)
        q, k, v, iq, ik, iw = np.split(w, np.cumsum([1024, 64, 64, 512, 64])[:].tolist(), axis=1)
        shared["w1"] = np.ascontiguousarray(np.concatenate([q, iq, k, k, ik, ik, v, iw], axis=1))
        shared["wo1"] = np.ascontiguousarray(np.asarray(inputs["w_out1"][0], np.float32).reshape(16, 64, D).transpose(1, 0, 2))
        shared["g_mix1"] = fm(inputs["norm_mix"][1])
        shared.update(dsa_consts())
    maps = []
    for c in range(8):
        m = dict(shared)
        m["x"] = np.ascontiguousarray(x[c * NSEQ:(c + 1) * NSEQ].reshape(NTOK, D))
        maps.append(m)
    return maps


PHASES = ("L0", "F0", "L1", "F1")
DBG = {"nseq": NSEQ, "qbs": None, "d1_tiles": None, "l0_tiles": None, "cores": 8}


def kernel(phases=PHASES, **inputs):
    nc = build(phases)
    maps = host_inputs(inputs, phases)
    ncores = DBG["cores"]
    res = run_bass_kernel_spmd(nc, maps[:ncores], core_ids=list(range(ncores)))
    outs = [np.asarray(r["out"], np.float32).reshape(NSEQ, SEQ, D) for r in res.results]
    outs += [np.zeros((NSEQ, SEQ, D), np.float32)] * (8 - ncores)
    return np.concatenate(outs, axis=0)


W1C = 1864
NIT = 24
TOPK = 256


def phase_dsa(nc, S, src, dst, Dm):
    with ExitStack() as st:
        C = Ctx(nc, S, st)
        S.barrier()
        w1, _ = C.sb([128, 8, W1C], BF16, "w1")
        Rw1 = [S.res() for _ in range(8)]
        load_weight(S, "pool", w1, Dm["w1"], 8, Rw1)
        wo, Rwo = C.sb([64, 16, D], BF16, "wo")
        for hh_ in range(16):
            S.dma("pool", lambda e, hh_=hh_: e.dma_start(out=wo[:, hh_, :], in_=Dm["wo1"][:, hh_, :]), writes=[Rwo])
        gain, Rg = C.sb([128, 8], F32, "gain")
        S.dma("sp", lambda e: e.dma_start(out=gain[:], in_=Dm["g_mix1"]), writes=[Rg])
        pm_, Rpm = C.sb([128, 128], BF16, "Pm")
        S.dma("pool", lambda e: e.dma_start(out=pm_[:], in_=Dm["c_pm"]), writes=[Rpm])
        negm, Rnegm = C.sb([128, 128], F32, "negm")
        S.dma("sp", lambda e: e.dma_start(out=negm[:], in_=Dm["c_negmask"]), writes=[Rnegm])
        pow2, Rpow2 = C.sb([128, NIT + 1], F32, "pow2")
        S.dma("sp", lambda e: e.dma_start(out=pow2[:], in_=Dm["c_pow2"]), writes=[Rpow2])
        idf, Ridf, idb, Ridb = make_ident(S, C)
        onesf, Rones = C.sb([128, 64], F32, "onesf")
        S.op("pool", lambda e: e.memset(onesf[:], 1.0), writes=[Rones])
        thrc, Rthrc = C.sb([128, 1], F32, "thrc")
        S.op("pool", lambda e: e.memset(thrc[:], -1e29), writes=[Rthrc])

        qT, RqT = C.sb([128, 8, SEQ], BF16, "qT")
        iqT, RiqT = C.sb([128, 4, SEQ], BF16, "iqT")
        kT2, RkT2 = C.sb([128, SEQ], BF16, "kT2")
        ikT2, RikT2 = C.sb([128, SEQ], BF16, "ikT2")
        Vaug, RV = C.sb([128, 16, 65], BF16, "Vaug")
        iw, Riw = C.sb([128, 16, 8], F32, "iw")
        S.op("pool", lambda e: e.memset(Vaug[:], 1.0), writes=[RV])

        T1 = 256
        xts = [C.sb([128, 2, D], F32) for _ in range(1)]
        hT, RhT = C.sb([128, 8, T1], BF16, "hT")
        junk, Rjunk = C.sb([128, D], BF16, "junk")
        ss, Rss = C.sb([128, 1], F32, "ss")
        xn, Rxn = C.sb([128, D], BF16, "xn")
        pbs = [C.sb([128, T1], BF16) for _ in range(2)]
        t1s = [C.sb([128, T1], F32) for _ in range(2)]
        t2s = [C.sb([128, T1], F32) for _ in range(2)]
        css = [C.sb([128, 2, T1], F32) for _ in range(2)]
        scs = [C.sb([128, SEQ], F32) for _ in range(2)]
        score, Rscore = scs[0]
        jcnt, Rjcnt = C.sb([128, SEQ], BF16, "jcnt")
        maskTs = [C.sb([128, 16, 128], BF16) for _ in range(2)]
        maskb, Rmaskb = C.sb([128, SEQ], BF16, "maskb")
        rls = [C.sb([128, 512], F32) for _ in range(2)]
        Es = [C.sb([128, 4, 128], BF16) for _ in range(2)]
        PTs = [C.sb([128, 4, 128], BF16) for _ in range(2)]
        rs, Rrs = C.sb([128, 512], F32, "rs")
        bc, Rbc = C.sb([64, 512], F32, "bc")
        attnT = [C.sb([64, 4, 128], BF16) for _ in range(4)]
        xres, Rxres = C.sb([128, D], F32, "xres")
        bis, Rbis = C.sb([128, 8], F32, "bis")
        steps, Rsteps = C.sb([128, NIT + 1], F32, "steps")
        cnts, Rcnts = C.sb([128, NIT], F32, "cnts")
        pT, RpT = C.ps([128, 8, 128], BF16, "pT")
        B = [C.ps([128, 4, 128], F32) for _ in range(7)]

        for sq in range(DBG["nseq"]):
            base = sq * SEQ
            for tt in range(DBG["d1_tiles"] or SEQ // T1):
                t0 = tt * T1
                xt, Rxt = xts[0]
                cs, Rcs = css[tt % 2]
                S.dma("sp", lambda e, xt=xt, a=base + t0: e.dma_start(
                    out=xt[:], in_=src[a:a + T1, :].rearrange("(s p) d -> p s d", p=128)), writes=[Rxt])
                S.dma("sp", lambda e, cs=cs, t0=t0: e.dma_start(out=cs[:, 0, :], in_=Dm["c_cos"][:, t0:t0 + T1]),
                      writes=[Rcs])
                S.dma("sp", lambda e, cs=cs, t0=t0: e.dma_start(out=cs[:, 1, :], in_=Dm["c_sin"][:, t0:t0 + T1]),
                      writes=[Rcs])
                for s in range(T1 // 128):
                    norm_T(S, xt[:, s, :], Rxt, gain[:], Rg, hT[:, :, s * 128:(s + 1) * 128], RhT,
                           (junk, Rjunk, ss, Rss, xn, Rxn, pT, RpT, idb, Ridb))
                for nt in range(DBG.get("d1_nt", 14)):
                    pp, Rpp = B[nt % 2]
                    pr, Rpr = B[2 + nt % 2]
                    pb, Rpb = pbs[nt % 2]
                    ta, Rta = t1s[nt % 2]
                    tb, Rtb = t2s[nt % 2]
                    ppv = pp[:].rearrange("p a b -> p (a b)")[:, 0:T1]
                    prv = pr[:].rearrange("p a b -> p (a b)")[:, 0:T1]
                    for k in range(8):
                        S.op("pe", lambda e, k=k, nt=nt, ppv=ppv: e.matmul(
                            ppv, lhsT=w1[:, k, nt * 128:(nt + 1) * 128], rhs=hT[:, k, :], start=(k == 0), stop=(k == 7)),
                            reads=[Rw1[k], RhT], writes=[Rpp])
                    lvl = DBG.get("d1_ops", 9)
                    if lvl < 2:
                        continue
                    S.op("act", lambda e, pb=pb, ppv=ppv: e.copy(out=pb[:], in_=ppv), reads=[Rpp], writes=[Rpb])
                    if lvl < 3:
                        continue
                    S.op("pe", lambda e, pb=pb, prv=prv: e.matmul(prv, lhsT=pm_[:], rhs=pb[:], start=True, stop=True),
                         reads=[Rpm, Rpb], writes=[Rpr])
                    if lvl < 4:
                        continue
                    if DBG.get("tt", 3) & 1:
                        S.op("dve", lambda e, ta=ta, ppv=ppv, cs=cs: e.tensor_tensor(out=ta[:], in0=ppv, in1=cs[:, 0, :],
                                                                                    op=ALU.mult),
                             reads=[Rpp, Rcs, Rpb], writes=[Rta])
                    if DBG.get("tt", 3) & 2:
                        S.op("dve", lambda e, tb=tb, prv=prv, cs=cs: e.tensor_tensor(out=tb[:], in0=prv, in1=cs[:, 1, :],
                                                                                    op=ALU.mult),
                             reads=[Rpr, Rcs], writes=[Rtb])
                    if DBG.get("tt", 3) & 4:
                        S.op("dve", lambda e, ta=ta, ppv=ppv, cs=cs: e.tensor_tensor(out=ta[:], in0=ppv, in1=ta[:],
                                                                                    op=ALU.mult),
                             reads=[Rpp, Rcs], writes=[Rta])
                    if lvl < 5:
                        continue
                    if nt < 8:
                        dest, Rd = qT[:, nt, t0:t0 + T1], RqT
                    elif nt < 12:
                        dest, Rd = iqT[:, nt - 8, t0:t0 + T1], RiqT
                    elif nt == 12:
                        dest, Rd = kT2[:, t0:t0 + T1], RkT2
                    else:
                        dest, Rd = ikT2[:, t0:t0 + T1], RikT2
                    S.op(DBG.get("addeng", "pool"), lambda e, dest=dest, ta=ta, tb=tb: e.tensor_tensor(out=dest, in0=ta[:], in1=tb[:],
                                                                                   op=ALU.add),
                         reads=[Rta, Rtb], writes=[Rd])
                for s in range(DBG.get("d1_v", T1 // 128)):
                    blk = t0 // 128 + s
                    pv, Rpv = B[4]
                    pvv = pv[:].rearrange("p a b -> p (a b)")[:, 0:72]
                    for k in range(8):
                        S.op("pe", lambda e, k=k, s=s, pvv=pvv: e.matmul(
                            pvv, lhsT=hT[:, k, s * 128:(s + 1) * 128], rhs=w1[:, k, 1792:1864], start=(k == 0),
                            stop=(k == 7)), reads=[Rw1[k], RhT], writes=[Rpv])
                    S.op("act", lambda e, blk=blk, pvv=pvv: e.copy(out=Vaug[:, blk, 0:64], in_=pvv[:, 0:64]),
                         reads=[Rpv], writes=[RV])
                    S.op("dve", lambda e, blk=blk, pvv=pvv: e.tensor_scalar(
                        out=iw[:, blk, :], in0=pvv[:, 64:72], scalar1=1.0 / (8.0 * 8.0 ** 0.5), scalar2=None,
                        op0=ALU.mult), reads=[Rpv], writes=[Riw])

            if DBG.get("dump_d1"):
                dd = [(qT[:, 0, 0:512], 0, 0, 512), (kT2[:, 0:512], 0, 512, 512), (iqT[:, 0, 0:512], 128, 0, 512),
                      (ikT2[:, 0:512], 128, 512, 512), (Vaug[:, 0:4, :].rearrange("p a b -> p (a b)"), 256, 0, 260),
                      (iw[:, 0:4, :].rearrange("p a b -> p (a b)"), 256, 512, 32)]
                for ap_, r0, c0_, w_ in dd:
                    S.dma("pool", lambda e, ap_=ap_, r0=r0, c0_=c0_, w_=w_: e.dma_start(
                        out=dst[r0:r0 + 128, c0_:c0_ + w_], in_=ap_), reads=[RqT, RkT2, RiqT, RikT2, RV, Riw], writes=[])
            def idx_items(qb, pos):
                nk = (qb + 1) * 128
                q0 = qb * 128
                sc, Rsc = scs[pos % 2]
                pi, Rpi = B[6]
                items = []
                n = 0
                for h in range(8):
                    pr_ = (h % 2) * 64
                    for c0 in range(0, nk, 512):
                        w = min(512, nk - c0)
                        rl, Rrl = rls[n % 2]
                        n += 1

                        def it(h=h, pr_=pr_, c0=c0, w=w, rl=rl, Rrl=Rrl):
                            piv = pi[:].rearrange("p a b -> p (a b)")[:, 0:w]
                            S.op("pe", lambda e: e.matmul(
                                piv, lhsT=iqT[pr_:pr_ + 64, h // 2, q0:q0 + 128], rhs=ikT2[pr_:pr_ + 64, c0:c0 + w],
                                start=True, stop=True), reads=[RiqT, RikT2], writes=[Rpi])
                            S.op("act", lambda e: e.activation(out=rl[:, 0:w], in_=piv, func=AF.Relu),
                                 reads=[Rpi], writes=[Rrl])
                            if h == 0:
                                S.op("dve", lambda e: e.tensor_scalar(
                                    out=sc[:, c0:c0 + w], in0=rl[:, 0:w], scalar1=iw[:, qb, 0:1], scalar2=None,
                                    op0=ALU.mult), reads=[Rrl, Riw], writes=[Rsc])
                            else:
                                S.op("dve", lambda e: e.scalar_tensor_tensor(
                                    out=sc[:, c0:c0 + w], in0=rl[:, 0:w], scalar=iw[:, qb, h:h + 1],
                                    in1=sc[:, c0:c0 + w], op0=ALU.mult, op1=ALU.add), reads=[Rrl, Riw, Rsc],
                                    writes=[Rsc])
                        items.append(it)
                return items

            def bis_items(qb, pos):
                nk = (qb + 1) * 128
                q0 = qb * 128
                sc, Rsc = scs[pos % 2]
                items = []

                def pre():
                    S.op("dve", lambda e: e.tensor_tensor(out=sc[:, q0:nk], in0=sc[:, q0:nk], in1=negm[:], op=ALU.add),
                         reads=[Rsc, Rnegm], writes=[Rsc])
                    if qb >= 2:
                        S.op("dve", lambda e: e.tensor_reduce(out=bis[:, 0:1], in_=sc[:, 0:nk], axis=AX.X, op=ALU.max),
                             reads=[Rsc], writes=[Rbis])
                        S.op("dve", lambda e: e.tensor_reduce(out=bis[:, 1:2], in_=sc[:, 0:nk - 128], axis=AX.X,
                                                              op=ALU.min), reads=[Rsc], writes=[Rbis])
                        S.op("dve", lambda e: e.tensor_tensor(out=bis[:, 2:3], in0=bis[:, 0:1], in1=bis[:, 1:2],
                                                              op=ALU.subtract), reads=[Rbis], writes=[Rbis])
                        S.op("dve", lambda e: e.tensor_scalar(out=steps[:], in0=pow2[:], scalar1=bis[:, 2:3],
                                                              scalar2=None, op0=ALU.mult), reads=[Rbis, Rpow2],
                             writes=[Rsteps])
                        S.op("pool", lambda e: e.memset(cnts[:], 0.0), writes=[Rcnts])
                        S.op("dve", lambda e: e.tensor_tensor(out=bis[:, 3:4], in0=bis[:, 1:2], in1=steps[:, 0:1],
                                                              op=ALU.add), reads=[Rbis, Rsteps], writes=[Rbis])
                items.append(pre)
                if qb >= 2:
                    for k in range(NIT):
                        def itk(k=k):
                            S.op("dve", lambda e: e.tensor_scalar(
                                out=jcnt[:, 0:nk], in0=sc[:, 0:nk], scalar1=bis[:, 3:4], scalar2=0.0, op0=ALU.is_ge,
                                op1=ALU.add, accum_out=cnts[:, k:k + 1]), reads=[Rsc, Rbis, Rcnts],
                                writes=[Rjcnt, Rcnts])
                            S.op("dve", lambda e: e.scalar_tensor_tensor(
                                out=bis[:, 4:5], in0=cnts[:, k:k + 1], scalar=float(TOPK), in1=steps[:, k:k + 1],
                                op0=ALU.is_ge, op1=ALU.mult), reads=[Rcnts, Rsteps], writes=[Rbis])
                            S.op("dve", lambda e: e.scalar_tensor_tensor(
                                out=bis[:, 3:4], in0=bis[:, 3:4], scalar=steps[:, k + 1:k + 2], in1=bis[:, 4:5],
                                op0=ALU.subtract, op1=ALU.add), reads=[Rbis, Rsteps], writes=[Rbis])
                        items.append(itk)

                def post():
                    if qb >= 2:
                        S.op("dve", lambda e: e.tensor_tensor(out=bis[:, 1:2], in0=bis[:, 3:4],
                                                              in1=steps[:, NIT:NIT + 1], op=ALU.subtract),
                             reads=[Rbis, Rsteps], writes=[Rbis])
                        thr, Rthr = bis[:, 1:2], Rbis
                    else:
                        thr, Rthr = thrc[:, 0:1], Rthrc
                    S.op("dve", lambda e: e.tensor_scalar(out=maskb[:, 0:nk], in0=sc[:, 0:nk], scalar1=thr, scalar2=None,
                                                          op0=ALU.is_ge), reads=[Rsc, Rthr], writes=[Rmaskb])
                items.append(post)
                return items

            def stage_maskT(qb, pos):
                mT, RmT = maskTs[pos % 2]
                for jb in range(0, qb + 1, 8):
                    nb = min(8, qb + 1 - jb)
                    for i in range(nb):
                        S.op("pe", lambda e: e.transpose(pT[:, i, :], maskb[:, (jb + i) * 128:(jb + i + 1) * 128], idb[:]),
                             reads=[Rmaskb, Ridb], writes=[RpT])
                    S.op("act", lambda e: e.copy(out=mT[:, jb:jb + nb, :], in_=pT[:, 0:nb, :]), reads=[RpT], writes=[RmT])

            def attn_items(qb, pos):
                q0 = qb * 128
                mT, RmT = maskTs[pos % 2]
                items = []
                n = 0
                for j in range(qb + 1):
                    for g in range(4):
                        items.append((j, g, n))
                        n += 1

                def mk(j, g, n):
                    def it():
                        par, half = g % 2, g // 2
                        pst, Rpst = B[4 + n % 2]
                        E, RE = Es[n % 2]
                        PT, RPT = PTs[n % 2]
                        S.op("pe", lambda e: e.matmul(
                            pst[:], lhsT=kT2[par * 64:par * 64 + 64, j * 128:(j + 1) * 128],
                            rhs=qT[par * 64:par * 64 + 64, 4 * half:4 * half + 4, q0:q0 + 128], start=True, stop=True),
                            reads=[RkT2, RqT], writes=[Rpst])
                        S.op("act", lambda e: e.activation(out=E[:], in_=pst[:], func=AF.Exp, scale=0.125),
                             reads=[Rpst], writes=[RE])
                        S.op("pool", lambda e: e.tensor_tensor(
                            out=PT[:], in0=E[:], in1=mT[:, j:j + 1, :].to_broadcast([128, 4, 128]), op=ALU.mult),
                            reads=[RE, RmT], writes=[RPT])
                        ot, Rot = B[g]
                        S.op("pe", lambda e: e.matmul(
                            ot[0:65], lhsT=Vaug[:, j, :], rhs=PT[:], start=(j == 0), stop=(j == qb)),
                            reads=[RV, RPT], writes=[Rot])
                    return it
                return [mk(j, g, n) for (j, g, n) in items]

            def interleave(lists):
                lists = [l for l in lists if l]
                if not lists:
                    return
                L = max(len(l) for l in lists)
                pos = [0] * len(lists)
                for s_ in range(L):
                    for li, l in enumerate(lists):
                        tgt = ((s_ + 1) * len(l)) // L
                        while pos[li] < tgt:
                            l[pos[li]]()
                            pos[li] += 1

            def stage_out(qb):
                q0 = qb * 128
                S.dma("sp", lambda e: e.dma_start(out=xres[:], in_=src[base + q0:base + q0 + 128, :]), writes=[Rxres])
                for g in range(4):
                    ot, Rot = B[g]
                    otv = ot[:].rearrange("p a b -> p (a b)")
                    pbc, Rpbc = B[6]
                    pbcv = pbc[:].rearrange("p a b -> p (a b)")
                    at, Rat = attnT[g]
                    S.op("dve", lambda e: e.reciprocal(out=rs[64:65, :], in_=otv[64:65, :]), reads=[Rot], writes=[Rrs])
                    S.op("pe", lambda e: e.matmul(pbcv[0:64, :], lhsT=onesf[64:65, 0:64], rhs=rs[64:65, :],
                                                  start=True, stop=True), reads=[Rones, Rrs], writes=[Rpbc])
                    S.op("act", lambda e: e.copy(out=bc[:], in_=pbcv[0:64, :]), reads=[Rpbc], writes=[Rbc])
                    S.op("dve", lambda e: e.tensor_tensor(
                        out=at[:].rearrange("p a b -> p (a b)"), in0=otv[0:64, :], in1=bc[:], op=ALU.mult),
                        reads=[Rot, Rbc], writes=[Rat])
                for dh in range(2):
                    po, Rpo = B[6]
                    pov = po[:].rearrange("p a b -> p (a b)")
                    n2 = 0
                    for g in range(4):
                        par, half = g % 2, g // 2
                        at, Rat = attnT[g]
                        for i in range(4):
                            hh = 2 * (4 * half + i) + par
                            S.op("pe", lambda e: e.matmul(
                                pov, lhsT=at[:, i, :], rhs=wo[:, hh, dh * 512:(dh + 1) * 512], start=(n2 == 0),
                                stop=(n2 == 15)), reads=[Rat, Rwo], writes=[Rpo])
                            n2 += 1
                    S.op("dve", lambda e: e.tensor_tensor(
                        out=xres[:, dh * 512:(dh + 1) * 512], in0=pov, in1=xres[:, dh * 512:(dh + 1) * 512], op=ALU.add),
                        reads=[Rpo, Rxres], writes=[Rxres])
                S.dma("pool", lambda e: e.dma_start(out=dst[base + q0:base + q0 + 128, :], in_=xres[:]), reads=[Rxres],
                      writes=[])

            qbl = list(DBG["qbs"]) if DBG["qbs"] is not None else list(range(SEQ // 128))
            nq = len(qbl)
            if nq:
                interleave([idx_items(qbl[0], 0)])
                interleave([bis_items(qbl[0], 0)])
                stage_maskT(qbl[0], 0)
            if nq > 1:
                interleave([idx_items(qbl[1], 1)])
            for ii, qb in enumerate(qbl):
                interleave([attn_items(qb, ii),
                            idx_items(qbl[ii + 2], ii + 2) if ii + 2 < nq else [],
                            bis_items(qbl[ii + 1], ii + 1) if ii + 1 < nq else []])
                if ii + 1 < nq:
                    stage_maskT(qbl[ii + 1], ii + 1)
                stage_out(qb)
        S.flush()


def dsa_consts():
    inv = (10000.0 ** (-(np.arange(0, 64, 2, dtype=np.float32)) / np.float32(64))).astype(np.float32)
    ang = (np.arange(SEQ, dtype=np.float32)[:, None] * inv[None, :]).astype(np.float32)
    cos, sin = np.cos(ang).astype(np.float32), np.sin(ang).astype(np.float32)
    p = np.arange(128)
    d = p % 64
    c_cos = np.ascontiguousarray(cos[:, d % 32].T)
    sgn = np.where(d < 32, -1.0, 1.0).astype(np.float32)
    c_sin = np.ascontiguousarray((sin[:, d % 32] * sgn[None, :]).T.astype(np.float32))
    pm = np.zeros((128, 128), np.float32)
    partner = (d + 32) % 64 + 64 * (p // 64)
    pm[p, partner] = 1.0
    negmask = np.zeros((128, 128), np.float32)
    negmask[:64, 64:] = -1e30
    pow2 = np.tile((2.0 ** -(1.0 + np.arange(NIT + 1))).astype(np.float32)[None, :], (128, 1))
    return {"c_cos": c_cos, "c_sin": c_sin, "c_pm": pm, "c_negmask": negmask, "c_pow2": np.ascontiguousarray(pow2)}


TL = 256
NCH = TL // 128
CDEC = float(np.exp(-0.5))
P_MU, P_W0, P_AB, P_KK, P_KA, P_RK, P_CB, P_BRG, P_BIG, P_LAM, P_CW, P_G = 0, 14, 18, 22, 26, 30, 34, 38, 42, 46, 50, 66
NP0 = 74


def phase_l0(nc, S, src, dst, Dm):
    with ExitStack() as st:
        C = Ctx(nc, S, st)
        S.barrier()
        win, _ = C.sb([128, 8, 2816], BF16, "win")
        Rwin = [S.res() for _ in range(8)]
        load_weight(S, "pool", win, Dm["w_in0"], 8, Rwin)
        wo, _ = C.sb([128, 8, D], BF16, "wo0")
        Rwo = [S.res() for _ in range(8)]
        load_weight(S, "pool", wo, Dm["w_out0"], 8, Rwo)
        P0, RP0 = C.sb([128, NP0], F32, "P0")
        S.dma("sp", lambda e: e.dma_start(out=P0[:], in_=Dm["p0"]), writes=[RP0])
        lora, Rlora = C.sb([128, 512], BF16, "lora")
        S.dma("pool", lambda e: e.dma_start(out=lora[:], in_=Dm["lora0"]), writes=[Rlora])
        wgu, Rwgu = C.sb([128, 512], BF16, "wgu")
        S.dma("pool", lambda e: e.dma_start(out=wgu[:], in_=Dm["w_g_up0"]), writes=[Rwgu])
        gbd, Rgbd = C.sb([128, 8, 128], BF16, "gbd")
        S.dma("pool", lambda e: e.dma_start(out=gbd[:], in_=Dm["gates_bd0"]), writes=[Rgbd])
        lnw, Rlnw = C.sb([128, 2, 512], F32, "lnw")
        S.dma("sp", lambda e: e.dma_start(out=lnw[:, 0, :], in_=Dm["lnx0"][0, :].partition_broadcast(128)), writes=[Rlnw])
        S.dma("sp", lambda e: e.dma_start(out=lnw[:, 1, :], in_=Dm["lnx0"][1, :].partition_broadcast(128)), writes=[Rlnw])
        cm, Rcm = C.sb([128, 128 + 256 + 256 + 128 + 2], BF16, "cm")
        S.dma("pool", lambda e: e.dma_start(out=cm[:], in_=Dm["c_masks0"]), writes=[Rcm])
        maskL, maskU2, maskU3, BD, IND = cm[:, 0:128], cm[:, 128:384], cm[:, 384:640], cm[:, 640:768], cm[:, 768:770]
        idf, Ridf, idb, Ridb = make_ident(S, C)
        onesT, Rones = C.sb([128, 128], F32, "onesT")
        S.op("pool", lambda e: e.memset(onesT[:], 1.0), writes=[Rones])
        spt, Rspt = C.sb([128, 16], F32, "spt")
        S.op("act", lambda e: e.activation(out=spt[:, 0:4], in_=P0[:, P_LAM:P_LAM + 4], func=AF.Exp, scale=-1.0),
             reads=[RP0], writes=[Rspt])
        S.op("act", lambda e: e.activation(out=spt[:, 0:4], in_=spt[:, 0:4], func=AF.Ln, bias=1.0), reads=[Rspt],
             writes=[Rspt])
        for i, m in ((1, 8.0), (2, -8.0), (3, -16.0)):
            S.op("dve", lambda e, i=i, m=m: e.tensor_scalar(out=spt[:, 4 * i:4 * i + 4], in0=spt[:, 0:4], scalar1=m,
                                                            scalar2=None, op0=ALU.mult), reads=[Rspt], writes=[Rspt])

        xts = [C.sb([128, NCH, D], F32) for _ in range(2)]
        hT, RhT = C.sb([128, 8, TL], BF16, "hT")
        junk, Rjunk = C.sb([128, D], BF16, "junk")
        ss, Rss = C.sb([128, 1], F32, "ss")
        xn, Rxn = C.sb([128, D], BF16, "xn")
        Pb = [C.sb([128, TL + 1], F32) for _ in range(14)]
        LX = [C.sb([128, TL + 3], F32) for _ in range(4)]
        hprev, Rhprev = C.sb([128, 4], F32, "hprev")
        tmps = {}

        def tmp(name, dt=F32, shape=None):
            if name not in tmps:
                tmps[name] = C.sb(shape or [128, TL], dt, "tmp_" + name)
            return tmps[name]

        ART = [C.sb([128, NCH, 2, 128], BF16) for _ in range(4)]
        BTt = [C.sb([128, TL], BF16) for _ in range(4)]
        KTt = [C.sb([128, TL], BF16) for _ in range(4)]
        BTm = [C.sb([128, 2, TL], BF16) for _ in range(4)]
        KTm = [C.sb([128, 2, TL], BF16) for _ in range(4)]
        Hm = [C.sb([128, 2, 64], BF16) for _ in range(4)]
        HfA, RHfA = C.sb([128, 4, 64], F32, "HfA")
        HmA, RHmA = C.sb([128, 4, 2, 64], BF16, "HmA")
        XbA, RXbA = C.sb([128, 4, 2, 64], BF16, "XbA")
        UnA, RUnA = C.sb([128, 4, 2, 64], BF16, "UnA")
        hmk, Rhmk = C.sb([128, 2], F32, "hmk")
        S.op("pool", lambda e: e.memset(hmk[:], 0.0), writes=[Rhmk])
        S.op("pool", lambda e: e.memset(hmk[0:64, 0:1], 1.0), writes=[Rhmk])
        S.op("pool", lambda e: e.memset(hmk[64:128, 1:2], 1.0), writes=[Rhmk])
        TM = [C.sb([128, 3, 4, 128], BF16) for _ in range(NCH)]
        WL, RWL = C.sb([128, 4, NCH], F32, "WL")
        Hs = [C.sb([128, 64], F32) for _ in range(4)]
        Hb = [C.sb([128, 64], BF16) for _ in range(4)]
        mixT, RmixT = C.sb([128, 8, TL], BF16, "mixT")
        bons, Rbons = C.sb([128, NCH, 8], F32, "bons")
        pT, RpT = C.ps([128, 8, 128], BF16, "pT")
        PP, RPP_ = C.ps([128, 2, TL], F32, "PP")
        RPP = [S.res(bank=RPP_.bank), S.res(bank=RPP_.bank)]
        BKA, RBKA_ = C.ps([128, 2, 256], F32, "BKA")
        BKB, RBKB_ = C.ps([128, 2, 256], F32, "BKB")
        RBK = [S.res(bank=RBKA_.bank), S.res(bank=RBKA_.bank), S.res(bank=RBKB_.bank), S.res(bank=RBKB_.bank)]
        slots = [BKA[:, 0, :], BKA[:, 1, :], BKB[:, 0, :], BKB[:, 1, :]]
        BKC, RBKC_ = C.ps([128, 4, 128], F32, "BKC")
        RBKC = [S.res(bank=RBKC_.bank), S.res(bank=RBKC_.bank)]
        BKD, RBKD_ = C.ps([128, 512], F32, "BKD")
        RX, RU, RdH, Rbon = [S.res(bank=RBKD_.bank) for _ in range(4)]
        YT, RYT = C.ps([128, 8, 64], F32, "YT")
        GP, RGP = C.ps([128, 512], F32, "GP")
        nslot = [0]

        def slot():
            i = nslot[0] % 4
            nslot[0] += 1
            return slots[i], RBK[i]

        def PV(col, n=4):
            return P0[:, col:col + 1] if n == 1 else P0[:, col:col + n]

        for sq in range(DBG["nseq"]):
            base = sq * SEQ
            for i in range(14):
                S.op("pool", lambda e, i=i: e.memset(Pb[i][0][:, 0:1], 0.0), writes=[Pb[i][1]])
            for i in range(4):
                S.op("pool", lambda e, i=i: e.memset(LX[i][0][:, 0:3], 0.0), writes=[LX[i][1]])
                S.op("pool", lambda e, i=i: e.memset(Hs[i][0][:], 0.0), writes=[Hs[i][1]])
                S.op("pool", lambda e, i=i: e.memset(Hb[i][0][:], 0.0), writes=[Hb[i][1]])
                S.op("pool", lambda e, i=i: e.memset(Hm[i][0][:], 0.0), writes=[Hm[i][1]])
            S.op("pool", lambda e: e.memset(hprev[:], 0.0), writes=[Rhprev])
            S.op("pool", lambda e: e.memset(HfA[:], 0.0), writes=[RHfA])
            S.op("pool", lambda e: e.memset(HmA[:], 0.0), writes=[RHmA])
            for tt in range(DBG["l0_tiles"] or SEQ // TL):
                t0 = tt * TL
                xt, Rxt = xts[tt % 2]
                S.dma("sp", lambda e, xt=xt, a=base + t0: e.dma_start(
                    out=xt[:], in_=src[a:a + TL, :].rearrange("(s p) d -> p s d", p=128)), writes=[Rxt])
                for s in range(NCH):
                    norm_T(S, xt[:, s, :], Rxt, P0[:, P_G:P_G + 8], RP0, hT[:, :, s * 128:(s + 1) * 128], RhT,
                           (junk, Rjunk, ss, Rss, xn, Rxn, pT, RpT, idb, Ridb))
                npp = [0]

                def proj(nt):
                    i = npp[0] % 2
                    npp[0] += 1
                    tgt, Rt = (PP[:, 0, :], RPP[0]) if i == 0 else (GP[:, 0:TL], RGP)
                    for k in range(8):
                        S.op("pe", lambda e: e.matmul(
                            tgt, lhsT=win[:, k, nt * 128:(nt + 1) * 128], rhs=hT[:, k, :], start=(k == 0),
                            stop=(k == 7)), reads=[Rwin[k], RhT], writes=[Rt])
                    return tgt, Rt

                def shift(nt, name):
                    pp, Rpp = proj(nt)
                    Pt, RPt = Pb[nt]
                    o, Ro = tmp(name)
                    d_, Rd_ = tmp("shd")
                    S.op("act", lambda e: e.copy(out=Pt[:, 1:TL + 1], in_=pp), reads=[Rpp], writes=[RPt])
                    S.op("dve", lambda e: e.tensor_tensor(out=d_[:], in0=Pt[:, 0:TL], in1=Pt[:, 1:TL + 1], op=ALU.subtract),
                         reads=[RPt], writes=[Rd_])
                    S.op("dve", lambda e: e.scalar_tensor_tensor(out=o[:], in0=d_[:], scalar=P0[:, P_MU + nt:P_MU + nt + 1],
                                                                 in1=Pt[:, 1:TL + 1], op0=ALU.mult, op1=ALU.add),
                         reads=[Rd_, RPt, RP0], writes=[Ro])
                    S.op("act", lambda e: e.copy(out=Pt[:, 0:1], in_=Pt[:, TL:TL + 1]), reads=[RPt], writes=[RPt])
                    return o, Ro

                LV = DBG.get('l0_lvl', 9)
                if LV < 2:
                    continue
                swa, Rswa = shift(12, "s_wa")
                lor, Rlor = tmp("lor", BF16)
                S.op("act", lambda e: e.activation(out=lor[0:64, :], in_=swa[0:64, :], func=AF.Tanh), reads=[Rswa],
                     writes=[Rlor])
                S.op("act", lambda e: e.copy(out=lor[64:128, :], in_=swa[64:128, :]), reads=[Rswa], writes=[Rlor])
                sgd_, Rsgd_ = shift(13, "s_gd")
                sgd, Rsgd = tmp("sgd", BF16)
                S.op("act", lambda e: e.activation(out=sgd[:], in_=sgd_[:], func=AF.Sigmoid), reads=[Rsgd_], writes=[Rsgd])

                if LV < 3:
                    continue
                for j in range(4):
                    r_, Rr = shift(j, "s_r")
                    k_, Rk = shift(4 + j, "s_k")
                    v_, Rv = shift(8 + j, "s_v")
                    art, Rart = ART[j]
                    bt, Rbt = BTt[j]
                    kt, Rkt = KTt[j]
                    zw, Rzw = slot()
                    S.op("pe", lambda e, j=j, zw=zw: e.matmul(zw, lhsT=lora[0:64, j * 128:(j + 1) * 128], rhs=lor[0:64, :],
                                                              start=True, stop=True), reads=[Rlora, Rlor], writes=[Rzw])
                    ee, Ree = tmp("ee")
                    S.op("act", lambda e, j=j, zw=zw: e.activation(out=ee[:], in_=zw, func=AF.Sigmoid,
                                                                  bias=P0[:, P_W0 + j:P_W0 + j + 1]),
                         reads=[Rzw, RP0], writes=[Ree])
                    za, Rza = slot()
                    S.op("pe", lambda e, j=j, za=za: e.matmul(za, lhsT=lora[64:128, j * 128:(j + 1) * 128],
                                                              rhs=lor[64:128, :], start=True, stop=True),
                         reads=[Rlora, Rlor], writes=[Rza])
                    aa, Raa = tmp("aa")
                    S.op("act", lambda e, j=j, za=za: e.activation(out=aa[:], in_=za, func=AF.Sigmoid,
                                                                  bias=P0[:, P_AB + j:P_AB + j + 1]),
                         reads=[Rza, RP0], writes=[Raa])
                    cs, Rcs = tmp("cs")
                    for c in range(NCH):
                        S.op("dve", lambda e, c=c: e.tensor_tensor_scan(
                            out=cs[:, c * 128:(c + 1) * 128], data0=onesT[:], data1=ee[:, c * 128:(c + 1) * 128],
                            initial=0.0, op0=ALU.mult, op1=ALU.add), reads=[Rones, Ree], writes=[Rcs])
                    csm, Rcsm = tmp("csm")
                    S.op("dve", lambda e: e.tensor_tensor(out=csm[:], in0=cs[:], in1=ee[:], op=ALU.subtract),
                         reads=[Rcs, Ree], writes=[Rcsm])
                    einv, Reinv = tmp("einv")
                    edec, Redec = tmp("edec")
                    eprev, Reprev = tmp("eprev")
                    S.op("act", lambda e: e.activation(out=einv[:], in_=cs[:], func=AF.Exp, scale=CDEC), reads=[Rcs],
                         writes=[Reinv])
                    S.op("act", lambda e: e.activation(out=edec[:], in_=cs[:], func=AF.Exp, scale=-CDEC), reads=[Rcs],
                         writes=[Redec])
                    S.op("act", lambda e: e.activation(out=eprev[:], in_=csm[:], func=AF.Exp, scale=-CDEC), reads=[Rcsm],
                         writes=[Reprev])
                    S.op("dve", lambda e, j=j: e.tensor_copy(
                        out=WL[:, j, :], in_=edec[:].rearrange("p (c t) -> p c t", t=128)[:, :, 127]), reads=[Redec],
                        writes=[RWL])
                    kk, Rkk = tmp("kk")
                    S.op("dve", lambda e, j=j: e.tensor_scalar(out=kk[:], in0=k_[:], scalar1=P0[:, P_KK + j:P_KK + j + 1],
                                                               scalar2=None, op0=ALU.mult), reads=[Rk, RP0], writes=[Rkk])
                    kk2, Rkk2 = tmp("kk2", BF16)
                    S.op("pool", lambda e: e.tensor_tensor(out=kk2[:], in0=kk[:], in1=kk[:], op=ALU.mult), reads=[Rkk],
                         writes=[Rkk2])
                    zs, Rzs = slot()
                    S.op("pe", lambda e, zs=zs: e.matmul(zs, lhsT=BD, rhs=kk2[:], start=True, stop=True),
                         reads=[Rcm, Rkk2], writes=[Rzs])
                    rn, Rrn = tmp("rn")
                    S.op("act", lambda e, zs=zs: e.activation(out=rn[:], in_=zs, func=AF.Ln, bias=1e-24), reads=[Rzs],
                         writes=[Rrn])
                    S.op("act", lambda e: e.activation(out=rn[:], in_=rn[:], func=AF.Exp, scale=-0.5), reads=[Rrn],
                         writes=[Rrn])
                    S.op("dve", lambda e: e.tensor_tensor(out=kk[:], in0=kk[:], in1=rn[:], op=ALU.mult), reads=[Rkk, Rrn],
                         writes=[Rkk])
                    km, Rkm = tmp("km")
                    S.op("dve", lambda e, j=j: e.tensor_scalar(out=km[:], in0=aa[:], scalar1=-1.0,
                                                               scalar2=P0[:, P_KA + j:P_KA + j + 1], op0=ALU.add,
                                                               op1=ALU.mult), reads=[Raa, RP0], writes=[Rkm])
                    S.op("dve", lambda e: e.scalar_tensor_tensor(out=km[:], in0=km[:], scalar=1.0, in1=k_[:], op0=ALU.add,
                                                                 op1=ALU.mult), reads=[Rkm, Rk], writes=[Rkm])
                    S.op("dve", lambda e, art=art: e.tensor_tensor(
                        out=art[:, :, 0, :], in0=kk[:].rearrange("p (c t) -> p c t", t=128),
                        in1=eprev[:].rearrange("p (c t) -> p c t", t=128), op=ALU.mult), reads=[Rkk, Reprev], writes=[Rart])
                    S.op("pool", lambda e, art=art: e.tensor_tensor(
                        out=art[:, :, 1, :], in0=r_[:].rearrange("p (c t) -> p c t", t=128),
                        in1=edec[:].rearrange("p (c t) -> p c t", t=128), op=ALU.mult), reads=[Rr, Redec], writes=[Rart])
                    tb, Rtb = tmp("tb")
                    S.op("pool", lambda e: e.tensor_tensor(out=tb[:], in0=kk[:], in1=aa[:], op=ALU.mult), reads=[Rkk, Raa],
                         writes=[Rtb])
                    S.op("dve", lambda e, bt=bt: e.tensor_tensor(out=bt[:], in0=tb[:], in1=einv[:], op=ALU.mult),
                         reads=[Rtb, Reinv], writes=[Rbt])
                    S.op("pool", lambda e, kt=kt: e.tensor_tensor(out=kt[:], in0=km[:], in1=einv[:], op=ALU.mult),
                         reads=[Rkm, Reinv], writes=[Rkt])
                    btm, Rbtm = BTm[j]
                    ktm, Rktm = KTm[j]
                    for h in range(2):
                        S.op("dve", lambda e, h=h, btm=btm, bt=bt: e.tensor_scalar(
                            out=btm[:, h, :], in0=bt[:], scalar1=hmk[:, h:h + 1], scalar2=None, op0=ALU.mult),
                            reads=[Rbt, Rhmk], writes=[Rbtm])
                        S.op("pool", lambda e, h=h, ktm=ktm, kt=kt: e.tensor_scalar(
                            out=ktm[:, h, :], in0=kt[:], scalar1=hmk[:, h:h + 1], scalar2=None, op0=ALU.mult),
                            reads=[Rkt, Rhmk], writes=[Rktm])
                    rkr, Rrkr = tmp("rkr", BF16)
                    S.op("dve", lambda e, j=j: e.scalar_tensor_tensor(out=rkr[:], in0=r_[:],
                                                                      scalar=P0[:, P_RK + j:P_RK + j + 1], in1=km[:],
                                                                      op0=ALU.mult, op1=ALU.mult), reads=[Rr, Rkm, RP0],
                         writes=[Rrkr])
                    for c in range(NCH):
                        S.op("pe", lambda e, c=c, j=j: e.matmul(BKD[:, 384 + c * 8 + 2 * j:384 + c * 8 + 2 * j + 2],
                                                                lhsT=rkr[:, c * 128:(c + 1) * 128], rhs=IND, start=True,
                                                                stop=True), reads=[Rrkr, Rcm], writes=[Rbon])
                    vb, Rvb = tmp("vb", BF16)
                    S.op("act", lambda e: e.copy(out=vb[:], in_=v_[:]), reads=[Rv], writes=[Rvb])
                    for c in range(NCH):
                        tm, Rtm = TM[c]
                        for i, (srct, Rs) in enumerate(((kt, Rkt), (bt, Rbt), (vb, Rvb))):
                            S.op("pe", lambda e, i=i, c=c, srct=srct: e.transpose(
                                pT[:, i, :], srct[:, c * 128:(c + 1) * 128], idb[:]), reads=[Rs, Ridb], writes=[RpT])
                        S.op("act", lambda e, tm=tm, j=j: e.copy(out=tm[:, :, j, :], in_=pT[:, 0:3, :]), reads=[RpT],
                             writes=[Rtm])
                S.op("act", lambda e: e.copy(out=bons[:].rearrange("p c h -> p (c h)"), in_=BKD[:, 384:384 + NCH * 8]),
                     reads=[Rbon], writes=[Rbons])

                if DBG.get("dummy_alloc"):
                    for j_ in range(4):
                        tmp(f"M0_{j_}", BF16, [128, 2, 128]); tmp(f"NB_{j_}", BF16, [128, 2, 256]); tmp(f"NK_{j_}", BF16, [128, 2, 256])
                if LV < 4:
                    continue
                for c in range(NCH):
                    tm, Rtm = TM[c]
                    chain = []
                    for j in range(4):
                        art, Rart = ART[j]
                        bt, Rbt = BTt[j]
                        kt, Rkt = KTt[j]
                        Hf, RHf = Hs[j]
                        Hh, RHh = Hm[j]
                        btm, Rbtm = BTm[j]
                        ktm, Rktm = KTm[j]
                        M0, RM0 = tmp(f"M0_{j}", BF16, [128, 2, 128])
                        NB, RNB = tmp(f"NB_{j}", BF16, [128, 2, 256])
                        NK, RNK = tmp(f"NK_{j}", BF16, [128, 2, 256])
                        pa, Rpa = BKC[:, 0:2, :], RBKC[0]
                        for h in range(2):
                            po = h * 64
                            v_ = DBG.get("p1var", 0)
                            if v_ == 3:
                                if c == 0 and j == 0 and h == 0:
                                    S.op("pe", lambda e, h=h, po=po, art=art, bt=bt, c=c: e.matmul(
                                        GP[:, 0:128], lhsT=bt[0:64, 0:128], rhs=bt[0:64, 0:128],
                                        start=True, stop=True), reads=[Rbt], writes=[RGP])
                                continue
                            if v_ in (5, 6, 7):
                                if (v_ == 5 and c == 0) or (v_ == 6 and j == 0) or (v_ == 7 and h == 0):
                                    S.op("pe", lambda e, h=h, po=po, art=art, bt=bt, c=c: e.matmul(
                                        GP[:, 0:128], lhsT=bt[po:po + 64, 0:128], rhs=bt[po:po + 64, 0:128],
                                        start=True, stop=True), reads=[Rbt], writes=[RGP])
                                continue
                            if v_ == 4:
                                if c == 0 and j == 0 and h == 0:
                                    S.op("pe", lambda e, h=h, po=po, art=art, bt=bt, c=c: e.matmul(
                                        GP[:, 0:128], lhsT=idb[:], rhs=idb[:],
                                        start=True, stop=True), reads=[Ridb], writes=[RGP])
                                continue
                            if v_ == 1:
                                S.op("pe", lambda e, h=h, po=po, art=art, bt=bt, c=c: e.matmul(
                                    BKC[:, h, :], lhsT=bt[po:po + 64, c * 128:(c + 1) * 128], rhs=bt[po:po + 64, c * 128:(c + 1) * 128],
                                    start=True, stop=True), reads=[Rbt], writes=[Rpa])
                                continue
                            if v_ == 2:
                                S.op("pe", lambda e, h=h, po=po, art=art, bt=bt, c=c: e.matmul(
                                    GP[:, h * 128:(h + 1) * 128], lhsT=art[po:po + 64, c, 0, :], rhs=bt[po:po + 64, c * 128:(c + 1) * 128],
                                    start=True, stop=True), reads=[Rart, Rbt], writes=[RGP])
                                continue
                            S.op("pe", lambda e, h=h, po=po, art=art, btm=btm, c=c: e.matmul(
                                BKC[:, h, :], lhsT=art[:, c, 0, :], rhs=btm[:, h, c * 128:(c + 1) * 128],
                                start=True, stop=True), reads=[Rart, Rbtm], writes=[Rpa])
                        if DBG.get('l0_n', 9) < 1:
                            continue
                        S.op("dve", lambda e, M0=M0: e.scalar_tensor_tensor(
                            out=M0[:], in0=BKC[:, 0:2, :], scalar=-1.0, in1=maskL.unsqueeze(1).to_broadcast([128, 2, 128]),
                            op0=ALU.mult, op1=ALU.mult), reads=[Rpa, Rcm], writes=[RM0])
                        if DBG.get('l0_n', 9) < 2:
                            continue
                        for h in range(2):
                            po = h * 64
                            S.op("pe", lambda e, h=h, po=po, art=art, btm=btm, c=c: e.matmul(
                                BKA[:, h, :], lhsT=btm[:, h, c * 128:(c + 1) * 128],
                                rhs=art[:, c, :, :].rearrange("p a t -> p (a t)"), start=True, stop=True),
                                reads=[Rart, Rbtm], writes=[RBK[0], RBK[1]])
                        if DBG.get('l0_n', 9) < 3:
                            continue
                        S.op("dve", lambda e, NB=NB: e.tensor_tensor(
                            out=NB[:], in0=BKA[:], in1=maskU2.unsqueeze(1).to_broadcast([128, 2, 256]), op=ALU.mult),
                            reads=[RBK[0], RBK[1], Rcm], writes=[RNB])
                        for h in range(2):
                            po = h * 64
                            S.op("pe", lambda e, h=h, po=po, art=art, ktm=ktm, c=c: e.matmul(
                                BKB[:, h, :], lhsT=ktm[:, h, c * 128:(c + 1) * 128],
                                rhs=art[:, c, :, :].rearrange("p a t -> p (a t)"), start=True, stop=True),
                                reads=[Rart, Rktm], writes=[RBK[2], RBK[3]])
                        S.op("dve", lambda e, NK=NK: e.tensor_tensor(
                            out=NK[:], in0=BKB[:], in1=maskU3.unsqueeze(1).to_broadcast([128, 2, 256]), op=ALU.mult),
                            reads=[RBK[2], RBK[3], Rcm], writes=[RNK])
                        SL = DBG.get('l0_sub', 9)
                        if SL < 2:
                            continue
                        Q, RQ = tmp(f"Q_{j}", BF16, [128, 2, 128])
                        S.op("pool", lambda e, Q=Q, NB=NB: e.tensor_tensor(
                            out=Q[:], in0=NB[:, :, 0:128], in1=idb[:].unsqueeze(1).to_broadcast([128, 2, 128]), op=ALU.add),
                            reads=[RNB, Ridb], writes=[RQ])
                        Mc, RMc = M0, RM0
                        McT, RMcT = NB[:, :, 0:128], RNB
                        for i in range(1, 7):
                            Mn, RMn = tmp(f"M{i % 2}_{j}", BF16, [128, 2, 128])
                            for h in range(2):
                                S.op("pe", lambda e, h=h, Mc=Mc, McT=McT: e.matmul(
                                    BKC[:, 2 + h, :], lhsT=McT[:, h, :], rhs=Mc[:, h, :], start=True, stop=True),
                                    reads=[RMc, RMcT], writes=[RBKC[1]])
                            S.op("act", lambda e, Mn=Mn: e.copy(out=Mn[:], in_=BKC[:, 2:4, :]), reads=[RBKC[1]],
                                 writes=[RMn])
                            if i < 6:
                                MnT, RMnT = tmp(f"MT{i % 2}_{j}", BF16, [128, 2, 128])
                                for h in range(2):
                                    S.op("pe", lambda e, h=h, Mc=Mc, McT=McT: e.matmul(
                                        BKC[:, h, :], lhsT=Mc[:, h, :], rhs=McT[:, h, :], start=True, stop=True),
                                        reads=[RMc, RMcT], writes=[RBKC[0]])
                                S.op("act", lambda e, MnT=MnT: e.copy(out=MnT[:], in_=BKC[:, 0:2, :]), reads=[RBKC[0]],
                                     writes=[RMnT])
                            qs, Rqs = slot()
                            qsv = qs.rearrange("p (a t) -> p a t", t=128)
                            for h in range(2):
                                S.op("pe", lambda e, h=h, Mn=Mn, Q=Q, qsv=qsv: e.matmul(
                                    qsv[:, h, :], lhsT=Mn[:, h, :], rhs=Q[:, h, :], start=True, stop=True),
                                    reads=[RMn, RQ], writes=[Rqs])
                            S.op("dve", lambda e, Q=Q, qsv=qsv: e.tensor_tensor(out=Q[:], in0=qsv, in1=Q[:], op=ALU.add),
                                 reads=[Rqs, RQ], writes=[RQ])
                            Mc, RMc = Mn, RMn
                            if i < 6:
                                McT, RMcT = MnT[:], RMnT
                        chain.append((art, NK, NB, Q, RNK, RNB, RQ, Rart))

                    Xp = BKA[:].rearrange("p a b -> p (a b)").rearrange("p (j h v) -> p j h v", j=4, h=2)
                    Up = BKB[:].rearrange("p a b -> p (a b)").rearrange("p (j h v) -> p j h v", j=4, h=2)
                    dHp = BKC[:].rearrange("p a (h v) -> p a h v", h=2)
                    RXp, RUp, RdHp = [RBK[0], RBK[1]], [RBK[2], RBK[3]], [RBKC[0], RBKC[1]]
                    for j in range(4):
                        art, NK, NB, Q, RNK, RNB, RQ, Rart = chain[j]
                        for h in range(2):
                            po = h * 64
                            S.op("pe", lambda e: e.matmul(Xp[:, j, h, :], lhsT=art[:, c, 0, :], rhs=HmA[:, j, h, :],
                                                          start=True, stop=False), reads=[Rart, RHmA], writes=RXp)
                            S.op("pe", lambda e: e.matmul(Xp[:, j, h, :], lhsT=NK[:, h, 0:128], rhs=tm[:, 2, j, po:po + 64],
                                                          start=False, stop=True), reads=[RNK, Rtm], writes=RXp)
                    S.op("act", lambda e: e.copy(out=XbA[:], in_=Xp), reads=RXp, writes=[RXbA])
                    for j in range(4):
                        art, NK, NB, Q, RNK, RNB, RQ, Rart = chain[j]
                        for h in range(2):
                            S.op("pe", lambda e: e.matmul(Up[:, j, h, :], lhsT=Q[:, h, :], rhs=XbA[:, j, h, :], start=True,
                                                          stop=True), reads=[RQ, RXbA], writes=RUp)
                    S.op("act", lambda e: e.mul(out=UnA[:], in_=Up, mul=-1.0), reads=RUp, writes=[RUnA])
                    for j in range(4):
                        art, NK, NB, Q, RNK, RNB, RQ, Rart = chain[j]
                        for h in range(2):
                            po = h * 64
                            hd = 2 * j + h
                            S.op("pe", lambda e: e.matmul(YT[:, hd, :], lhsT=art[:, c, 1, :], rhs=HmA[:, j, h, :], start=True,
                                                          stop=False), reads=[Rart, RHmA], writes=[RYT])
                            S.op("pe", lambda e: e.matmul(YT[:, hd, :], lhsT=NK[:, h, 128:256], rhs=tm[:, 2, j, po:po + 64],
                                                          start=False, stop=False), reads=[RNK, Rtm], writes=[RYT])
                            S.op("pe", lambda e: e.matmul(YT[:, hd, :], lhsT=NB[:, h, 128:256], rhs=UnA[:, j, h, :],
                                                          start=False, stop=True), reads=[RNB, RUnA], writes=[RYT])
                    for j in range(4):
                        for h in range(2):
                            po = h * 64
                            S.op("pe", lambda e: e.matmul(dHp[:, j, h, :], lhsT=tm[:, 0, j, :], rhs=tm[:, 2, j, po:po + 64],
                                                          start=True, stop=False), reads=[Rtm], writes=RdHp)
                            S.op("pe", lambda e: e.matmul(dHp[:, j, h, :], lhsT=tm[:, 1, j, :], rhs=UnA[:, j, h, :],
                                                          start=False, stop=True), reads=[Rtm, RUnA], writes=RdHp)
                    for h in range(2):
                        po = h * 64
                        S.op("dve", lambda e: e.tensor_tensor(out=HfA[po:po + 64, :, :], in0=dHp[po:po + 64, :, h, :],
                                                              in1=HfA[po:po + 64, :, :], op=ALU.add), reads=RdHp + [RHfA],
                             writes=[RHfA])
                    S.op("dve", lambda e: e.tensor_tensor(out=HfA[:], in0=HfA[:],
                                                          in1=WL[:, :, c:c + 1].to_broadcast([128, 4, 64]), op=ALU.mult),
                         reads=[RHfA, RWL], writes=[RHfA])
                    for h in range(2):
                        S.op("act", lambda e: e.activation(out=HmA[:, :, h, :], in_=HfA[:], func=AF.Copy,
                                                           scale=hmk[:, h:h + 1]), reads=[RHfA, Rhmk], writes=[RHmA])

                    if LV < 5:
                        continue
                    ysb, Rysb = tmp("ysb", F32, [128, 8, 64])
                    st8, Rst8 = tmp("st8", F32, [128, 16])
                    S.op("act", lambda e: e.copy(out=ysb[:], in_=YT[:]), reads=[RYT], writes=[Rysb])
                    S.op("dve", lambda e: e.tensor_reduce(out=st8[:, 0:8], in_=ysb[:], axis=AX.X, op=ALU.add), reads=[Rysb],
                         writes=[Rst8])
                    S.op("dve", lambda e: e.tensor_scalar(out=st8[:, 0:8], in0=st8[:, 0:8], scalar1=1.0 / 64, scalar2=None,
                                                          op0=ALU.mult), reads=[Rst8], writes=[Rst8])
                    S.op("dve", lambda e: e.tensor_tensor(out=ysb[:], in0=ysb[:],
                                                          in1=st8[:, 0:8].unsqueeze(2).to_broadcast([128, 8, 64]),
                                                          op=ALU.subtract), reads=[Rysb, Rst8], writes=[Rysb])
                    ysq, Rysq = tmp("ysq", F32, [128, 8, 64])
                    S.op("pool", lambda e: e.tensor_tensor(out=ysq[:], in0=ysb[:], in1=ysb[:], op=ALU.mult), reads=[Rysb],
                         writes=[Rysq])
                    S.op("dve", lambda e: e.tensor_reduce(out=st8[:, 8:16], in_=ysq[:], axis=AX.X, op=ALU.add), reads=[Rysq],
                         writes=[Rst8])
                    sv, Rsv = tmp("sv", F32, [128, 8])
                    S.op("dve", lambda e: e.tensor_copy(out=sv[:], in_=st8[:, 8:16]), reads=[Rst8], writes=[Rsv])
                    rstd_ops(S, sv, Rsv, n=64, eps=64e-5)
                    S.op("dve", lambda e: e.tensor_tensor(out=ysb[:], in0=ysb[:],
                                                          in1=sv[:].unsqueeze(2).to_broadcast([128, 8, 64]), op=ALU.mult),
                         reads=[Rysb, Rsv], writes=[Rysb])
                    yf = ysb[:].rearrange("p h v -> p (h v)")
                    S.op("dve", lambda e: e.tensor_tensor(out=yf, in0=yf, in1=lnw[:, 0, :], op=ALU.mult), reads=[Rysb, Rlnw],
                         writes=[Rysb])
                    S.op("pool", lambda e: e.tensor_tensor(out=yf, in0=yf, in1=lnw[:, 1, :], op=ALU.add), reads=[Rysb, Rlnw],
                         writes=[Rysb])
                    bv, Rbv = tmp("bv", F32, [128, 8, 64])
                    S.op("pool", lambda e, tm=tm, c=c: e.tensor_tensor(
                        out=bv[:], in0=tm[:, 2, :, :].rearrange("p j (h v) -> p (j h) v", v=64),
                        in1=bons[:, c, :].unsqueeze(2).to_broadcast([128, 8, 64]), op=ALU.mult), reads=[Rtm, Rbons],
                        writes=[Rbv])
                    S.op("dve", lambda e: e.tensor_tensor(out=ysb[:], in0=ysb[:], in1=bv[:], op=ALU.add), reads=[Rysb, Rbv],
                         writes=[Rysb])
                    S.op("pe", lambda e, c=c: e.matmul(GP[:], lhsT=sgd[:, c * 128:(c + 1) * 128], rhs=wgu[:], start=True,
                                                       stop=True), reads=[Rsgd, Rwgu], writes=[RGP])
                    rwb, Rrwb = tmp("rwb", BF16, [128, 512])
                    S.op("dve", lambda e: e.tensor_tensor(out=rwb[:], in0=GP[:], in1=yf, op=ALU.mult), reads=[RGP, Rysb],
                         writes=[Rrwb])
                    for jj in range(4):
                        S.op("pe", lambda e, jj=jj: e.transpose(pT[:, 4 + jj, :], rwb[:, jj * 128:(jj + 1) * 128], idb[:]),
                             reads=[Rrwb, Ridb], writes=[RpT])
                    S.op("act", lambda e, c=c: e.copy(out=mixT[:, 0:4, c * 128:(c + 1) * 128], in_=pT[:, 4:8, :]),
                         reads=[RpT], writes=[RmixT])

                if LV < 6:
                    continue
                for jt in range(4):
                    lx, Rlx = LX[jt]
                    pp, Rpp = proj(14 + jt)
                    S.op("act", lambda e, lx=lx, pp=pp: e.copy(out=lx[:, 3:TL + 3], in_=pp), reads=[Rpp], writes=[Rlx])
                    xc, Rxc = tmp("ee")
                    cw = P_CW + 4 * jt
                    S.op("dve", lambda e, lx=lx, cw=cw, jt=jt: e.tensor_scalar(
                        out=xc[:], in0=lx[:, 3:TL + 3], scalar1=P0[:, cw + 3:cw + 4], scalar2=P0[:, P_CB + jt:P_CB + jt + 1],
                        op0=ALU.mult, op1=ALU.add), reads=[Rlx, RP0], writes=[Rxc])
                    for i in range(3):
                        S.op("dve", lambda e, lx=lx, cw=cw, i=i: e.scalar_tensor_tensor(
                            out=xc[:], in0=lx[:, i:TL + i], scalar=P0[:, cw + i:cw + i + 1], in1=xc[:], op0=ALU.mult,
                            op1=ALU.add), reads=[Rlx, RP0, Rxc], writes=[Rxc])
                    S.op("act", lambda e, lx=lx: e.copy(out=lx[:, 0:3], in_=lx[:, TL:TL + 3]), reads=[Rlx], writes=[Rlx])
                    xcb, Rxcb = tmp("kk2", BF16)
                    S.op("act", lambda e: e.copy(out=xcb[:], in_=xc[:]), reads=[Rxc], writes=[Rxcb])
                    zr, Rzr = slot()
                    S.op("pe", lambda e, zr=zr, jt=jt: e.matmul(zr, lhsT=gbd[:, jt, :], rhs=xcb[:], start=True, stop=True),
                         reads=[Rgbd, Rxcb], writes=[Rzr])
                    zi, Rzi = slot()
                    S.op("pe", lambda e, zi=zi, jt=jt: e.matmul(zi, lhsT=gbd[:, 4 + jt, :], rhs=xcb[:], start=True,
                                                                stop=True), reads=[Rgbd, Rxcb], writes=[Rzi])
                    rg, Rrg = tmp("aa")
                    ig, Rig = tmp("cs")
                    S.op("act", lambda e, zr=zr, jt=jt: e.activation(out=rg[:], in_=zr, func=AF.Sigmoid,
                                                                    bias=P0[:, P_BRG + jt:P_BRG + jt + 1]),
                         reads=[Rzr, RP0], writes=[Rrg])
                    S.op("act", lambda e, zi=zi, jt=jt: e.activation(out=ig[:], in_=zi, func=AF.Sigmoid,
                                                                    bias=P0[:, P_BIG + jt:P_BIG + jt + 1]),
                         reads=[Rzi, RP0], writes=[Rig])
                    th, Rth = tmp("csm")
                    a2, Ra2 = tmp("einv")
                    at_, Rat_ = tmp("edec")
                    S.op("act", lambda e, jt=jt: e.activation(out=th[:], in_=rg[:], func=AF.Tanh,
                                                             scale=spt[:, 4 + jt:5 + jt]), reads=[Rrg, Rspt], writes=[Rth])
                    S.op("act", lambda e, jt=jt: e.activation(out=a2[:], in_=rg[:], func=AF.Exp,
                                                             scale=spt[:, 12 + jt:13 + jt]), reads=[Rrg, Rspt], writes=[Ra2])
                    S.op("act", lambda e, jt=jt: e.activation(out=at_[:], in_=rg[:], func=AF.Exp,
                                                             scale=spt[:, 8 + jt:9 + jt]), reads=[Rrg, Rspt], writes=[Rat_])
                    S.op("dve", lambda e: e.scalar_tensor_tensor(out=a2[:], in0=a2[:], scalar=1.0, in1=th[:], op0=ALU.add,
                                                                 op1=ALU.mult), reads=[Ra2, Rth], writes=[Ra2])
                    S.op("act", lambda e: e.activation(out=a2[:], in_=a2[:], func=AF.Sqrt), reads=[Ra2], writes=[Ra2])
                    S.op("pool", lambda e: e.tensor_tensor(out=xc[:], in0=xc[:], in1=ig[:], op=ALU.mult), reads=[Rxc, Rig],
                         writes=[Rxc])
                    S.op("dve", lambda e: e.tensor_tensor(out=xc[:], in0=xc[:], in1=a2[:], op=ALU.mult), reads=[Rxc, Ra2],
                         writes=[Rxc])
                    hs, Rhs = tmp("eprev")
                    S.op("dve", lambda e, jt=jt: e.tensor_tensor_scan(out=hs[:], data0=at_[:], data1=xc[:],
                                                                      initial=hprev[:, jt:jt + 1], op0=ALU.mult,
                                                                      op1=ALU.add), reads=[Rat_, Rxc, Rhprev], writes=[Rhs])
                    S.op("dve", lambda e, jt=jt: e.tensor_copy(out=hprev[:, jt:jt + 1], in_=hs[:, TL - 1:TL]), reads=[Rhs],
                         writes=[Rhprev])
                    pg, Rpg = proj(18 + jt)
                    lg, Rlg = tmp("kk")
                    sqg, Rsqg = tmp("rn")
                    S.op("act", lambda e, pg=pg: e.copy(out=lg[:], in_=pg), reads=[Rpg], writes=[Rlg])
                    S.op("act", lambda e, pg=pg: e.activation(out=sqg[:], in_=pg, func=AF.Square), reads=[Rpg], writes=[Rsqg])
                    S.op("dve", lambda e: e.tensor_scalar(out=sqg[:], in0=sqg[:], scalar1=0.044715, scalar2=1.0, op0=ALU.mult,
                                                          op1=ALU.add), reads=[Rsqg], writes=[Rsqg])
                    S.op("pool", lambda e: e.tensor_tensor(out=sqg[:], in0=sqg[:], in1=lg[:], op=ALU.mult), reads=[Rsqg, Rlg],
                         writes=[Rsqg])
                    S.op("act", lambda e: e.activation(out=sqg[:], in_=sqg[:], func=AF.Sigmoid, scale=1.5957691216),
                         reads=[Rsqg], writes=[Rsqg])
                    S.op("pool", lambda e: e.tensor_tensor(out=sqg[:], in0=sqg[:], in1=lg[:], op=ALU.mult), reads=[Rsqg, Rlg],
                         writes=[Rsqg])
                    S.op("dve", lambda e, jt=jt: e.tensor_tensor(out=mixT[:, 4 + jt, :], in0=sqg[:], in1=hs[:], op=ALU.mult),
                         reads=[Rsqg, Rhs], writes=[RmixT])

                if LV < 7:
                    continue
                n = 0
                for s in range(NCH):
                    for dh in range(2):
                        po_, Rpo_ = slot()
                        pov = po_
                        for kc in range(8):
                            S.op("pe", lambda e, kc=kc, s=s, dh=dh: e.matmul(
                                GP[:], lhsT=mixT[:, kc, s * 128:(s + 1) * 128], rhs=wo[:, kc, dh * 512:(dh + 1) * 512],
                                start=(kc == 0), stop=(kc == 7)), reads=[RmixT, Rwo[kc]], writes=[RGP])
                        S.op("dve", lambda e, s=s, dh=dh, xt=xt: e.tensor_tensor(
                            out=xt[:, s, dh * 512:(dh + 1) * 512], in0=GP[:], in1=xt[:, s, dh * 512:(dh + 1) * 512],
                            op=ALU.add), reads=[RGP, Rxt], writes=[Rxt])
                S.dma("pool", lambda e, xt=xt, a=base + t0: e.dma_start(
                    out=dst[a:a + TL, :].rearrange("(s p) d -> p s d", p=128), in_=xt[:]), reads=[Rxt], writes=[])
        if DBG.get("verbose"):
            print("L0 sbuf bytes remaining", nc.sbuf_bytes_remaining, "ops", S.n_ops, {e: len(v) for e, v in S.prog.items()})
        S.flush()


def l0_host(inputs):
    f = lambda a: np.asarray(a, np.float32)
    p0 = np.zeros((128, NP0), np.float32)
    p0[:, P_MU:P_MU + 14] = fm(inputs["mu_shift"][0])
    p0[:, P_W0:P_W0 + 4] = fm(inputs["w_decay0"][0])
    p0[:, P_AB:P_AB + 4] = fm(inputs["a_bias"][0])
    p0[:, P_KK:P_KK + 4] = fm(inputs["k_k"][0])
    p0[:, P_KA:P_KA + 4] = fm(inputs["k_a"][0])
    p0[:, P_RK:P_RK + 4] = fm(f(inputs["r_k"][0]).reshape(-1))
    p0[:, P_CB:P_CB + 4] = fm(inputs["conv_b"][0])
    p0[:, P_BRG:P_BRG + 4] = fm(f(inputs["b_rgate"][0]).reshape(-1))
    p0[:, P_BIG:P_BIG + 4] = fm(f(inputs["b_igate"][0]).reshape(-1))
    p0[:, P_LAM:P_LAM + 4] = fm(inputs["lru_lambda"][0])
    cw = f(inputs["conv_w"][0])
    for jt in range(4):
        p0[:, P_CW + 4 * jt:P_CW + 4 * jt + 4] = cw[:, jt * 128:(jt + 1) * 128].T
    p0[:, P_G:P_G + 8] = fm(inputs["norm_mix"][0])
    lora = np.concatenate([f(inputs["w_decay_up"][0]), f(inputs["w_a_up"][0])], axis=0)
    gbd = np.zeros((128, 8, 128), np.float32)
    for gi, w in enumerate((f(inputs["w_rgate"][0]), f(inputs["w_igate"][0]))):
        for jt in range(4):
            gbd[0:64, gi * 4 + jt, 0:64] = w[2 * jt]
            gbd[64:128, gi * 4 + jt, 64:128] = w[2 * jt + 1]
    lnx = np.stack([f(inputs["lnx_w"][0]), f(inputs["lnx_b"][0])], axis=0)
    i = np.arange(128)
    low = (i[:, None] > i[None, :]).astype(np.float32)
    up_s = (i[:, None] < i[None, :]).astype(np.float32)
    up_i = (i[:, None] <= i[None, :]).astype(np.float32)
    bd = np.zeros((128, 128), np.float32)
    bd[:64, :64] = 1.0
    bd[64:, 64:] = 1.0
    ind = np.zeros((128, 2), np.float32)
    ind[:64, 0] = 1.0
    ind[64:, 1] = 1.0
    masks = np.concatenate([low, -up_s, up_i, up_s, up_i, bd, ind], axis=1)
    return {"w_in0": np.ascontiguousarray(f(inputs["w_in0"][0])), "w_out0": np.ascontiguousarray(f(inputs["w_out0"][0])),
            "p0": p0, "lora0": np.ascontiguousarray(lora), "w_g_up0": np.ascontiguousarray(f(inputs["w_g_up"][0])),
            "gates_bd0": gbd, "lnx0": np.ascontiguousarray(lnx), "c_masks0": np.ascontiguousarray(masks)}
```
